# Optimizing a Trainium2 kernel written in Bass

```python
import math
import jax
import jax.numpy as jnp
from jax import lax
import numpy as np


D_MODEL = 1024
BATCH = 32
SEQ = 2048
DEPTH = 2

CHUNK = 64
D_MIX = D_MODEL
HEAD_DIM = 64
D_RWKV = D_MIX // 2
D_SB = D_MIX - D_RWKV
H_RWKV = D_RWKV // HEAD_DIM
H_SB = D_SB // HEAD_DIM
DECAY_LORA = 64
ICLR_LORA = 64
GATE_LORA = 128
VRES_LORA = 32
C_RWKV = 3 * D_RWKV + DECAY_LORA + ICLR_LORA + GATE_LORA
C_IN = C_RWKV + 3 * D_SB
SB_BLOCK = 128
N_EXPERTS = 32
TOP_K = 4
D_FF_EXPERT = D_MODEL
SWIGLU_ALPHA = 1.702
SWIGLU_LIMIT = 7.0
MOE_BLOCK = 128
NORM_EPS = 1e-6
GN_EPS = 1e-5 * HEAD_DIM
L2_EPS = 1e-12

kernel_name = 'hybrid_rwkv7_stickbreak_moe_adaln'


def rms_norm(x, g):
    xf = x.astype(jnp.float32)
    y = xf * lax.rsqrt(jnp.mean(jnp.square(xf), axis=-1, keepdims=True) + NORM_EPS)
    return (y * g.astype(jnp.float32)).astype(x.dtype)


def token_shift(z, mu):
    z_prev = jnp.pad(z[:, :-1], ((0, 0), (1, 0), (0, 0)))
    return z + (z_prev - z) * mu


def split_heads(t, n_heads):
    bsz, seq, _ = t.shape
    return t.reshape(bsz, seq, n_heads, HEAD_DIM)


def rwkv7_recurrence(r, decay, k, v, kk, a):
    bsz, seq, nh, hd = r.shape
    n_chunks = seq // CHUNK

    def to_chunks(t):
        return jnp.transpose(t, (1, 0, 2, 3)).reshape(n_chunks, CHUNK, bsz, nh, hd)

    def step(state, inp):
        r_t, w_t, k_t, v_t, kk_t, a_t = inp
        s_kk = jnp.einsum('bhvk,bhk->bhv', state, kk_t)
        state = (state * w_t[:, :, None, :]
                 - s_kk[..., None] * (kk_t * a_t)[:, :, None, :]
                 + v_t[..., None] * k_t[:, :, None, :])
        return state, jnp.einsum('bhvk,bhk->bhv', state, r_t)

    def chunk_step(state, chunk):
        return lax.scan(step, state, chunk)

    s0 = jnp.zeros((bsz, nh, hd, hd), jnp.float32)
    xs = tuple(to_chunks(t) for t in (r, decay, k, v, kk, a))
    _, y = lax.scan(chunk_step, s0, xs)
    return jnp.transpose(y.reshape(seq, bsz, nh, hd), (1, 0, 2, 3))


def rwkv7_time_mix(z, v_first, mu, decay_w0, decay_up, iclr_a0, iclr_up, gate_up,
                   k_k, k_a, r_k, lnx_w, lnx_b, vres):
    bsz, seq, _ = z.shape
    zs = token_shift(z, mu)
    r = zs[..., :D_RWKV]
    k = zs[..., D_RWKV:2 * D_RWKV]
    v = zs[..., 2 * D_RWKV:3 * D_RWKV]
    o = 3 * D_RWKV
    w_lo = zs[..., o:o + DECAY_LORA]
    a_lo = zs[..., o + DECAY_LORA:o + DECAY_LORA + ICLR_LORA]
    g_lo = zs[..., o + DECAY_LORA + ICLR_LORA:]
    w_pre = (decay_w0 + jnp.tanh(w_lo) @ decay_up).astype(jnp.float32)
    log_decay = -jnp.exp(-jax.nn.softplus(-w_pre) - 0.5)
    a = jax.nn.sigmoid(iclr_a0 + a_lo @ iclr_up)
    g = jax.nn.sigmoid(g_lo) @ gate_up
    if vres is None:
        v_first = v
    else:
        vres_down, vres_up, vres_b = vres
        v = v + (v_first - v) * jax.nn.sigmoid(vres_b + (v @ vres_down) @ vres_up)
    kk = split_heads((k * k_k).astype(jnp.float32), H_RWKV)
    kk = kk / jnp.maximum(jnp.sqrt(jnp.sum(jnp.square(kk), axis=-1, keepdims=True)), L2_EPS)
    k = k * (1.0 + (a - 1.0) * k_a)
    rh = split_heads(r.astype(jnp.float32), H_RWKV)
    kh = split_heads(k.astype(jnp.float32), H_RWKV)
    vh = split_heads(v.astype(jnp.float32), H_RWKV)
    ah = split_heads(a.astype(jnp.float32), H_RWKV)
    wh = split_heads(jnp.exp(log_decay), H_RWKV)
    y = rwkv7_recurrence(rh, wh, kh, vh, kk, ah)
    mean = jnp.mean(y, axis=-1, keepdims=True)
    var = jnp.mean(jnp.square(y - mean), axis=-1, keepdims=True)
    y = ((y - mean) * lax.rsqrt(var + GN_EPS)).reshape(bsz, seq, D_RWKV)
    y = y * lnx_w.astype(jnp.float32) + lnx_b.astype(jnp.float32)
    bonus = jnp.sum(rh * kh * r_k.astype(jnp.float32), axis=-1, keepdims=True) * vh
    out = (y + bonus.reshape(bsz, seq, D_RWKV)) * g.astype(jnp.float32)
    return out.astype(z.dtype), v_first


def stick_breaking_attention(q, k, v, q_norm_g, k_norm_g, out_g):
    bsz, seq, _ = q.shape
    qh = jnp.transpose(rms_norm(split_heads(q, H_SB), q_norm_g).astype(jnp.float32), (0, 2, 1, 3))
    kh = jnp.transpose(rms_norm(split_heads(k, H_SB), k_norm_g).astype(jnp.float32), (0, 2, 1, 3))
    vh = jnp.transpose(split_heads(v, H_SB).astype(jnp.float32), (0, 2, 1, 3))
    scale = 1.0 / math.sqrt(HEAD_DIM)
    blocks = []
    for blk in range(seq // SB_BLOCK):
        t0 = blk * SB_BLOCK
        t1 = t0 + SB_BLOCK
        logits = jnp.einsum('bhqd,bhkd->bhqk', qh[:, :, t0:t1], kh[:, :, :t1]) * scale
        mask = jnp.arange(t1)[None, :] < jnp.arange(t0, t1)[:, None]
        log_keep = jnp.where(mask, jax.nn.log_sigmoid(-logits), 0.0)
        log_between = lax.cumsum(log_keep, axis=3, reverse=True) - log_keep
        weight = jnp.where(mask, jnp.exp(jax.nn.log_sigmoid(logits) + log_between), 0.0)
        blocks.append(jnp.einsum('bhqk,bhkd->bhqd', weight, vh[:, :, :t1]))
    o = jnp.transpose(jnp.concatenate(blocks, axis=2), (0, 2, 1, 3))
    o = rms_norm(o, out_g.reshape(H_SB, HEAD_DIM))
    return o.reshape(bsz, seq, D_SB).astype(q.dtype)


def moe_ffn(h, router_w, router_b, w1, b1, w2, b2):
    bsz, seq, d = h.shape
    n_tok = bsz * seq
    xt = h.reshape(n_tok, d)
    logits = xt.astype(jnp.float32) @ router_w.astype(jnp.float32) + router_b.astype(jnp.float32)
    top_logit, top_idx = lax.top_k(logits, TOP_K)
    top_gate = jax.nn.softmax(top_logit, axis=-1)
    n_assign = n_tok * TOP_K
    flat_e = top_idx.reshape(-1)
    flat_tok = jnp.arange(n_assign, dtype=jnp.int32) // TOP_K
    order = jnp.argsort(flat_e, stable=True)
    e_sorted = flat_e[order]
    counts = jnp.bincount(flat_e, length=N_EXPERTS)
    padded = (counts + MOE_BLOCK - 1) // MOE_BLOCK * MOE_BLOCK
    padded_end = jnp.cumsum(padded)
    padded_start = padded_end - padded
    start = jnp.cumsum(counts) - counts
    dest = padded_start[e_sorted] + jnp.arange(n_assign, dtype=jnp.int32) - start[e_sorted]
    n_rows = n_assign + N_EXPERTS * MOE_BLOCK
    n_blocks = n_rows // MOE_BLOCK
    row_tok = jnp.zeros((n_rows,), jnp.int32).at[dest].set(flat_tok[order])
    row_gate = jnp.zeros((n_rows,), h.dtype).at[dest].set(top_gate.reshape(-1)[order].astype(h.dtype))
    block_expert = jnp.minimum(
        jnp.searchsorted(padded_end, jnp.arange(n_blocks, dtype=jnp.int32) * MOE_BLOCK, side='right'),
        N_EXPERTS - 1)

    def expert_block(args):
        tok, gate, e = args
        xb = xt[tok]
        hid = xb @ w1[e] + b1[e]
        glu = jnp.minimum(hid[:, :D_FF_EXPERT], SWIGLU_LIMIT)
        lin = jnp.clip(hid[:, D_FF_EXPERT:], -SWIGLU_LIMIT, SWIGLU_LIMIT)
        act = glu * jax.nn.sigmoid(SWIGLU_ALPHA * glu) * (lin + 1.0)
        return (act @ w2[e] + b2[e]) * gate[:, None]

    y = lax.map(expert_block, (row_tok.reshape(n_blocks, MOE_BLOCK),
                               row_gate.reshape(n_blocks, MOE_BLOCK), block_expert))
    out = jax.ops.segment_sum(y.reshape(n_rows, d), row_tok, num_segments=n_tok)
    return out.reshape(bsz, seq, d)


def setup_inputs(seed: int = 0) -> dict:
    key = jax.random.key(seed)
    ks = iter(jax.random.split(key, 40))

    def normal(shape, scale):
        return jax.random.normal(next(ks), shape, jnp.float32) * scale

    def gain(shape, s=0.02):
        return 1.0 + normal(shape, s)

    L = DEPTH
    LV = max(DEPTH - 1, 0)
    return {
        'x': normal((BATCH, SEQ, D_MODEL), 1.0),
        'c': normal((BATCH, D_MODEL), 1.0),
        'norm1_g': gain((L, D_MODEL)),
        'norm2_g': gain((L, D_MODEL)),
        'ada_w': normal((L, D_MODEL, 6 * D_MODEL), 0.5 * D_MODEL ** -0.5),
        'ada_b': normal((L, 6 * D_MODEL), 0.02),
        'w_in': normal((L, D_MODEL, C_IN), D_MODEL ** -0.5),
        'shift_mu': jax.random.uniform(next(ks), (L, C_RWKV), jnp.float32),
        'decay_w0': jax.random.uniform(next(ks), (L, D_RWKV), jnp.float32, -1.5, 1.5),
        'decay_up': normal((L, DECAY_LORA, D_RWKV), 0.5 * DECAY_LORA ** -0.5),
        'iclr_a0': normal((L, D_RWKV), 0.1),
        'iclr_up': normal((L, ICLR_LORA, D_RWKV), 0.5 * ICLR_LORA ** -0.5),
        'gate_up': normal((L, GATE_LORA, D_RWKV), GATE_LORA ** -0.5),
        'k_k': gain((L, D_RWKV), 0.1),
        'k_a': gain((L, D_RWKV), 0.1),
        'r_k': normal((L, H_RWKV, HEAD_DIM), 0.1),
        'lnx_w': gain((L, D_RWKV)),
        'lnx_b': normal((L, D_RWKV), 0.02),
        'vres_down': normal((LV, D_RWKV, VRES_LORA), D_RWKV ** -0.5),
        'vres_up': normal((LV, VRES_LORA, D_RWKV), 0.5 * VRES_LORA ** -0.5),
        'vres_b': normal((LV, D_RWKV), 0.1),
        'q_norm_g': gain((L, HEAD_DIM)),
        'k_norm_g': gain((L, HEAD_DIM)),
        'sb_out_g': gain((L, D_SB)),
        'w_out': normal((L, D_MIX, D_MODEL), D_MIX ** -0.5),
        'router_w': normal((L, D_MODEL, N_EXPERTS), D_MODEL ** -0.5),
        'router_b': normal((L, N_EXPERTS), 0.01),
        'exp_w1': normal((L, N_EXPERTS, D_MODEL, 2 * D_FF_EXPERT), D_MODEL ** -0.5),
        'exp_b1': normal((L, N_EXPERTS, 2 * D_FF_EXPERT), 0.02),
        'exp_w2': normal((L, N_EXPERTS, D_FF_EXPERT, D_MODEL), D_FF_EXPERT ** -0.5),
        'exp_b2': normal((L, N_EXPERTS, D_MODEL), 0.02),
    }


def reference(x, c, norm1_g, norm2_g, ada_w, ada_b, w_in, shift_mu, decay_w0, decay_up,
              iclr_a0, iclr_up, gate_up, k_k, k_a, r_k, lnx_w, lnx_b, vres_down, vres_up,
              vres_b, q_norm_g, k_norm_g, sb_out_g, w_out, router_w, router_b,
              exp_w1, exp_b1, exp_w2, exp_b2):
    cond = jax.nn.silu(c)
    v_first = None
    for l in range(DEPTH):
        mod = cond @ ada_w[l] + ada_b[l]
        shift1, scale1, gate1, shift2, scale2, gate2 = jnp.split(mod[:, None, :], 6, axis=-1)
        h = rms_norm(x, norm1_g[l]) * (1.0 + scale1) + shift1
        z = h @ w_in[l]
        vres = None if l == 0 else (vres_down[l - 1], vres_up[l - 1], vres_b[l - 1])
        y_rwkv, v_first = rwkv7_time_mix(
            z[..., :C_RWKV], v_first, shift_mu[l], decay_w0[l], decay_up[l], iclr_a0[l],
            iclr_up[l], gate_up[l], k_k[l], k_a[l], r_k[l], lnx_w[l], lnx_b[l], vres)
        z_sb = z[..., C_RWKV:]
        y_sb = stick_breaking_attention(
            z_sb[..., :D_SB], z_sb[..., D_SB:2 * D_SB], z_sb[..., 2 * D_SB:],
            q_norm_g[l], k_norm_g[l], sb_out_g[l])
        mixed = jnp.concatenate([y_rwkv, y_sb], axis=-1) @ w_out[l]
        x = x + gate1 * mixed
        h2 = rms_norm(x, norm2_g[l]) * (1.0 + scale2) + shift2
        x = x + gate2 * moe_ffn(h2, router_w[l], router_b[l], exp_w1[l], exp_b1[l],
                                exp_w2[l], exp_b2[l])
    return x
```

```python
import math
from contextlib import ExitStack
import numpy as np
import concourse.bass as bass
import concourse.mybir as mybir
from concourse.bass_utils import run_bass_kernel_spmd

F32 = mybir.dt.float32
BF16 = mybir.dt.bfloat16
AF = mybir.ActivationFunctionType
ALU = mybir.AluOpType
AX = mybir.AxisListType

ENG = ("pe", "act", "dve", "pool", "sp")
SEM_ROT = 30000


class Op:
    __slots__ = ("eng", "fn", "deps", "raw", "signal", "is_dma", "token", "pos")

    def __init__(self, eng, fn, is_dma):
        self.eng = eng
        self.fn = fn
        self.deps = []
        self.raw = ()
        self.signal = False
        self.is_dma = is_dma
        self.token = None
        self.pos = 0


class Prog:
    def __init__(self, nc, stack):
        self.nc = nc
        self.stack = stack
        self.ops = []
        self.by_eng = {e: [] for e in ENG}
        self.last_w = {}
        self.readers = {}
        self.dma_counts = {}
        self.pending_bar = {e: [] for e in ENG}
        self.last_dma_tokens = {}
        self.dma_rr = {e: 0 for e in ENG}
        self.dma_last_on_sem = {}
        self.flushed = {e: 0 for e in ENG}
        self.cnt = {e: 0 for e in ENG}
        self.waited = {e: {} for e in ENG}
        self.sem_cache = {}

    def op(self, eng, fn, reads=(), writes=(), dma_sem=None):
        o = Op(eng, fn, dma_sem is not None)
        deps = set()
        raw = set()
        for k in reads:
            w = self.last_w.get(k)
            if w is not None:
                deps.add(w)
                raw.add(w)
        for k in writes:
            w = self.last_w.get(k)
            if w is not None:
                deps.add(w)
            for r in self.readers.get(k, ()):
                deps.add(r)
        for b in self.pending_bar[eng]:
            deps.add(b)
            raw.add(b)
        self.pending_bar[eng] = []
        deps.discard(o)
        o.deps = list(deps)
        o.raw = raw
        if dma_sem is not None:
            K = 16
            dma_sem = "%s%d" % (eng, self.dma_rr[eng] % K)
            self.dma_rr[eng] += 1
            prev = self.dma_last_on_sem.get(dma_sem)
            if prev is not None:
                o.deps.append(prev)
            self.dma_last_on_sem[dma_sem] = o
            c = self.dma_counts.get(dma_sem, 0) + 16
            self.dma_counts[dma_sem] = c
            o.token = (dma_sem, c)
            self.last_dma_tokens[dma_sem] = o
        for k in reads:
            self.readers.setdefault(k, []).append(o)
        for k in writes:
            self.last_w[k] = o
            self.readers[k] = []
        o.pos = len(self.by_eng[eng])
        self.ops.append(o)
        self.by_eng[eng].append(o)
        return o

    def barrier(self):
        lasts = []
        for e in ENG:
            for o in reversed(self.by_eng[e][self.flushed[e]:]):
                if not o.is_dma:
                    o.signal = True
                    lasts.append(o)
                    break
        lasts += list(self.last_dma_tokens.values())
        for e in ENG:
            self.pending_bar[e] = list(set(self.pending_bar[e]) | set(lasts))
        self.last_w = {}
        self.readers = {}

    @staticmethod
    def _needs_wait(o, d):
        if d.is_dma:
            return True
        if d.eng != o.eng:
            return True
        if o.is_dma:
            return True
        if d.eng != "pe" and d in o.raw and (o.pos - d.pos) <= 2:
            return True
        return False

    def get_sem(self, name):
        if name not in self.sem_cache:
            self.sem_cache[name] = self.stack.enter_context(self.nc.semaphore(name))
        return self.sem_cache[name]

    def flush(self, final=False):
        nc = self.nc
        new = {e: self.by_eng[e][self.flushed[e]:] for e in ENG}
        for e in ENG:
            for o in new[e]:
                for d in o.deps:
                    if not d.is_dma and self._needs_wait(o, d):
                        assert not (d.fn is None and not d.signal), "dependency on an already-emitted unsignalled op"
                        d.signal = True
        for e in ENG:
            for o in new[e]:
                if o.is_dma:
                    if not isinstance(o.token[0], str) or not o.token[0].startswith("d_"):
                        o.token = ("d_%s" % str(o.token[0]), o.token[1])
                elif o.signal:
                    self.cnt[e] += 1
                    c = self.cnt[e]
                    o.token = ("e_%s_%d" % (e, (c - 1) // SEM_ROT), (c - 1) % SEM_ROT + 1)
                else:
                    o.token = None
        for e in ENG:
            for o in new[e]:
                if o.token is not None:
                    self.get_sem(o.token[0])
        engs = {"pe": nc.tensor, "act": nc.scalar, "dve": nc.vector, "pool": nc.gpsimd, "sp": nc.sync}
        with nc.Block() as block:
            def run(e):
                eng = engs[e]
                waited = self.waited[e]
                for o in new[e]:
                    need = {}
                    for d in o.deps:
                        if not self._needs_wait(o, d):
                            continue
                        sk, v = d.token
                        if waited.get(sk, 0) >= v:
                            continue
                        if need.get(sk, 0) < v:
                            need[sk] = v
                    for sk, v in need.items():
                        eng.wait_ge(self.get_sem(sk), v)
                        waited[sk] = v
                    ins = o.fn(eng)
                    if o.is_dma:
                        ins.then_inc(self.get_sem(o.token[0]), 16)
                    elif o.signal:
                        ins.then_inc(self.get_sem(o.token[0]), 1)
                    o.fn = None

            @block.tensor
            def _(t):
                run("pe")

            @block.scalar
            def _(s):
                run("act")

            @block.vector
            def _(v):
                run("dve")

            @block.gpsimd
            def _(g):
                run("pool")

            @block.sync
            def _(s):
                run("sp")
                if final:
                    for sk, o in self.last_dma_tokens.items():
                        s.wait_ge(self.get_sem(o.token[0]), o.token[1])
        for e in ENG:
            self.flushed[e] = len(self.by_eng[e])

    def emit(self):
        self.barrier()
        self.flush(final=True)


class Buf:
    __slots__ = ("ap", "key")

    def __init__(self, ap, key):
        self.ap = ap
        self.key = key


class Arena:
    def __init__(self, ap_f32, name):
        self.ap = ap_f32
        self.W = ap_f32.shape[1]
        self.off = 0
        self.name = name
        self.gen = 0

    def reset(self):
        self.off = 0
        self.gen += 1

    def alloc(self, tag, parts, free, dtype=F32):
        n = 1
        for f in free:
            n *= f
        words = n if dtype == F32 else (n + 1) // 2
        assert self.off + words <= self.W, (self.name, tag, self.off, words, self.W)
        v = self.ap[0:parts, self.off:self.off + words]
        self.off += words
        if dtype != F32:
            v = v.bitcast(dtype)[:, 0:n]
        if len(free) == 2:
            v = v.rearrange("p (a b) -> p a b", a=free[0])
        elif len(free) == 3:
            v = v.rearrange("p (a b c) -> p a b c", a=free[0], b=free[1])
        return Buf(v, (self.name, self.gen, tag))


D = 1024
KC = 8
HD = 64
NH = 8
D_RWKV = 512
C_RWKV = 3 * D_RWKV + 64 + 64 + 128
C_IN = C_RWKV + 3 * 512
NORM_EPS = 1e-6
GN_EPS = 1e-5 * HD
ALPHA = 1.702
C0 = math.exp(-0.5)

P128 = {}
_o = 0
for _n, _w in (("ada_b", 48), ("n1g", 8), ("n2g", 8), ("mu_g", 1), ("qg", 1), ("kg", 1), ("rb", 32)):
    P128[_n] = (_o, _w)
    _o += _w
P128_W = _o
P64 = {}
_o = 0
for _n, _w in (("mu_r", 8), ("mu_k", 8), ("mu_v", 8), ("mu_w", 1), ("mu_a", 1), ("w0", 8), ("a0", 8),
               ("k_k", 8), ("k_a", 8), ("r_k", 8), ("lnw", 8), ("lnb", 8), ("vrb", 8), ("sbg", 8)):
    P64[_n] = (_o, _w)
    _o += _w
P64_W = _o


def build_program(T, NSEQ, E, DEPTH=2, debug=False):
    assert T % 512 == 0
    NTT = T // 512
    NCH = T // 64
    NG = T // 128
    nc = bass.Bass("TRN2", target_bir_lowering=False)

    def din(name, shape):
        return nc.dram_tensor(name, list(shape), F32, kind="ExternalInput").ap()

    xT_d = din("xT", [NSEQ, D, T])
    cT_d = din("cT", [128, KC, NSEQ])
    ada_w_d = din("ada_w", [DEPTH, D, 6 * D])
    w_in_d = din("w_in", [DEPTH, D, C_IN])
    w_out_d = din("w_out", [DEPTH, D, D])
    w1_d = din("exp_w1r", [DEPTH, E, 4, 128, 4096])
    w2_d = din("exp_w2r", [DEPTH, E, 2, 128, 4096])
    pk128_d = din("pk128", [DEPTH, 128, P128_W])
    pk64_d = din("pk64", [DEPTH, 64, P64_W])
    b1T_d = din("b1T", [DEPTH, 128, E * 16])
    b2_d = din("exp_b2", [DEPTH, E, D])
    rw_d = din("router_w", [DEPTH, D, E])
    dup_d = din("decay_up", [DEPTH, 64, 512])
    iup_d = din("iclr_up", [DEPTH, 64, 512])
    gup_d = din("gate_up", [DEPTH, 128, 512])
    vdn_d = din("vres_down", [max(DEPTH - 1, 1), 512, 32])
    vup_d = din("vres_up", [max(DEPTH - 1, 1), 32, 512])
    outT_d = nc.dram_tensor("outT", [NSEQ, D, T], F32, kind="ExternalOutput").ap()
    gT_scr = nc.dram_tensor("gT_scr", [E, T], F32, kind="Internal").ap()
    vf_scr = nc.dram_tensor("vf_scr", [T // 64, 64, NH * 64], F32, kind="Internal").ap()
    dbg_d = {}

    st = ExitStack()
    with st:
        def sb(name, shape, dt=F32):
            return st.enter_context(nc.sbuf_tensor(name, list(shape), dt))

        P = Prog(nc, st)

        xT = hT = wsl_t = wout_t = local_t = A = psum = None
        uid = [0]
        scopes = {"seq": None, "sub": None, "phase": None}

        def uname(n):
            uid[0] += 1
            return "%s_%d" % (n, uid[0])

        def seq_begin():
            nonlocal xT
            scopes["seq"] = ExitStack()
            xT = scopes["seq"].enter_context(nc.sbuf_tensor(uname("xT"), [128, KC, T], F32))

        def seq_end():
            P.barrier()
            scopes["seq"].close()

        def sub_begin():
            nonlocal hT, wsl_t, wout_t
            ss = scopes["sub"] = ExitStack()
            hT = ss.enter_context(nc.sbuf_tensor(uname("hT"), [128, KC, T], BF16))
            wsl_t = ss.enter_context(nc.sbuf_tensor(uname("wsl"), [128, 3 * 512 * KC], BF16))
            wout_t = ss.enter_context(nc.sbuf_tensor(uname("wout"), [64, 8, D], BF16))

        def sub_end():
            scopes["sub"].close()

        def phase_begin():
            nonlocal local_t, A, psum
            ps_ = scopes["phase"] = ExitStack()
            local_t = ps_.enter_context(nc.sbuf_tensor(uname("loc"), [128, 45 * 256], F32))
            A = Arena(local_t[:], "loc")
            psum = [ps_.enter_context(nc.psum_tensor(uname("ps"), [128, 512], F32)) for i in range(8)]

        def phase_end():
            P.barrier()
            scopes["phase"].close()
        cst_t = sb("consts", [128, 1100])
        cstb_t = sb("constsb", [128, 512], BF16)
        pk128 = sb("pk128_sb", [128, P128_W])
        pk64 = sb("pk64_sb", [64, P64_W])
        mods = sb("mods", [128, DEPTH * 48 * NSEQ])
        der = sb("derived", [128, 64])
        lora_t = sb("lora", [128, 512 * 3 + 8 * 32 + 512])
        state_t = sb("state", [64, 4 * 64])
        carry_t = sb("carry", [128, 32])

        def PS(i):
            return Buf(psum[i][:], ("ps", i))

        def mm(out, lhsT, rhs, start, stop, reads, writes):
            P.op("pe", lambda e: e.matmul(out, lhsT=lhsT, rhs=rhs, start=start, stop=stop), reads, writes)

        def tr(out, in_, ident, reads, writes):
            P.op("pe", lambda e: e.transpose(out, in_, ident), reads, writes)

        def act(out, in_, func, reads, writes, bias=None, scale=None, eng="act"):
            kw = {}
            if bias is not None:
                kw["bias"] = bias
            if scale is not None:
                kw["scale"] = scale
            P.op(eng, lambda e: e.activation(out=out, in_=in_, func=func, **kw), reads, writes)

        def tt(eng, out, in0, in1, op, reads, writes):
            P.op(eng, lambda e: e.tensor_tensor(out=out, in0=in0, in1=in1, op=op), reads, writes)

        def ts(eng, out, in0, s1, s2, op0, op1, reads, writes):
            if s2 is None:
                P.op(eng, lambda e: e.tensor_scalar(out=out, in0=in0, scalar1=s1, scalar2=None, op0=op0), reads, writes)
            else:
                P.op(eng, lambda e: e.tensor_scalar(out=out, in0=in0, scalar1=s1, scalar2=s2, op0=op0, op1=op1), reads, writes)

        def stt(out, in0, scalar, in1, op0, op1, reads, writes):
            P.op("dve", lambda e: e.scalar_tensor_tensor(out=out, in0=in0, scalar=scalar, in1=in1, op0=op0, op1=op1), reads, writes)

        def cp(eng, out, in_, reads, writes):
            if eng == "act":
                P.op("act", lambda e: e.activation(out=out, in_=in_, func=AF.Copy), reads, writes)
            else:
                P.op(eng, lambda e: e.tensor_copy(out=out, in_=in_), reads, writes)

        def memset(eng, ap, val, writes):
            P.op(eng, lambda e: e.memset(ap, val), (), writes)

        def dma(eng, out, in_, reads, writes, sem):
            P.op(eng, lambda e: e.dma_start(out=out, in_=in_), reads, writes, dma_sem=sem)

        def rsqrt(out, in_, scale, bias, reads, writes, tmp):
            act(tmp.ap, in_, AF.Ln, list(reads) + ["c_small"], [tmp.key], bias=bias[0:in_.shape[0], :], scale=scale)
            act(out, tmp.ap, AF.Exp, [tmp.key], writes, scale=-0.5)

        ones_f = Buf(cst_t[:, 0:128], "c_ones")
        ident_f = Buf(cst_t[:, 128:256], "c_ident")
        blk_f = Buf(cst_t[:, 256:384], "c_blk")
        mU = Buf(cst_t[0:64, 384:448], "c_mU")
        mL = Buf(cst_t[0:64, 448:512], "c_mL")
        mUi = Buf(cst_t[0:64, 512:576], "c_mUi")
        scanm = Buf(cst_t[0:64, 576:1088], "c_scan")
        ntri_b = Buf(cstb_t[:, 0:128], "c_ntri")
        nones_b = Buf(cstb_t[:, 128:256], "c_nones")
        zeros_b = Buf(cstb_t[:, 256:320], "c_zeros")
        memset("pool", ones_f.ap, 1.0, [ones_f.key])
        c_eps_n = cst_t[:, 1088:1089]
        c_eps_g = cst_t[:, 1089:1090]
        c_one = cst_t[:, 1090:1091]
        c_zero = cst_t[:, 1091:1092]
        memset("pool", c_eps_n, NORM_EPS, ["c_small"])
        memset("pool", c_eps_g, GN_EPS, ["c_small"])
        memset("pool", c_one, 1.0, ["c_small"])
        memset("pool", c_zero, 0.0, ["c_small"])
        memset("pool", cst_t[:, 256:384], 0.0, [blk_f.key])
        memset("pool", cst_t[0:64, 256:320], 1.0, [blk_f.key])
        memset("pool", cst_t[64:128, 320:384], 1.0, [blk_f.key])

        def asel(out, in_, pattern, cmp_op, base, cm, reads, writes, fill=0.0):
            P.op("pool", lambda e: e.affine_select(out=out, in_=in_, pattern=pattern, compare_op=cmp_op,
                                                   fill=fill, base=base, channel_multiplier=cm), reads, writes)
        asel(ident_f.ap, ones_f.ap, [[-1, 128]], ALU.is_equal, 0, 1, [ones_f.key], [ident_f.key])
        asel(mU.ap, ones_f.ap[0:64, 0:64], [[1, 64]], ALU.is_gt, 0, -1, [ones_f.key], [mU.key])
        asel(mL.ap, ones_f.ap[0:64, 0:64], [[-1, 64]], ALU.is_gt, 0, 1, [ones_f.key], [mL.key])
        asel(mUi.ap, ones_f.ap[0:64, 0:64], [[1, 64]], ALU.is_ge, 0, -1, [ones_f.key], [mUi.key])
        memset("pool", scanm.ap, 1.0, [scanm.key])
        memset("pool", scanm.ap.rearrange("p (h t) -> p h t", h=NH)[:, :, 0:1], 0.0, [scanm.key])
        memset("pool", nones_b.ap, -1.0, [nones_b.key])
        memset("pool", zeros_b.ap, 0.0, [zeros_b.key])
        asel(ntri_b.ap, nones_b.ap, [[-1, 128]], ALU.is_ge, 0, 1, [nones_b.key], [ntri_b.key])

        def slot(i):
            return Buf(wsl_t[:, i * 4096:(i + 1) * 4096].rearrange("p (k c) -> p k c", k=KC), ("wslot", i))
        slot_rr = [0]

        def load_cols(slot_b, segs, w_ap):
            off = 0
            for (c0, n) in segs:
                src = w_ap[:, c0:c0 + n].rearrange("(k p) c -> p k c", p=128)
                dma("pool", slot_b.ap[:, :, off:off + n], src, [], [slot_b.key], ("w", slot_b.key[1]))
                off += n

        sub_begin()
        phase_begin()
        ct = A.alloc("ct", 128, [KC, NSEQ])
        ctb = A.alloc("ctb", 128, [KC, NSEQ], BF16)
        dma("sp", ct.ap, cT_d, [], [ct.key], "misc")
        act(ctb.ap, ct.ap, AF.Silu, [ct.key], [ctb.key])
        mods_v = mods[:].rearrange("p (l j s) -> p l j s", l=DEPTH, j=48)
        mods_b = Buf(mods_v, "mods")
        for l in range(DEPTH):
            pkb = A.alloc("pkb%d" % l, 128, [48])
            dma("sp", pkb.ap, pk128_d[l, :, 0:48], [], [pkb.key], "misc")
            for gi in range(12):
                sl = slot(slot_rr[0] % 3)
                slot_rr[0] += 1
                load_cols(sl, [(gi * 512, 512)], ada_w_d[l])
                pb = PS(gi % 2)
                for j in range(4):
                    for kc in range(KC):
                        mm(pb.ap[:, j * NSEQ:(j + 1) * NSEQ], sl.ap[:, kc, j * 128:(j + 1) * 128], ctb.ap[:, kc, :],
                           kc == 0, kc == KC - 1, [sl.key, ctb.key], [pb.key])
                for j in range(4):
                    jj = gi * 4 + j
                    act(mods_v[:, l, jj, :], pb.ap[:, j * NSEQ:(j + 1) * NSEQ], AF.Identity, [pb.key, pkb.key],
                        [mods_b.key], bias=pkb.ap[:, jj:jj + 1])
        phase_end()
        sub_end()

        def pk(name, c=None):
            o, w = P128[name]
            return pk128[:, o:o + w] if c is None else pk128[:, o + c:o + c + 1]

        def p64(name, h=None):
            o, w = P64[name]
            return pk64[:, o:o + w] if h is None else pk64[:, o + h:o + h + 1]

        def p64b(name, n):
            o, w = P64[name]
            return pk64[:, o:o + w].unsqueeze(2).to_broadcast([64, w, n])

        der_b = Buf(der[:], "der")
        omk_t = Buf(carry_t[0:64, 24:32], "omk")

        def load_layer_params(l, s):
            dma("sp", pk128[:], pk128_d[l], [], ["pk128"], "misc")
            dma("sp", pk64[:], pk64_d[l], [], ["pk64"], "misc")
            mv = mods_v[:, l, :, s]
            o1, _ = P128["n1g"]
            o2, _ = P128["n2g"]
            stt(der[:, 0:8], mv[:, 8:16], 1.0, pk128[:, o1:o1 + 8], ALU.add, ALU.mult, ["mods", "pk128"], ["der"])
            cp("dve", der[:, 8:16], mv[:, 0:8], ["mods"], ["der"])
            cp("dve", der[:, 16:24], mv[:, 16:24], ["mods"], ["der"])
            stt(der[:, 24:32], mv[:, 32:40], 1.0, pk128[:, o2:o2 + 8], ALU.add, ALU.mult, ["mods", "pk128"], ["der"])
            cp("dve", der[:, 32:40], mv[:, 24:32], ["mods"], ["der"])
            cp("dve", der[:, 40:48], mv[:, 40:48], ["mods"], ["der"])
            ts("dve", der[:, 48:49], pk("qg"), 0.125, None, ALU.mult, None, ["pk128"], ["der"])
            ts("dve", omk_t.ap, p64("k_a"), -1.0, 1.0, ALU.mult, ALU.add, ["pk64"], [omk_t.key])
            dma("sp", lora_t[0:64, 0:512], dup_d[l], [], ["lora"], "misc")
            dma("sp", lora_t[0:64, 512:1024], iup_d[l], [], ["lora"], "misc")
            dma("sp", lora_t[:, 1024:1536], gup_d[l], [], ["lora"], "misc")
            if l > 0:
                dma("sp", lora_t[0:64, 1536:1792].rearrange("p (h c) -> p h c", h=NH),
                    vdn_d[l - 1].rearrange("(h p) c -> p h c", p=64), [], ["lora"], "misc")
                dma("sp", lora_t[0:32, 1792:2304], vup_d[l - 1], [], ["lora"], "misc")

        def rms_modulate(gcol, shcol, tmpA, emit_tile, tmpH=None):
            for tti in range(NTT):
                tsl = slice(tti * 512, (tti + 1) * 512)
                pss = PS(7)
                for kc in range(KC):
                    sq = tmpA[kc % 2]
                    act(sq.ap, xT[:, kc, tsl], AF.Square, [("xT", kc, tti)], [sq.key])
                    mm(pss.ap, ones_f.ap, sq.ap, kc == 0, kc == KC - 1, [ones_f.key, sq.key], [pss.key])
                rstd = tmpA[2]
                rsqrt(rstd.ap, pss.ap, 1.0 / D, c_eps_n, [pss.key], [rstd.key], tmpA[3])
                tiles = []
                for kc in range(KC):
                    t1 = tmpH[kc] if tmpH is not None else tmpA[4 + kc % 2]
                    tt("dve", t1.ap, xT[:, kc, tsl], rstd.ap, ALU.mult, [("xT", kc, tti), rstd.key], [t1.key])
                    ts("pool", t1.ap, t1.ap, der[:, gcol + kc:gcol + kc + 1], der[:, shcol + kc:shcol + kc + 1],
                       ALU.mult, ALU.add, [t1.key, "der"], [t1.key])
                    cp("act", hT[:, kc, tsl], t1.ap, [t1.key], [("hT", kc, tti)])
                    tiles.append(t1)
                if emit_tile is not None:
                    emit_tile(tti, tiles)

        def attention_sublayer(l, s):
            sub_begin()
            phase_begin()
            tmpA = [A.alloc("nt%d" % i, 128, [512]) for i in range(6)]
            rms_modulate(0, 8, tmpA, None)
            phase_end()

            wl = w_in_d[l]
            woutb = Buf(wout_t[:], "wout")
            dma("pool", wout_t[:], w_out_d[l, 0:512, :].rearrange("(h p) c -> p h c", p=64), [], [woutb.key], "wout")
            HP = 4
            for hg in range(2):
                phase_begin()
                wr = Buf(wsl_t[:, 0:KC * 1280].rearrange("p (k c) -> p k c", k=KC), ("wslot", "rwkv", hg))
                segs = [(hg * 256, 256, 0), (512 + hg * 256, 256, 256), (1024, 512, 512), (1536, 256, 1024)]
                for (c0_, n_, o_) in segs:
                    dma("pool", wr.ap[:, :, o_:o_ + n_], wl[:, c0_:c0_ + n_].rearrange("(k p) c -> p k c", p=128),
                        [], [wr.key, ("wslot", 0), ("wslot", 1), ("wslot", 2)], "wrwkv")

                def HB(tag, dt=F32, nh=HP):
                    return A.alloc(tag, 64, [nh, 64], dt)
                zb = {"r": A.alloc("zb_r", 64, [HP, 65]), "k": A.alloc("zb_k", 64, [HP, 65]), "v": A.alloc("zb_v", 64, [NH, 65])}
                zbl = A.alloc("zb_l", 64, [2, 65])
                zbg = A.alloc("zb_g", 128, [65])
                for b_ in list(zb.values()) + [zbl, zbg]:
                    memset("pool", b_.ap, 0.0, [b_.key])
                R, K = HB("R"), HB("K")
                V8 = HB("V8", nh=NH)
                V = Buf(V8.ap[:, hg * HP:(hg + 1) * HP, :], V8.key)
                T8 = HB("T8", nh=NH)
                B1, B2, B3, B4, B5, B6 = (HB("B%d" % i) for i in range(1, 7))
                lw = A.alloc("lw", 64, [2, 64])
                lg = A.alloc("lg", 128, [64])
                pcs = A.alloc("pcs", 64, [HP, 1])
                vd = A.alloc("vd", 32, [64])
                Nn, NTt, Pa, PTa = HB("N"), HB("NT"), HB("Pa"), HB("PTa")
                S_ = HB("S")
                AakT, ArkT, ArbT = HB("AakT"), HB("ArkT"), HB("ArbT")
                Vt, Ktt, Btt = HB("Vt"), HB("Ktt"), HB("Btt")
                RHS, U_ = B1, B4
                yr = A.alloc("yr", 64, [HP, 64], BF16)
                xtmp = A.alloc("xtmp", 128, [KC, 64])
                M = Buf(state_t[:].rearrange("p (h v) -> p h v", h=HP), "state")
                memset("pool", M.ap, 0.0, [M.key])
                dup = lora_t[0:64, 0:512]
                iup = lora_t[0:64, 512:1024]
                gup = lora_t[:, 1024:1536]
                vdn = lora_t[0:64, 1536:1792].rearrange("p (h c) -> p h c", h=NH)
                vup = lora_t[0:32, 1792:2304]
                idf = ident_f.ap[0:64, 0:64]
                o64 = ones_f.ap[0:64, 0:64]
                mUb = mU.ap.unsqueeze(1).to_broadcast([64, HP, 64])
                mLb = mL.ap.unsqueeze(1).to_broadcast([64, HP, 64])
                mUib = mUi.ap.unsqueeze(1).to_broadcast([64, HP, 64])
                idb = idf.unsqueeze(1).to_broadcast([64, HP, 64])
                gate1b = der[:, 16:24].unsqueeze(2).to_broadcast([128, KC, 64])
                scm = scanm.ap[:, 0:HP * 64]
                W4 = HP * 64

                def f2(b_):
                    return b_.ap.rearrange("p h t -> p (h t)")

                def pv(pb_, nh=HP):
                    return pb_.ap[0:64, 0:nh * 64].rearrange("p (h t) -> p h t", h=nh)

                def pg(name, n=64):
                    o, w = P64[name]
                    return pk64[:, o + hg * HP:o + (hg + 1) * HP].unsqueeze(2).to_broadcast([64, HP, n])

                for c in range(NCH):
                    t0 = c * 64
                    tsl = slice(t0, t0 + 64)
                    hk = [("hT", kc, t0 // 512) for kc in range(KC)]
                    pb = PS(0)
                    for gi in range(2):
                        for h in range(HP):
                            col = gi * 256 + h * 64
                            oc = gi * 256 + h * 64
                            for kc in range(KC):
                                mm(pb.ap[0:64, oc:oc + 64], wr.ap[:, kc, col:col + 64], hT[:, kc, tsl],
                                   kc == 0, kc == KC - 1, [wr.key, hk[kc]], [pb.key])
                    cp("act", zb["r"].ap[:, :, 1:65], pb.ap[0:64, 0:256].rearrange("p (h t) -> p h t", h=HP), [pb.key], [zb["r"].key])
                    cp("act", zb["k"].ap[:, :, 1:65], pb.ap[0:64, 256:512].rearrange("p (h t) -> p h t", h=HP), [pb.key], [zb["k"].key])
                    pb = PS(1)
                    for h in range(NH):
                        for kc in range(KC):
                            mm(pb.ap[0:64, h * 64:(h + 1) * 64], wr.ap[:, kc, 512 + h * 64:512 + (h + 1) * 64], hT[:, kc, tsl],
                               kc == 0, kc == KC - 1, [wr.key, hk[kc]], [pb.key])
                    cp("act", zb["v"].ap[:, :, 1:65], pv(pb, NH), [pb.key], [zb["v"].key])
                    pb = PS(3)
                    for j in range(2):
                        for kc in range(KC):
                            mm(pb.ap[0:64, j * 64:(j + 1) * 64], wr.ap[:, kc, 1024 + j * 64:1024 + (j + 1) * 64], hT[:, kc, tsl],
                               kc == 0, kc == KC - 1, [wr.key, hk[kc]], [pb.key])
                    for kc in range(KC):
                        mm(pb.ap[:, 128:192], wr.ap[:, kc, 1152:1280], hT[:, kc, tsl], kc == 0, kc == KC - 1, [wr.key, hk[kc]], [pb.key])
                    cp("act", zbl.ap[:, :, 1:65], pb.ap[0:64, 0:128].rearrange("p (h t) -> p h t", h=2), [pb.key], [zbl.key])
                    cp("act", zbg.ap[:, 1:65], pb.ap[:, 128:192], [pb.key], [zbg.key])
                    for n, dst, tmpb, mub in (("r", R, B1, pg("mu_r")), ("k", K, B1, pg("mu_k")), ("v", V8, T8, p64b("mu_v", 64))):
                        z = zb[n]
                        tt("dve", tmpb.ap, z.ap[:, :, 0:64], z.ap[:, :, 1:65], ALU.subtract, [z.key], [tmpb.key])
                        tt("pool", tmpb.ap, tmpb.ap, mub, ALU.mult, [tmpb.key, "pk64"], [tmpb.key])
                        tt("dve", dst.ap, tmpb.ap, z.ap[:, :, 1:65], ALU.add, [tmpb.key, z.key], [dst.key])
                        cp("act", z.ap[:, :, 0:1], z.ap[:, :, 64:65], [z.key], [z.key])
                    tt("dve", lw.ap, zbl.ap[:, :, 0:64], zbl.ap[:, :, 1:65], ALU.subtract, [zbl.key], [lw.key])
                    o_w, _ = P64["mu_w"]
                    tt("dve", lw.ap, lw.ap, pk64[:, o_w:o_w + 2].unsqueeze(2).to_broadcast([64, 2, 64]), ALU.mult, [lw.key, "pk64"], [lw.key])
                    tt("dve", lw.ap, lw.ap, zbl.ap[:, :, 1:65], ALU.add, [lw.key, zbl.key], [lw.key])
                    cp("act", zbl.ap[:, :, 0:1], zbl.ap[:, :, 64:65], [zbl.key], [zbl.key])
                    tt("dve", lg.ap, zbg.ap[:, 0:64], zbg.ap[:, 1:65], ALU.subtract, [zbg.key], [lg.key])
                    stt(lg.ap, lg.ap, pk("mu_g"), zbg.ap[:, 1:65], ALU.mult, ALU.add, [lg.key, zbg.key, "pk128"], [lg.key])
                    cp("act", zbg.ap[:, 0:1], zbg.ap[:, 64:65], [zbg.key], [zbg.key])
                    act(lw.ap[:, 0, :], lw.ap[:, 0, :], AF.Tanh, [lw.key], [lw.key])
                    act(lg.ap, lg.ap, AF.Tanh, [lg.key], [lg.key], scale=0.5)
                    ts("pool", lg.ap, lg.ap, 0.5, 0.5, ALU.mult, ALU.add, [lg.key], [lg.key])
                    pb = PS(4)
                    for h in range(HP):
                        hgl = hg * HP + h
                        mm(pb.ap[0:64, h * 64:(h + 1) * 64], dup[:, hgl * 64:(hgl + 1) * 64], lw.ap[:, 0, :], True, True, ["lora", lw.key], [pb.key])
                    tt("dve", B1.ap, pv(pb), pg("w0"), ALU.add, [pb.key, "pk64"], [B1.key])
                    act(B1.ap, B1.ap, AF.Tanh, [B1.key], [B1.key], scale=0.5)
                    ts("pool", B1.ap, B1.ap, 0.5, 0.5, ALU.mult, ALU.add, [B1.key], [B1.key])
                    P.op("dve", lambda e, B1=B1, B2=B2: e.tensor_tensor_scan(out=f2(B2), data0=scm, data1=f2(B1), initial=0.0,
                                                                             op0=ALU.mult, op1=ALU.add), [B1.key, scanm.key], [B2.key])
                    act(B3.ap, B2.ap, AF.Exp, [B2.key], [B3.key], scale=-C0)
                    act(B4.ap, B2.ap, AF.Exp, [B2.key], [B4.key], scale=C0)
                    tt("pool", B1.ap, B2.ap, B1.ap, ALU.subtract, [B2.key, B1.key], [B1.key])
                    act(B1.ap, B1.ap, AF.Exp, [B1.key], [B1.key], scale=-C0)
                    cp("act", pcs.ap, B3.ap[:, :, 63:64], [B3.key], [pcs.key])
                    pb = PS(5)
                    for h in range(HP):
                        hgl = hg * HP + h
                        mm(pb.ap[0:64, h * 64:(h + 1) * 64], iup[:, hgl * 64:(hgl + 1) * 64], lw.ap[:, 1, :], True, True, ["lora", lw.key], [pb.key])
                    tt("dve", B2.ap, pv(pb), pg("a0"), ALU.add, [pb.key, "pk64"], [B2.key])
                    act(B2.ap, B2.ap, AF.Tanh, [B2.key], [B2.key], scale=0.5)
                    ts("pool", B2.ap, B2.ap, 0.5, 0.5, ALU.mult, ALU.add, [B2.key], [B2.key])
                    if l == 0:
                        if hg == 0:
                            dma("sp", vf_scr[c], f2(V8), [V8.key], [("vf", c)], "vf")
                    else:
                        dma("sp", f2(B5), vf_scr[c, :, hg * HP * 64:(hg + 1) * HP * 64], [("vf", c)], [B5.key], "vf")
                        pb = PS(6)
                        for h in range(NH):
                            mm(pb.ap[0:32, 0:64], vdn[:, h, :], V8.ap[:, h, :], h == 0, h == NH - 1, ["lora", V8.key], [pb.key])
                        cp("act", vd.ap, pb.ap[0:32, 0:64], [pb.key], [vd.key])
                        pb = PS(7)
                        for h in range(HP):
                            hgl = hg * HP + h
                            mm(pb.ap[0:64, h * 64:(h + 1) * 64], vup[:, hgl * 64:(hgl + 1) * 64], vd.ap, True, True, ["lora", vd.key], [pb.key])
                        tt("dve", B6.ap, pv(pb), pg("vrb"), ALU.add, [pb.key, "pk64"], [B6.key])
                        act(B6.ap, B6.ap, AF.Tanh, [B6.key], [B6.key], scale=0.5)
                        ts("pool", B6.ap, B6.ap, 0.5, 0.5, ALU.mult, ALU.add, [B6.key], [B6.key])
                        tt("dve", B5.ap, B5.ap, V.ap, ALU.subtract, [B5.key, V.key], [B5.key])
                        tt("pool", B5.ap, B5.ap, B6.ap, ALU.mult, [B5.key, B6.key], [B5.key])
                        tt("dve", V.ap, V.ap, B5.ap, ALU.add, [V.key, B5.key], [V.key])
                    tt("pool", B5.ap, K.ap, pg("k_k"), ALU.mult, [K.key, "pk64"], [B5.key])
                    act(B6.ap, B5.ap, AF.Square, [B5.key], [B6.key])
                    pb = PS(6)
                    mm(pb.ap[0:64, 0:W4], o64, f2(B6), True, True, [ones_f.key, B6.key], [pb.key])
                    ts("dve", B6.ap, pv(pb), 1e-24, None, ALU.max, None, [pb.key], [B6.key])
                    rsqrt(B6.ap, B6.ap, 1.0, c_zero, [B6.key], [B6.key], Nn)
                    tt("dve", B5.ap, B5.ap, B6.ap, ALU.mult, [B5.key, B6.key], [B5.key])
                    stt(B6.ap, B5.ap, -1.0, B2.ap, ALU.mult, ALU.mult, [B5.key, B2.key], [B6.key])
                    tt("pool", B6.ap, B6.ap, B4.ap, ALU.mult, [B6.key, B4.key], [B6.key])
                    tt("dve", B5.ap, B5.ap, B1.ap, ALU.mult, [B5.key, B1.key], [B5.key])
                    tt("pool", B1.ap, B2.ap, pg("k_a"), ALU.mult, [B2.key, "pk64"], [B1.key])
                    tt("pool", B1.ap, B1.ap, omk_t.ap[:, hg * HP:(hg + 1) * HP].unsqueeze(2).to_broadcast([64, HP, 64]), ALU.add,
                       [B1.key, omk_t.key], [B1.key])
                    tt("dve", B1.ap, B1.ap, K.ap, ALU.mult, [B1.key, K.key], [B1.key])
                    tt("pool", B2.ap, R.ap, B1.ap, ALU.mult, [R.key, B1.key], [B2.key])
                    tt("pool", B2.ap, B2.ap, pg("r_k"), ALU.mult, [B2.key, "pk64"], [B2.key])
                    pb = PS(7)
                    mm(pb.ap[0:64, 0:W4], o64, f2(B2), True, True, [ones_f.key, B2.key], [pb.key])
                    tt("dve", B2.ap, pv(pb), V.ap, ALU.mult, [pb.key, V.key], [B2.key])
                    tt("dve", K.ap, B1.ap, B4.ap, ALU.mult, [B1.key, B4.key], [K.key])
                    tt("pool", R.ap, R.ap, B3.ap, ALU.mult, [R.key, B3.key], [R.key])
                    pb = PS(4)
                    for h in range(HP):
                        hgl = hg * HP + h
                        mm(pb.ap[0:64, h * 64:(h + 1) * 64], gup[:, hgl * 64:(hgl + 1) * 64], lg.ap, True, True, ["lora", lg.key], [pb.key])
                    cp("act", B3.ap, pv(pb), [pb.key], [B3.key])
                    for src, dstb, bank in ((V, Vt, 0), (K, Ktt, 1), (B6, Btt, 2)):
                        pb = PS(bank)
                        for h in range(HP):
                            tr(pb.ap[0:64, h * 64:(h + 1) * 64], src.ap[:, h, :], idf, [src.key, ident_f.key], [pb.key])
                        cp("act", f2(dstb), pb.ap[0:64, 0:W4], [pb.key], [dstb.key])
                    pb, pb2 = PS(3), PS(5)
                    for h in range(HP):
                        mm(pb.ap[0:64, h * 64:(h + 1) * 64], B6.ap[:, h, :], B5.ap[:, h, :], True, True, [B6.key, B5.key], [pb.key])
                        mm(pb2.ap[0:64, h * 64:(h + 1) * 64], B5.ap[:, h, :], B6.ap[:, h, :], True, True, [B6.key, B5.key], [pb2.key])
                    tt("dve", Nn.ap, pv(pb), mUb, ALU.mult, [pb.key, mU.key], [Nn.key])
                    tt("dve", NTt.ap, pv(pb2), mLb, ALU.mult, [pb2.key, mL.key], [NTt.key])
                    tt("pool", S_.ap, Nn.ap, idb, ALU.add, [Nn.key, ident_f.key], [S_.key])
                    pb = PS(6)
                    for h in range(HP):
                        mm(pb.ap[0:64, h * 64:(h + 1) * 64], K.ap[:, h, :], B5.ap[:, h, :], True, True, [K.key, B5.key], [pb.key])
                    tt("dve", AakT.ap, pv(pb), mUb, ALU.mult, [pb.key, mU.key], [AakT.key])
                    pb = PS(7)
                    for h in range(HP):
                        mm(pb.ap[0:64, h * 64:(h + 1) * 64], K.ap[:, h, :], R.ap[:, h, :], True, True, [K.key, R.key], [pb.key])
                    tt("dve", ArkT.ap, pv(pb), mUib, ALU.mult, [pb.key, mUi.key], [ArkT.key])
                    pb = PS(4)
                    for h in range(HP):
                        mm(pb.ap[0:64, h * 64:(h + 1) * 64], B6.ap[:, h, :], R.ap[:, h, :], True, True, [B6.key, R.key], [pb.key])
                    tt("dve", ArbT.ap, pv(pb), mUib, ALU.mult, [pb.key, mUi.key], [ArbT.key])
                    pb = PS(6)
                    for h in range(HP):
                        mm(pb.ap[0:64, h * 64:(h + 1) * 64], B5.ap[:, h, :], M.ap[:, h, :], True, False, [B5.key, M.key], [pb.key])
                        mm(pb.ap[0:64, h * 64:(h + 1) * 64], AakT.ap[:, h, :], Vt.ap[:, h, :], False, True, [AakT.key, Vt.key], [pb.key])
                    cp("act", f2(RHS), pb.ap[0:64, 0:W4], [pb.key], [RHS.key])
                    Pc, PTc, Pn, PTn = Nn, NTt, Pa, PTa
                    for lev in range(1, 6):
                        last = lev == 5
                        pbT = PS(lev % 2)
                        for h in range(HP):
                            mm(pbT.ap[0:64, h * 64:(h + 1) * 64], Pc.ap[:, h, :], PTc.ap[:, h, :], True, True, [Pc.key, PTc.key], [pbT.key])
                        if not last:
                            pbN = PS(2 + lev % 2)
                            for h in range(HP):
                                mm(pbN.ap[0:64, h * 64:(h + 1) * 64], PTc.ap[:, h, :], Pc.ap[:, h, :], True, True, [Pc.key, PTc.key], [pbN.key])
                        cp("act", f2(PTn), pbT.ap[0:64, 0:W4], [pbT.key], [PTn.key])
                        if not last:
                            cp("dve", f2(Pn), pbN.ap[0:64, 0:W4], [pbN.key], [Pn.key])
                        pbS = PS(5)
                        for h in range(HP):
                            mm(pbS.ap[0:64, h * 64:(h + 1) * 64], PTn.ap[:, h, :], S_.ap[:, h, :], True, True, [PTn.key, S_.key], [pbS.key])
                        tt("dve", f2(S_), f2(S_), pbS.ap[0:64, 0:W4], ALU.add, [pbS.key, S_.key], [S_.key])
                        Pc, PTc, Pn, PTn = Pn, PTn, Pc, PTc
                    pb = PS(7)
                    for h in range(HP):
                        mm(pb.ap[0:64, h * 64:(h + 1) * 64], S_.ap[:, h, :], RHS.ap[:, h, :], True, True, [S_.key, RHS.key], [pb.key])
                    cp("act", f2(U_), pb.ap[0:64, 0:W4], [pb.key], [U_.key])
                    pby = PS(4)
                    for h in range(HP):
                        o_ = pby.ap[0:64, h * 64:(h + 1) * 64]
                        mm(o_, M.ap[:, h, :], R.ap[:, h, :], True, False, [M.key, R.key], [pby.key])
                        mm(o_, Vt.ap[:, h, :], ArkT.ap[:, h, :], False, False, [Vt.key, ArkT.key], [pby.key])
                        mm(o_, U_.ap[:, h, :], ArbT.ap[:, h, :], False, True, [U_.key, ArbT.key], [pby.key])
                    pb = PS(0)
                    for h in range(HP):
                        o_ = pb.ap[0:64, h * 64:(h + 1) * 64]
                        mm(o_, Ktt.ap[:, h, :], Vt.ap[:, h, :], True, False, [Ktt.key, Vt.key], [pb.key])
                        mm(o_, Btt.ap[:, h, :], U_.ap[:, h, :], False, True, [Btt.key, U_.key], [pb.key])
                    tt("dve", M.ap, M.ap, pv(pb), ALU.add, [M.key, pb.key], [M.key])
                    tt("pool", M.ap, M.ap, pcs.ap.to_broadcast([64, HP, 64]), ALU.mult, [M.key, pcs.key], [M.key])
                    cp("act", f2(B1), pby.ap[0:64, 0:W4], [pby.key], [B1.key])
                    pb = PS(1)
                    mm(pb.ap[0:64, 0:W4], o64, f2(B1), True, True, [ones_f.key, B1.key], [pb.key])
                    stt(f2(B1), pb.ap[0:64, 0:W4], -1.0 / 64, f2(B1), ALU.mult, ALU.add, [pb.key, B1.key], [B1.key])
                    act(B4.ap, B1.ap, AF.Square, [B1.key], [B4.key])
                    pb = PS(2)
                    mm(pb.ap[0:64, 0:W4], o64, f2(B4), True, True, [ones_f.key, B4.key], [pb.key])
                    rsqrt(f2(B4), pb.ap[0:64, 0:W4], 1.0 / 64, c_eps_g, [pb.key], [B4.key], Buf(f2(Nn), Nn.key))
                    tt("dve", B1.ap, B1.ap, B4.ap, ALU.mult, [B1.key, B4.key], [B1.key])
                    tt("pool", B1.ap, B1.ap, pg("lnw"), ALU.mult, [B1.key, "pk64"], [B1.key])
                    tt("pool", B1.ap, B1.ap, pg("lnb"), ALU.add, [B1.key, "pk64"], [B1.key])
                    tt("dve", B1.ap, B1.ap, B2.ap, ALU.add, [B1.key, B2.key], [B1.key])
                    tt("dve", yr.ap, B1.ap, B3.ap, ALU.mult, [B1.key, B3.key], [yr.key])
                    pb = PS(3)
                    for dc in range(KC):
                        for h in range(HP):
                            mm(pb.ap[:, dc * 64:(dc + 1) * 64], wout_t[:, hg * HP + h, dc * 128:(dc + 1) * 128], yr.ap[:, h, :],
                               h == 0, h == HP - 1, [woutb.key, yr.key], [pb.key])
                    tt("dve", xtmp.ap, pb.ap.rearrange("p (k t) -> p k t", k=KC), gate1b, ALU.mult, [pb.key, "der"], [xtmp.key])
                    xk = [("xT", kc, t0 // 512) for kc in range(KC)]
                    tt("pool", xT[:, :, tsl], xT[:, :, tsl], xtmp.ap, ALU.add, [xtmp.key] + xk, xk)
                phase_end()

            pass
            phase_begin()
            dma("pool", wout_t[:], w_out_d[l, 512:1024, :].rearrange("(h p) c -> p h c", p=64), [], [woutb.key], "wout")
            qT = A.alloc("qT", 128, [T], BF16)
            kT = A.alloc("kT", 128, [T], BF16)
            Vs = A.alloc("Vs", 128, [NG, 128], BF16)
            qraw = [A.alloc("qraw%d" % i, 128, [512]) for i in range(2)]
            qsq = [A.alloc("qsq%d" % i, 128, [512]) for i in range(2)]
            etmp = [A.alloc("etmp%d" % i, 128, [512]) for i in range(2)]
            spb = [A.alloc("spb%d" % i, 128, [512], BF16) for i in range(2)]
            wTb = [A.alloc("wTb%d" % i, 128, [512], BF16) for i in range(2)]
            spsum = A.alloc("spsum", 128, [512])
            spsb = [A.alloc("spsb%d" % i, 128, [512], BF16) for i in range(2)]
            osb = A.alloc("osb", 64, [512])
            osq = A.alloc("osq", 64, [512])
            ors = A.alloc("ors", 64, [512])
            otm = A.alloc("otm", 64, [512])
            ysb = A.alloc("ysb", 64, [2, 512], BF16)
            it = [0]
            for hp in range(4):
                sl = slot(slot_rr[0] % 3)
                slot_rr[0] += 1
                load_cols(sl, [(C_RWKV + hp * 128, 128), (C_RWKV + 512 + hp * 128, 128), (C_RWKV + 1024 + hp * 128, 128)], wl)
                for which, dst, gcol in ((0, qT, der[:, 48:49]), (1, kT, pk("kg"))):
                    for tti in range(NTT):
                        tsl = slice(tti * 512, (tti + 1) * 512)
                        pb = PS(which * 2 + tti % 2)
                        for kc in range(KC):
                            mm(pb.ap, sl.ap[:, kc, which * 128:(which + 1) * 128], hT[:, kc, tsl], kc == 0, kc == KC - 1,
                               [sl.key, ("hT", kc, tti)], [pb.key])
                        qr, qs = qraw[tti % 2], qsq[tti % 2]
                        cp("act", qr.ap, pb.ap, [pb.key], [qr.key])
                        act(qs.ap, qr.ap, AF.Square, [qr.key], [qs.key])
                        pb2 = PS(4 + tti % 2)
                        mm(pb2.ap, blk_f.ap, qs.ap, True, True, [blk_f.key, qs.key], [pb2.key])
                        rsqrt(qs.ap, pb2.ap, 1.0 / 64, c_eps_n, [pb2.key], [qs.key], etmp[tti % 2])
                        tt("dve", qr.ap, qr.ap, qs.ap, ALU.mult, [qr.key, qs.key], [qr.key])
                        ts("dve", dst.ap[:, tsl], qr.ap, gcol, None, ALU.mult, None, [qr.key, "der", "pk128"], [(dst.key, tti)])
                for g in range(NG):
                    pb = PS(6 + g % 2)
                    for kc in range(KC):
                        mm(pb.ap[:, 0:128], hT[:, kc, g * 128:(g + 1) * 128], sl.ap[:, kc, 256:384], kc == 0, kc == KC - 1,
                           [sl.key, ("hT", kc, g // 4)], [pb.key])
                    cp("act", Vs.ap[:, g, :], pb.ap[:, 0:128], [pb.key], [(Vs.key, g)])
                qk_all = [(qT.key, i) for i in range(NTT)] + [(kT.key, i) for i in range(NTT)]
                for QT in range(NTT):
                    for hh in range(2):
                        pbase = hh * 64
                        po = PS(4 + hh)
                        qsl = slice(QT * 512, (QT + 1) * 512)
                        mm(po.ap[0:64, :], zeros_b.ap, kT.ap[:, qsl], True, False, [zeros_b.key] + qk_all, [po.key])
                        nkb = 4 * QT + 4
                        memset("pool", spsum.ap, 0.0, [spsum.key])
                        for sbk in range(nkb - 1, -1, -1):
                            c0 = max(0, 128 * sbk - 512 * QT)
                            diag = sbk >= 4 * QT
                            i2 = it[0] % 2
                            it[0] += 1
                            ksl = slice(sbk * 128, (sbk + 1) * 128)
                            q_ap = qT.ap[pbase:pbase + 64, QT * 512 + c0:(QT + 1) * 512]
                            k_ap = kT.ap[pbase:pbase + 64, ksl]
                            ps1 = PS(i2)
                            mm(ps1.ap[:, c0:512], k_ap, q_ap, True, True, qk_all, [ps1.key])
                            et, sp, wT_, sps = etmp[i2], spb[i2], wTb[i2], spsb[i2]
                            act(et.ap[:, c0:512], ps1.ap[:, c0:512], AF.Exp, [ps1.key], [et.key])
                            act(sp.ap[:, c0:512], et.ap[:, c0:512], AF.Ln, [et.key, "c_small"], [sp.key], bias=c_one)
                            if diag:
                                asel(sp.ap[:, c0:c0 + 128], sp.ap[:, c0:c0 + 128], [[1, 128]], ALU.is_gt, 0, -1, [sp.key], [sp.key])
                            ps2 = PS(2 + i2)
                            first = sbk == nkb - 1
                            mm(ps2.ap[:, c0:512], k_ap, q_ap, True, False, qk_all, [ps2.key])
                            mm(ps2.ap[:, c0:512], ntri_b.ap, sp.ap[:, c0:512], False, first, [ntri_b.key, sp.key], [ps2.key])
                            if not first:
                                cp("act", sps.ap[:, c0:512], spsum.ap[:, c0:512], [spsum.key], [sps.key])
                                mm(ps2.ap[:, c0:512], nones_b.ap, sps.ap[:, c0:512], False, True, [nones_b.key, sps.key], [ps2.key])
                            act(wT_.ap[:, c0:512], ps2.ap[:, c0:512], AF.Exp, [ps2.key], [wT_.key])
                            if diag:
                                asel(wT_.ap[:, c0:c0 + 128], wT_.ap[:, c0:c0 + 128], [[1, 128]], ALU.is_gt, 0, -1, [wT_.key], [wT_.key])
                            mm(po.ap[0:64, c0:512], Vs.ap[:, sbk, pbase:pbase + 64], wT_.ap[:, c0:512], False, sbk == 0,
                               [(Vs.key, sbk), wT_.key], [po.key])
                            if sbk > 0:
                                tt("dve", spsum.ap[:, c0:512], spsum.ap[:, c0:512], sp.ap[:, c0:512], ALU.add, [spsum.key, sp.key], [spsum.key])
                        cp("act", osb.ap, po.ap[0:64, :], [po.key], [osb.key])
                        act(osq.ap, osb.ap, AF.Square, [osb.key], [osq.key])
                        pb = PS(6)
                        mm(pb.ap[0:64, :], ones_f.ap[0:64, 0:64], osq.ap, True, True, [ones_f.key, osq.key], [pb.key])
                        rsqrt(ors.ap, pb.ap[0:64, :], 1.0 / 64, c_eps_n, [pb.key], [ors.key], otm)
                        tt("dve", osb.ap, osb.ap, ors.ap, ALU.mult, [osb.key, ors.key], [osb.key])
                        ts("dve", ysb.ap[:, hh, :], osb.ap, p64("sbg", hp * 2 + hh), None, ALU.mult, None, [osb.key, "pk64"], [(ysb.key, hh)])
                    for dc in range(KC):
                        pb = PS(6 + dc % 2)
                        for hh in range(2):
                            mm(pb.ap, wout_t[:, hp * 2 + hh, dc * 128:(dc + 1) * 128], ysb.ap[:, hh, :], hh == 0, hh == 1,
                               [woutb.key, (ysb.key, hh)], [pb.key])
                        stt(xT[:, dc, qsl], pb.ap, der[:, 16 + dc:17 + dc], xT[:, dc, qsl], ALU.mult, ALU.add,
                            [pb.key, "der", ("xT", dc, QT)], [("xT", dc, QT)])
            phase_end()
            sub_end()

        def moe_sublayer(l, s):
            sub_begin()
            phase_begin()
            tmpA = [A.alloc("nt%d" % i, 128, [512]) for i in range(4)]
            tmpH = [A.alloc("nh%d" % i, 128, [512]) for i in range(KC)]
            rw = A.alloc("rw", 128, [KC, E])
            dma("sp", rw.ap, rw_d[l].rearrange("(k p) e -> p k e", p=128), [], [rw.key], "misc")
            b2s = A.alloc("b2s", E, [D])
            dma("sp", b2s.ap, b2_d[l], [], [b2s.key], "misc")
            lgp = PS(6)
            lgv = lgp.ap[:, 0:NG * E].rearrange("p (g e) -> p g e", g=NG)

            def router_tile(tti, tiles):
                for g4 in range(4):
                    g = tti * 4 + g4
                    for kc in range(KC):
                        mm(lgv[:, g, :], tiles[kc].ap[:, g4 * 128:(g4 + 1) * 128], rw.ap[:, kc, :], kc == 0, kc == KC - 1,
                           [tiles[kc].key, rw.key], [lgp.key])
            rms_modulate(24, 32, tmpA, router_tile, tmpH)
            lgb = A.alloc("lgb", 128, [NG, E])
            gm = A.alloc("gm", 128, [NG, E])
            top8 = A.alloc("top8", 128, [NG, 8])
            gsum = A.alloc("gsum", 128, [NG, 1])
            o_rb, _ = P128["rb"]
            tt("dve", lgb.ap, lgv, pk128[:, o_rb:o_rb + E].unsqueeze(1).to_broadcast([128, NG, E]), ALU.add, [lgp.key, "pk128"], [lgb.key])
            for g in range(NG):
                P.op("dve", lambda e, g=g: e.max(out=top8.ap[:, g, :], in_=lgb.ap[:, g, :]), [lgb.key], [top8.key])
            tt("dve", gm.ap, lgb.ap, top8.ap[:, :, 3:4].to_broadcast([128, NG, E]), ALU.is_ge, [lgb.key, top8.key], [gm.key])
            tt("dve", lgb.ap, lgb.ap, top8.ap[:, :, 0:1].to_broadcast([128, NG, E]), ALU.subtract, [lgb.key, top8.key], [lgb.key])
            act(lgb.ap, lgb.ap, AF.Exp, [lgb.key], [lgb.key])
            tt("dve", gm.ap, gm.ap, lgb.ap, ALU.mult, [gm.key, lgb.key], [gm.key])
            P.op("dve", lambda e: e.tensor_reduce(out=gsum.ap[:, :, 0], in_=gm.ap, axis=AX.X, op=ALU.add), [gm.key], [gsum.key])
            P.op("dve", lambda e: e.reciprocal(out=gsum.ap, in_=gsum.ap), [gsum.key], [gsum.key])
            tt("dve", gm.ap, gm.ap, gsum.ap.to_broadcast([128, NG, E]), ALU.mult, [gm.key, gsum.key], [gm.key])
            GT = A.alloc("GT", E, [T])
            for tti in range(NTT):
                pb = PS(tti % 2)
                for g4 in range(4):
                    g = tti * 4 + g4
                    tr(pb.ap[0:E, g4 * 128:(g4 + 1) * 128], gm.ap[:, g, :], ident_f.ap, [gm.key, ident_f.key], [pb.key])
                cp("act", GT.ap[:, tti * 512:(tti + 1) * 512], pb.ap[0:E, :], [pb.key], [GT.key])
            dma("sp", gT_scr, GT.ap, [GT.key], ["gT_scr"], "gts")
            for tti in range(NTT):
                tsl = slice(tti * 512, (tti + 1) * 512)
                for dc in range(KC):
                    pb = PS(2 + dc % 2)
                    mm(pb.ap, b2s.ap[:, dc * 128:(dc + 1) * 128], GT.ap[:, tsl], True, True, [b2s.key, GT.key], [pb.key])
                    stt(xT[:, dc, tsl], pb.ap, der[:, 40 + dc:41 + dc], xT[:, dc, tsl], ALU.mult, ALU.add,
                        [pb.key, "der", ("xT", dc, tti)], [("xT", dc, tti)])
            phase_end()
            phase_begin()
            b1T = A.alloc("b1T", 128, [E, 16])
            dma("sp", b1T.ap, b1T_d[l].rearrange("p (e c) -> p e c", e=E), [], [b1T.key], "misc")
            ts("pool", b1T.ap[:, :, 8:16], b1T.ap[:, :, 8:16], 1.0, None, ALU.add, None, [b1T.key], [b1T.key])
            actT = A.alloc("actT", 128, [4, T], BF16)
            gb = [A.alloc("gb%d" % i, 128, [T]) for i in range(2)]
            gt_ = [A.alloc("g_%d" % i, 128, [512]) for i in range(2)]
            lt_ = [A.alloc("l_%d" % i, 128, [512]) for i in range(2)]
            st_ = [A.alloc("s_%d" % i, 128, [512], BF16) for i in range(2)]
            ctr = [0]
            for e in range(E):
                gbe = gb[e % 2]
                dma("sp", gbe.ap, gT_scr[e:e + 1, :].partition_broadcast(128), ["gT_scr"], [gbe.key], "gb")
                for half in range(2):
                    for cgi in range(2):
                        cg = half * 2 + cgi
                        sl = slot(slot_rr[0] % 3)
                        slot_rr[0] += 1
                        dma("pool", sl.ap.rearrange("p k c -> p (k c)"), w1_d[l, e, cg], [], [sl.key], "w")
                        for tti in range(NTT):
                            tsl = slice(tti * 512, (tti + 1) * 512)
                            for j in range(2):
                                i2 = ctr[0] % 2
                                ctr[0] += 1
                                ch = cg * 2 + j
                                chl = cgi * 2 + j
                                psg, psl = PS(i2 * 2), PS(i2 * 2 + 1)
                                for kc in range(KC):
                                    mm(psg.ap, sl.ap[:, kc, j * 128:(j + 1) * 128], hT[:, kc, tsl], kc == 0, kc == KC - 1,
                                       [sl.key, ("hT", kc, tti)], [psg.key])
                                for kc in range(KC):
                                    mm(psl.ap, sl.ap[:, kc, 256 + j * 128:256 + (j + 1) * 128], hT[:, kc, tsl], kc == 0, kc == KC - 1,
                                       [sl.key, ("hT", kc, tti)], [psl.key])
                                g_, s_, l_ = gt_[i2], st_[i2], lt_[i2]
                                ts("dve", g_.ap, psg.ap, b1T.ap[:, e, ch:ch + 1], 7.0, ALU.add, ALU.min, [psg.key, b1T.key], [g_.key])
                                act(s_.ap, g_.ap, AF.Sigmoid, [g_.key], [s_.key], scale=ALPHA)
                                ts("dve", l_.ap, psl.ap, b1T.ap[:, e, 8 + ch:9 + ch], -6.0, ALU.add, ALU.max, [psl.key, b1T.key], [l_.key])
                                tt("pool", g_.ap, g_.ap, s_.ap, ALU.mult, [g_.key, s_.key], [g_.key])
                                tt("pool", g_.ap, g_.ap, gbe.ap[:, tsl], ALU.mult, [g_.key, gbe.key], [g_.key])
                                stt(actT.ap[:, chl, tsl], l_.ap, 8.0, g_.ap, ALU.min, ALU.mult, [l_.key, g_.key], [(actT.key, chl, tti)])
                    sl = slot(slot_rr[0] % 3)
                    slot_rr[0] += 1
                    sl4 = sl.ap.rearrange("p k c -> p (k c)").rearrange("p (k c) -> p k c", k=4)
                    dma("pool", sl.ap.rearrange("p k c -> p (k c)"), w2_d[l, e, half], [], [sl.key], "w")
                    for tti in range(NTT):
                        tsl = slice(tti * 512, (tti + 1) * 512)
                        for dc in range(KC):
                            i2 = ctr[0] % 2
                            ctr[0] += 1
                            pb = PS(4 + i2)
                            for kc in range(4):
                                mm(pb.ap, sl4[:, kc, dc * 128:(dc + 1) * 128], actT.ap[:, kc, tsl], kc == 0, kc == 3,
                                   [sl.key, (actT.key, kc, tti)], [pb.key])
                            stt(xT[:, dc, tsl], pb.ap, der[:, 40 + dc:41 + dc], xT[:, dc, tsl], ALU.mult, ALU.add,
                                [pb.key, "der", ("xT", dc, tti)], [("xT", dc, tti)])
            phase_end()
            sub_end()

        for s in range(NSEQ):
            seq_begin()
            for kc in range(KC):
                dma("sp", xT[:, kc, :], xT_d[s, kc * 128:(kc + 1) * 128, :], [("xT", kc, t) for t in range(NTT)],
                    [("xT", kc, t) for t in range(NTT)], "xin")
            for l in range(DEPTH):
                load_layer_params(l, s)
                attention_sublayer(l, s)
                moe_sublayer(l, s)
            for kc in range(KC):
                dma("sp", outT_d[s, kc * 128:(kc + 1) * 128, :], xT[:, kc, :], [("xT", kc, t) for t in range(NTT)],
                    [("out", s, kc)], "xout")
            seq_end()
        P.emit()
    return nc


def pack_params(inp, DEPTH, E):
    pk128 = np.zeros((DEPTH, 128, P128_W), np.float32)
    pk64 = np.zeros((DEPTH, 64, P64_W), np.float32)

    def put128(l, name, arr):
        o, w = P128[name]
        pk128[l, :, o:o + w] = arr

    def put64(l, name, arr):
        o, w = P64[name]
        pk64[l, :, o:o + w] = arr

    def hl(v):
        return np.asarray(v).reshape(8, 64).T
    for l in range(DEPTH):
        put128(l, "ada_b", inp["ada_b"][l].reshape(48, 128).T)
        put128(l, "n1g", inp["norm1_g"][l].reshape(8, 128).T)
        put128(l, "n2g", inp["norm2_g"][l].reshape(8, 128).T)
        mu = inp["shift_mu"][l]
        put128(l, "mu_g", mu[1664:1792].reshape(128, 1))
        put128(l, "qg", np.tile(inp["q_norm_g"][l], 2).reshape(128, 1))
        put128(l, "kg", np.tile(inp["k_norm_g"][l], 2).reshape(128, 1))
        put128(l, "rb", np.broadcast_to(inp["router_b"][l][None, :], (128, E)) if E == 32 else
               np.pad(np.broadcast_to(inp["router_b"][l][None, :], (128, E)), ((0, 0), (0, 32 - E))))
        put64(l, "mu_r", hl(mu[0:512]))
        put64(l, "mu_k", hl(mu[512:1024]))
        put64(l, "mu_v", hl(mu[1024:1536]))
        put64(l, "mu_w", mu[1536:1600].reshape(64, 1))
        put64(l, "mu_a", mu[1600:1664].reshape(64, 1))
        put64(l, "w0", hl(inp["decay_w0"][l]))
        put64(l, "a0", hl(inp["iclr_a0"][l]))
        put64(l, "k_k", hl(inp["k_k"][l]))
        put64(l, "k_a", hl(inp["k_a"][l]))
        put64(l, "r_k", np.asarray(inp["r_k"][l]).T)
        put64(l, "lnw", hl(inp["lnx_w"][l]))
        put64(l, "lnb", hl(inp["lnx_b"][l]))
        if l > 0:
            put64(l, "vrb", hl(inp["vres_b"][l - 1]))
        put64(l, "sbg", hl(inp["sb_out_g"][l]))
    b1T = np.ascontiguousarray(
        np.asarray(inp["exp_b1"]).reshape(DEPTH, E, 16, 128).transpose(0, 3, 1, 2).reshape(DEPTH, 128, E * 16))
    return pk128, pk64, b1T


_CACHE = {}


def run_model(inp, T, NB, E, DEPTH, n_cores):
    NSEQ = NB // n_cores
    key = (T, NSEQ, E, DEPTH)
    if key not in _CACHE:
        _CACHE[key] = build_program(T, NSEQ, E, DEPTH)
    nc = _CACHE[key]
    f = lambda a: np.ascontiguousarray(np.asarray(a, dtype=np.float32))
    pk128, pk64, b1T = pack_params(inp, DEPTH, E)
    x = np.asarray(inp["x"], dtype=np.float32)
    c = np.asarray(inp["c"], dtype=np.float32)
    w1 = np.asarray(inp["exp_w1"], dtype=np.float32).reshape(DEPTH, E, 8, 128, 2, 4, 256)
    w1r = np.ascontiguousarray(w1.transpose(0, 1, 5, 3, 2, 4, 6)).reshape(DEPTH, E, 4, 128, 4096)
    w2 = np.asarray(inp["exp_w2"], dtype=np.float32).reshape(DEPTH, E, 2, 4, 128, 1024)
    w2r = np.ascontiguousarray(w2.transpose(0, 1, 2, 4, 3, 5)).reshape(DEPTH, E, 2, 128, 4096)
    shared = {
        "ada_w": f(inp["ada_w"]), "w_in": f(inp["w_in"]), "w_out": f(inp["w_out"]),
        "exp_w1r": w1r, "exp_w2r": w2r, "pk128": pk128, "pk64": pk64, "b1T": b1T,
        "exp_b2": f(inp["exp_b2"]), "router_w": f(inp["router_w"]), "decay_up": f(inp["decay_up"]),
        "iclr_up": f(inp["iclr_up"]), "gate_up": f(inp["gate_up"]),
        "vres_down": f(inp["vres_down"]), "vres_up": f(inp["vres_up"]),
    }
    in_maps = []
    for ci in range(n_cores):
        xs = x[ci * NSEQ:(ci + 1) * NSEQ]
        cs = c[ci * NSEQ:(ci + 1) * NSEQ]
        m = dict(shared)
        m["xT"] = np.ascontiguousarray(xs.transpose(0, 2, 1))
        m["cT"] = np.ascontiguousarray(cs.T.reshape(KC, 128, NSEQ).transpose(1, 0, 2))
        in_maps.append(m)
    res = run_bass_kernel_spmd(nc, in_maps, core_ids=list(range(n_cores)))
    outs = [np.asarray(r["outT"]).transpose(0, 2, 1) for r in res.results]
    return np.ascontiguousarray(np.concatenate(outs, axis=0).astype(np.float32))


def kernel(**inputs):
    return run_model(inputs, T=2048, NB=32, E=32, DEPTH=2, n_cores=8)
```

```python
import math
from contextlib import ExitStack
import numpy as np
import concourse.bass as bass
import concourse.mybir as mybir
from concourse.bass_utils import run_bass_kernel_spmd

F32 = mybir.dt.float32
BF16 = mybir.dt.bfloat16
AF = mybir.ActivationFunctionType
ALU = mybir.AluOpType
AX = mybir.AxisListType

ENG = ("pe", "act", "dve", "pool", "sp")
SEM_ROT = 30000


class Op:
    __slots__ = ("eng", "fn", "deps", "raw", "signal", "is_dma", "token", "pos")

    def __init__(self, eng, fn, is_dma):
        self.eng = eng
        self.fn = fn
        self.deps = []
        self.raw = ()
        self.signal = False
        self.is_dma = is_dma
        self.token = None
        self.pos = 0


class Prog:
    def __init__(self, nc, stack):
        self.nc = nc
        self.stack = stack
        self.ops = []
        self.by_eng = {e: [] for e in ENG}
        self.last_w = {}
        self.readers = {}
        self.dma_counts = {}
        self.pending_bar = {e: [] for e in ENG}
        self.last_dma_tokens = {}
        self.dma_rr = {e: 0 for e in ENG}
        self.dma_last_on_sem = {}
        self.flushed = {e: 0 for e in ENG}
        self.cnt = {e: 0 for e in ENG}
        self.waited = {e: {} for e in ENG}
        self.sem_cache = {}

    def op(self, eng, fn, reads=(), writes=(), dma_sem=None):
        o = Op(eng, fn, dma_sem is not None)
        deps = set()
        raw = set()
        for k in reads:
            w = self.last_w.get(k)
            if w is not None:
                deps.add(w)
                raw.add(w)
        for k in writes:
            w = self.last_w.get(k)
            if w is not None:
                deps.add(w)
            for r in self.readers.get(k, ()):
                deps.add(r)
        for b in self.pending_bar[eng]:
            deps.add(b)
            raw.add(b)
        self.pending_bar[eng] = []
        deps.discard(o)
        o.deps = list(deps)
        o.raw = raw
        if dma_sem is not None:
            K = 16
            dma_sem = "%s%d" % (eng, self.dma_rr[eng] % K)
            self.dma_rr[eng] += 1
            prev = self.dma_last_on_sem.get(dma_sem)
            if prev is not None:
                o.deps.append(prev)
            self.dma_last_on_sem[dma_sem] = o
            c = self.dma_counts.get(dma_sem, 0) + 16
            self.dma_counts[dma_sem] = c
            o.token = (dma_sem, c)
            self.last_dma_tokens[dma_sem] = o
        for k in reads:
            self.readers.setdefault(k, []).append(o)
        for k in writes:
            self.last_w[k] = o
            self.readers[k] = []
        o.pos = len(self.by_eng[eng])
        self.ops.append(o)
        self.by_eng[eng].append(o)
        return o

    def barrier(self):
        lasts = []
        for e in ENG:
            for o in reversed(self.by_eng[e][self.flushed[e]:]):
                if not o.is_dma:
                    o.signal = True
                    lasts.append(o)
                    break
        lasts += list(self.last_dma_tokens.values())
        for e in ENG:
            self.pending_bar[e] = list(set(self.pending_bar[e]) | set(lasts))
        self.last_w = {}
        self.readers = {}

    @staticmethod
    def _needs_wait(o, d):
        if d.is_dma:
            return True
        if d.eng != o.eng:
            return True
        if o.is_dma:
            return True
        if d.eng != "pe" and d in o.raw and (o.pos - d.pos) <= 2:
            return True
        return False

    def get_sem(self, name):
        if name not in self.sem_cache:
            self.sem_cache[name] = self.stack.enter_context(self.nc.semaphore(name))
        return self.sem_cache[name]

    def flush(self, final=False):
        nc = self.nc
        new = {e: self.by_eng[e][self.flushed[e]:] for e in ENG}
        for e in ENG:
            for o in new[e]:
                for d in o.deps:
                    if not d.is_dma and self._needs_wait(o, d):
                        assert not (d.fn is None and not d.signal), "dependency on an already-emitted unsignalled op"
                        d.signal = True
        for e in ENG:
            for o in new[e]:
                if o.is_dma:
                    if not isinstance(o.token[0], str) or not o.token[0].startswith("d_"):
                        o.token = ("d_%s" % str(o.token[0]), o.token[1])
                elif o.signal:
                    self.cnt[e] += 1
                    c = self.cnt[e]
                    o.token = ("e_%s_%d" % (e, (c - 1) // SEM_ROT), (c - 1) % SEM_ROT + 1)
                else:
                    o.token = None
        for e in ENG:
            for o in new[e]:
                if o.token is not None:
                    self.get_sem(o.token[0])
        engs = {"pe": nc.tensor, "act": nc.scalar, "dve": nc.vector, "pool": nc.gpsimd, "sp": nc.sync}
        with nc.Block() as block:
            def run(e):
                eng = engs[e]
                waited = self.waited[e]
                for o in new[e]:
                    need = {}
                    for d in o.deps:
                        if not self._needs_wait(o, d):
                            continue
                        sk, v = d.token
                        if waited.get(sk, 0) >= v:
                            continue
                        if need.get(sk, 0) < v:
                            need[sk] = v
                    for sk, v in need.items():
                        eng.wait_ge(self.get_sem(sk), v)
                        waited[sk] = v
                    ins = o.fn(eng)
                    if o.is_dma:
                        ins.then_inc(self.get_sem(o.token[0]), 16)
                    elif o.signal:
                        ins.then_inc(self.get_sem(o.token[0]), 1)
                    o.fn = None

            @block.tensor
            def _(t):
                run("pe")

            @block.scalar
            def _(s):
                run("act")

            @block.vector
            def _(v):
                run("dve")

            @block.gpsimd
            def _(g):
                run("pool")

            @block.sync
            def _(s):
                run("sp")
                if final:
                    for sk, o in self.last_dma_tokens.items():
                        s.wait_ge(self.get_sem(o.token[0]), o.token[1])
        for e in ENG:
            self.flushed[e] = len(self.by_eng[e])

    def emit(self):
        self.barrier()
        self.flush(final=True)


class Buf:
    __slots__ = ("ap", "key")

    def __init__(self, ap, key):
        self.ap = ap
        self.key = key


class Arena:
    def __init__(self, ap_f32, name):
        self.ap = ap_f32
        self.W = ap_f32.shape[1]
        self.off = 0
        self.name = name
        self.gen = 0

    def reset(self):
        self.off = 0
        self.gen += 1

    def alloc(self, tag, parts, free, dtype=F32):
        n = 1
        for f in free:
            n *= f
        words = n if dtype == F32 else (n + 1) // 2
        assert self.off + words <= self.W, (self.name, tag, self.off, words, self.W)
        v = self.ap[0:parts, self.off:self.off + words]
        self.off += words
        if dtype != F32:
            v = v.bitcast(dtype)[:, 0:n]
        if len(free) == 2:
            v = v.rearrange("p (a b) -> p a b", a=free[0])
        elif len(free) == 3:
            v = v.rearrange("p (a b c) -> p a b c", a=free[0], b=free[1])
        return Buf(v, (self.name, self.gen, tag))


D = 1024
KC = 8
HD = 64
NH = 8
D_RWKV = 512
C_RWKV = 3 * D_RWKV + 64 + 64 + 128
C_IN = C_RWKV + 3 * 512
NORM_EPS = 1e-6
GN_EPS = 1e-5 * HD
ALPHA = 1.702
C0 = math.exp(-0.5)

P128 = {}
_o = 0
for _n, _w in (("ada_b", 48), ("n1g", 8), ("n2g", 8), ("mu_g", 1), ("qg", 1), ("kg", 1), ("rb", 32)):
    P128[_n] = (_o, _w)
    _o += _w
P128_W = _o
P64 = {}
_o = 0
for _n, _w in (("mu_r", 8), ("mu_k", 8), ("mu_v", 8), ("mu_w", 1), ("mu_a", 1), ("w0", 8), ("a0", 8),
               ("k_k", 8), ("k_a", 8), ("r_k", 8), ("lnw", 8), ("lnb", 8), ("vrb", 8), ("sbg", 8)):
    P64[_n] = (_o, _w)
    _o += _w
P64_W = _o


def build_program(T, NSEQ, E, DEPTH=2, debug=False):
    assert T % 512 == 0
    NTT = T // 512
    NCH = T // 64
    NG = T // 128
    nc = bass.Bass("TRN2", target_bir_lowering=False)

    def din(name, shape):
        return nc.dram_tensor(name, list(shape), F32, kind="ExternalInput").ap()

    xT_d = din("xT", [NSEQ, D, T])
    cT_d = din("cT", [128, KC, NSEQ])
    ada_w_d = din("ada_w", [DEPTH, D, 6 * D])
    w_in_d = din("w_in", [DEPTH, D, C_IN])
    w_out_d = din("w_out", [DEPTH, D, D])
    w1_d = din("exp_w1r", [DEPTH, E, 4, 128, 4096])
    w2_d = din("exp_w2r", [DEPTH, E, 2, 128, 4096])
    pk128_d = din("pk128", [DEPTH, 128, P128_W])
    pk64_d = din("pk64", [DEPTH, 64, P64_W])
    b1T_d = din("b1T", [DEPTH, 128, E * 16])
    b2_d = din("exp_b2", [DEPTH, E, D])
    rw_d = din("router_w", [DEPTH, D, E])
    dup_d = din("decay_up", [DEPTH, 64, 512])
    iup_d = din("iclr_up", [DEPTH, 64, 512])
    gup_d = din("gate_up", [DEPTH, 128, 512])
    vdn_d = din("vres_down", [max(DEPTH - 1, 1), 512, 32])
    vup_d = din("vres_up", [max(DEPTH - 1, 1), 32, 512])
    outT_d = nc.dram_tensor("outT", [NSEQ, D, T], F32, kind="ExternalOutput").ap()
    gT_scr = nc.dram_tensor("gT_scr", [E, T], F32, kind="Internal").ap()
    vf_scr = nc.dram_tensor("vf_scr", [T // 64, 64, NH * 64], F32, kind="Internal").ap()
    dbg_d = {}

    st = ExitStack()
    with st:
        def sb(name, shape, dt=F32):
            return st.enter_context(nc.sbuf_tensor(name, list(shape), dt))

        P = Prog(nc, st)

        xT = hT = wsl_t = wout_t = local_t = A = psum = None
        uid = [0]
        scopes = {"seq": None, "sub": None, "phase": None}

        def uname(n):
            uid[0] += 1
            return "%s_%d" % (n, uid[0])

        def seq_begin():
            nonlocal xT
            scopes["seq"] = ExitStack()
            xT = scopes["seq"].enter_context(nc.sbuf_tensor(uname("xT"), [128, KC, T], F32))

        def seq_end():
            P.barrier()
            scopes["seq"].close()

        def sub_begin():
            nonlocal hT, wsl_t, wout_t
            ss = scopes["sub"] = ExitStack()
            hT = ss.enter_context(nc.sbuf_tensor(uname("hT"), [128, KC, T], BF16))
            wsl_t = ss.enter_context(nc.sbuf_tensor(uname("wsl"), [128, 3 * 512 * KC], BF16))
            wout_t = ss.enter_context(nc.sbuf_tensor(uname("wout"), [64, 8, D], BF16))

        def sub_end():
            scopes["sub"].close()

        def phase_begin():
            nonlocal local_t, A, psum
            ps_ = scopes["phase"] = ExitStack()
            local_t = ps_.enter_context(nc.sbuf_tensor(uname("loc"), [128, 49 * 256], F32))
            A = Arena(local_t[:], "loc")
            psum = [ps_.enter_context(nc.psum_tensor(uname("ps"), [128, 512], F32)) for i in range(8)]

        def phase_end():
            P.barrier()
            scopes["phase"].close()
        cst_t = sb("consts", [128, 1100])
        cstb_t = sb("constsb", [128, 512], BF16)
        pk128 = sb("pk128_sb", [128, P128_W])
        pk64 = sb("pk64_sb", [64, P64_W])
        mods = sb("mods", [128, DEPTH * 48 * NSEQ])
        der = sb("derived", [128, 64])
        lora_t = sb("lora", [128, 512 * 3 + 8 * 32 + 512])
        state_t = sb("state", [64, 4 * 64])
        carry_t = sb("carry", [128, 32])

        def PS(i):
            return Buf(psum[i][:], ("ps", i))

        def mm(out, lhsT, rhs, start, stop, reads, writes):
            P.op("pe", lambda e: e.matmul(out, lhsT=lhsT, rhs=rhs, start=start, stop=stop), reads, writes)

        def tr(out, in_, ident, reads, writes):
            P.op("pe", lambda e: e.transpose(out, in_, ident), reads, writes)

        def act(out, in_, func, reads, writes, bias=None, scale=None, eng="act"):
            kw = {}
            if bias is not None:
                kw["bias"] = bias
            if scale is not None:
                kw["scale"] = scale
            P.op(eng, lambda e: e.activation(out=out, in_=in_, func=func, **kw), reads, writes)

        def tt(eng, out, in0, in1, op, reads, writes):
            P.op(eng, lambda e: e.tensor_tensor(out=out, in0=in0, in1=in1, op=op), reads, writes)

        def ts(eng, out, in0, s1, s2, op0, op1, reads, writes):
            if s2 is None:
                P.op(eng, lambda e: e.tensor_scalar(out=out, in0=in0, scalar1=s1, scalar2=None, op0=op0), reads, writes)
            else:
                P.op(eng, lambda e: e.tensor_scalar(out=out, in0=in0, scalar1=s1, scalar2=s2, op0=op0, op1=op1), reads, writes)

        def stt(out, in0, scalar, in1, op0, op1, reads, writes):
            P.op("dve", lambda e: e.scalar_tensor_tensor(out=out, in0=in0, scalar=scalar, in1=in1, op0=op0, op1=op1), reads, writes)

        def cp(eng, out, in_, reads, writes):
            if eng == "act":
                P.op("act", lambda e: e.activation(out=out, in_=in_, func=AF.Copy), reads, writes)
            else:
                P.op(eng, lambda e: e.tensor_copy(out=out, in_=in_), reads, writes)

        def memset(eng, ap, val, writes):
            P.op(eng, lambda e: e.memset(ap, val), (), writes)

        def dma(eng, out, in_, reads, writes, sem):
            P.op(eng, lambda e: e.dma_start(out=out, in_=in_), reads, writes, dma_sem=sem)

        def rsqrt(out, in_, scale, bias, reads, writes, tmp):
            act(tmp.ap, in_, AF.Ln, list(reads) + ["c_small"], [tmp.key], bias=bias[0:in_.shape[0], :], scale=scale)
            act(out, tmp.ap, AF.Exp, [tmp.key], writes, scale=-0.5)

        ones_f = Buf(cst_t[:, 0:128], "c_ones")
        ident_f = Buf(cst_t[:, 128:256], "c_ident")
        blk_f = Buf(cst_t[:, 256:384], "c_blk")
        mU = Buf(cst_t[0:64, 384:448], "c_mU")
        mL = Buf(cst_t[0:64, 448:512], "c_mL")
        mUi = Buf(cst_t[0:64, 512:576], "c_mUi")
        scanm = Buf(cst_t[0:64, 576:1088], "c_scan")
        ntri_b = Buf(cstb_t[:, 0:128], "c_ntri")
        nones_b = Buf(cstb_t[:, 128:256], "c_nones")
        zeros_b = Buf(cstb_t[:, 256:320], "c_zeros")
        memset("pool", ones_f.ap, 1.0, [ones_f.key])
        c_eps_n = cst_t[:, 1088:1089]
        c_eps_g = cst_t[:, 1089:1090]
        c_one = cst_t[:, 1090:1091]
        c_zero = cst_t[:, 1091:1092]
        memset("pool", c_eps_n, NORM_EPS, ["c_small"])
        memset("pool", c_eps_g, GN_EPS, ["c_small"])
        memset("pool", c_one, 1.0, ["c_small"])
        memset("pool", c_zero, 0.0, ["c_small"])
        memset("pool", cst_t[:, 256:384], 0.0, [blk_f.key])
        memset("pool", cst_t[0:64, 256:320], 1.0, [blk_f.key])
        memset("pool", cst_t[64:128, 320:384], 1.0, [blk_f.key])

        def asel(out, in_, pattern, cmp_op, base, cm, reads, writes, fill=0.0):
            P.op("pool", lambda e: e.affine_select(out=out, in_=in_, pattern=pattern, compare_op=cmp_op,
                                                   fill=fill, base=base, channel_multiplier=cm), reads, writes)
        asel(ident_f.ap, ones_f.ap, [[-1, 128]], ALU.is_equal, 0, 1, [ones_f.key], [ident_f.key])
        asel(mU.ap, ones_f.ap[0:64, 0:64], [[1, 64]], ALU.is_gt, 0, -1, [ones_f.key], [mU.key])
        asel(mL.ap, ones_f.ap[0:64, 0:64], [[-1, 64]], ALU.is_gt, 0, 1, [ones_f.key], [mL.key])
        asel(mUi.ap, ones_f.ap[0:64, 0:64], [[1, 64]], ALU.is_ge, 0, -1, [ones_f.key], [mUi.key])
        memset("pool", scanm.ap, 1.0, [scanm.key])
        memset("pool", scanm.ap.rearrange("p (h t) -> p h t", h=NH)[:, :, 0:1], 0.0, [scanm.key])
        memset("pool", nones_b.ap, -1.0, [nones_b.key])
        memset("pool", zeros_b.ap, 0.0, [zeros_b.key])
        asel(ntri_b.ap, nones_b.ap, [[-1, 128]], ALU.is_ge, 0, 1, [nones_b.key], [ntri_b.key])

        def slot(i):
            return Buf(wsl_t[:, i * 4096:(i + 1) * 4096].rearrange("p (k c) -> p k c", k=KC), ("wslot", i))
        slot_rr = [0]

        def load_cols(slot_b, segs, w_ap):
            off = 0
            for (c0, n) in segs:
                src = w_ap[:, c0:c0 + n].rearrange("(k p) c -> p k c", p=128)
                dma("pool", slot_b.ap[:, :, off:off + n], src, [], [slot_b.key], ("w", slot_b.key[1]))
                off += n

        sub_begin()
        phase_begin()
        ct = A.alloc("ct", 128, [KC, NSEQ])
        ctb = A.alloc("ctb", 128, [KC, NSEQ], BF16)
        dma("sp", ct.ap, cT_d, [], [ct.key], "misc")
        act(ctb.ap, ct.ap, AF.Silu, [ct.key], [ctb.key])
        mods_v = mods[:].rearrange("p (l j s) -> p l j s", l=DEPTH, j=48)
        mods_b = Buf(mods_v, "mods")
        for l in range(DEPTH):
            pkb = A.alloc("pkb%d" % l, 128, [48])
            dma("sp", pkb.ap, pk128_d[l, :, 0:48], [], [pkb.key], "misc")
            for gi in range(12):
                sl = slot(slot_rr[0] % 3)
                slot_rr[0] += 1
                load_cols(sl, [(gi * 512, 512)], ada_w_d[l])
                pb = PS(gi % 2)
                for j in range(4):
                    for kc in range(KC):
                        mm(pb.ap[:, j * NSEQ:(j + 1) * NSEQ], sl.ap[:, kc, j * 128:(j + 1) * 128], ctb.ap[:, kc, :],
                           kc == 0, kc == KC - 1, [sl.key, ctb.key], [pb.key])
                for j in range(4):
                    jj = gi * 4 + j
                    act(mods_v[:, l, jj, :], pb.ap[:, j * NSEQ:(j + 1) * NSEQ], AF.Identity, [pb.key, pkb.key],
                        [mods_b.key], bias=pkb.ap[:, jj:jj + 1])
        phase_end()
        sub_end()

        def pk(name, c=None):
            o, w = P128[name]
            return pk128[:, o:o + w] if c is None else pk128[:, o + c:o + c + 1]

        def p64(name, h=None):
            o, w = P64[name]
            return pk64[:, o:o + w] if h is None else pk64[:, o + h:o + h + 1]

        def p64b(name, n):
            o, w = P64[name]
            return pk64[:, o:o + w].unsqueeze(2).to_broadcast([64, w, n])

        der_b = Buf(der[:], "der")
        omk_t = Buf(carry_t[0:64, 24:32], "omk")

        def load_layer_params(l, s):
            dma("sp", pk128[:], pk128_d[l], [], ["pk128"], "misc")
            dma("sp", pk64[:], pk64_d[l], [], ["pk64"], "misc")
            mv = mods_v[:, l, :, s]
            o1, _ = P128["n1g"]
            o2, _ = P128["n2g"]
            stt(der[:, 0:8], mv[:, 8:16], 1.0, pk128[:, o1:o1 + 8], ALU.add, ALU.mult, ["mods", "pk128"], ["der"])
            cp("dve", der[:, 8:16], mv[:, 0:8], ["mods"], ["der"])
            cp("dve", der[:, 16:24], mv[:, 16:24], ["mods"], ["der"])
            stt(der[:, 24:32], mv[:, 32:40], 1.0, pk128[:, o2:o2 + 8], ALU.add, ALU.mult, ["mods", "pk128"], ["der"])
            cp("dve", der[:, 32:40], mv[:, 24:32], ["mods"], ["der"])
            cp("dve", der[:, 40:48], mv[:, 40:48], ["mods"], ["der"])
            ts("dve", der[:, 48:49], pk("qg"), 0.125, None, ALU.mult, None, ["pk128"], ["der"])
            ts("dve", omk_t.ap, p64("k_a"), -1.0, 1.0, ALU.mult, ALU.add, ["pk64"], [omk_t.key])
            dma("sp", lora_t[0:64, 0:512], dup_d[l], [], ["lora"], "misc")
            dma("sp", lora_t[0:64, 512:1024], iup_d[l], [], ["lora"], "misc")
            dma("sp", lora_t[:, 1024:1536], gup_d[l], [], ["lora"], "misc")
            if l > 0:
                dma("sp", lora_t[0:64, 1536:1792].rearrange("p (h c) -> p h c", h=NH),
                    vdn_d[l - 1].rearrange("(h p) c -> p h c", p=64), [], ["lora"], "misc")
                dma("sp", lora_t[0:32, 1792:2304], vup_d[l - 1], [], ["lora"], "misc")

        def rms_modulate(gcol, shcol, tmpA, emit_tile, tmpH=None):
            for tti in range(NTT):
                tsl = slice(tti * 512, (tti + 1) * 512)
                pss = PS(7)
                for kc in range(KC):
                    sq = tmpA[kc % 2]
                    act(sq.ap, xT[:, kc, tsl], AF.Square, [("xT", kc, tti)], [sq.key])
                    mm(pss.ap, ones_f.ap, sq.ap, kc == 0, kc == KC - 1, [ones_f.key, sq.key], [pss.key])
                rstd = tmpA[2]
                rsqrt(rstd.ap, pss.ap, 1.0 / D, c_eps_n, [pss.key], [rstd.key], tmpA[3])
                tiles = []
                for kc in range(KC):
                    t1 = tmpH[kc] if tmpH is not None else tmpA[4 + kc % 2]
                    tt("dve", t1.ap, xT[:, kc, tsl], rstd.ap, ALU.mult, [("xT", kc, tti), rstd.key], [t1.key])
                    ts("pool", t1.ap, t1.ap, der[:, gcol + kc:gcol + kc + 1], der[:, shcol + kc:shcol + kc + 1],
                       ALU.mult, ALU.add, [t1.key, "der"], [t1.key])
                    cp("act", hT[:, kc, tsl], t1.ap, [t1.key], [("hT", kc, tti)])
                    tiles.append(t1)
                if emit_tile is not None:
                    emit_tile(tti, tiles)

        def attention_sublayer(l, s):
            sub_begin()
            phase_begin()
            tmpA = [A.alloc("nt%d" % i, 128, [512]) for i in range(6)]
            rms_modulate(0, 8, tmpA, None)
            phase_end()

            wl = w_in_d[l]
            woutb = Buf(wout_t[:], "wout")
            dma("pool", wout_t[:], w_out_d[l, 0:512, :].rearrange("(h p) c -> p h c", p=64), [], [woutb.key], "wout")
            HP = 4
            for hg in range(2):
                phase_begin()
                wr = Buf(wsl_t[:, 0:KC * 1280].rearrange("p (k c) -> p k c", k=KC), ("wslot", "rwkv", hg))
                segs = [(hg * 256, 256, 0), (512 + hg * 256, 256, 256), (1024, 512, 512), (1536, 256, 1024)]
                for (c0_, n_, o_) in segs:
                    dma("pool", wr.ap[:, :, o_:o_ + n_], wl[:, c0_:c0_ + n_].rearrange("(k p) c -> p k c", p=128),
                        [], [wr.key, ("wslot", 0), ("wslot", 1), ("wslot", 2)], "wrwkv")

                def HB(tag, dt=F32, nh=HP):
                    return A.alloc(tag, 64, [nh, 64], dt)
                zb = {"r": A.alloc("zb_r", 64, [HP, 65]), "k": A.alloc("zb_k", 64, [HP, 65]), "v": A.alloc("zb_v", 64, [NH, 65])}
                zbl = A.alloc("zb_l", 64, [2, 65])
                zbg = A.alloc("zb_g", 128, [65])
                for b_ in list(zb.values()) + [zbl, zbg]:
                    memset("pool", b_.ap, 0.0, [b_.key])
                R, K = HB("R"), HB("K")
                V8 = HB("V8", nh=NH)
                V = Buf(V8.ap[:, hg * HP:(hg + 1) * HP, :], V8.key)
                T8 = HB("T8", nh=NH)
                B1, B2, B3, B4, B5, B6 = (HB("B%d" % i) for i in range(1, 7))
                lw = A.alloc("lw", 64, [2, 64])
                lg = A.alloc("lg", 128, [64])
                pcs = A.alloc("pcs", 64, [HP, 1])
                vd = A.alloc("vd", 32, [64])
                Nn, NTt, Pa, PTa = HB("N"), HB("NT"), HB("Pa"), HB("PTa")
                S_ = HB("S")
                AakT, ArkT, ArbT = HB("AakT"), HB("ArkT"), HB("ArbT")
                Vt, Ktt, Btt = HB("Vt"), HB("Ktt"), HB("Btt")
                RHS, U_ = B1, B4
                yr = A.alloc("yr", 64, [HP, 64], BF16)
                xtmp = A.alloc("xtmp", 128, [KC, 64])
                M = Buf(state_t[:].rearrange("p (h v) -> p h v", h=HP), "state")
                memset("pool", M.ap, 0.0, [M.key])
                dup = lora_t[0:64, 0:512]
                iup = lora_t[0:64, 512:1024]
                gup = lora_t[:, 1024:1536]
                vdn = lora_t[0:64, 1536:1792].rearrange("p (h c) -> p h c", h=NH)
                vup = lora_t[0:32, 1792:2304]
                idf = ident_f.ap[0:64, 0:64]
                o64 = ones_f.ap[0:64, 0:64]
                mUb = mU.ap.unsqueeze(1).to_broadcast([64, HP, 64])
                mLb = mL.ap.unsqueeze(1).to_broadcast([64, HP, 64])
                mUib = mUi.ap.unsqueeze(1).to_broadcast([64, HP, 64])
                idb = idf.unsqueeze(1).to_broadcast([64, HP, 64])
                gate1b = der[:, 16:24].unsqueeze(2).to_broadcast([128, KC, 64])
                scm = scanm.ap[:, 0:HP * 64]
                W4 = HP * 64

                def f2(b_):
                    return b_.ap.rearrange("p h t -> p (h t)")

                def pv(pb_, nh=HP):
                    return pb_.ap[0:64, 0:nh * 64].rearrange("p (h t) -> p h t", h=nh)

                def pg(name, n=64):
                    o, w = P64[name]
                    return pk64[:, o + hg * HP:o + (hg + 1) * HP].unsqueeze(2).to_broadcast([64, HP, n])

                for c in range(NCH):
                    t0 = c * 64
                    tsl = slice(t0, t0 + 64)
                    hk = [("hT", kc, t0 // 512) for kc in range(KC)]
                    pb = PS(0)
                    for gi in range(2):
                        for h in range(HP):
                            col = gi * 256 + h * 64
                            oc = gi * 256 + h * 64
                            for kc in range(KC):
                                mm(pb.ap[0:64, oc:oc + 64], wr.ap[:, kc, col:col + 64], hT[:, kc, tsl],
                                   kc == 0, kc == KC - 1, [wr.key, hk[kc]], [pb.key])
                    cp("act", zb["r"].ap[:, :, 1:65], pb.ap[0:64, 0:256].rearrange("p (h t) -> p h t", h=HP), [pb.key], [zb["r"].key])
                    cp("act", zb["k"].ap[:, :, 1:65], pb.ap[0:64, 256:512].rearrange("p (h t) -> p h t", h=HP), [pb.key], [zb["k"].key])
                    pb = PS(1)
                    for h in range(NH):
                        for kc in range(KC):
                            mm(pb.ap[0:64, h * 64:(h + 1) * 64], wr.ap[:, kc, 512 + h * 64:512 + (h + 1) * 64], hT[:, kc, tsl],
                               kc == 0, kc == KC - 1, [wr.key, hk[kc]], [pb.key])
                    cp("act", zb["v"].ap[:, :, 1:65], pv(pb, NH), [pb.key], [zb["v"].key])
                    pb = PS(3)
                    for j in range(2):
                        for kc in range(KC):
                            mm(pb.ap[0:64, j * 64:(j + 1) * 64], wr.ap[:, kc, 1024 + j * 64:1024 + (j + 1) * 64], hT[:, kc, tsl],
                               kc == 0, kc == KC - 1, [wr.key, hk[kc]], [pb.key])
                    for kc in range(KC):
                        mm(pb.ap[:, 128:192], wr.ap[:, kc, 1152:1280], hT[:, kc, tsl], kc == 0, kc == KC - 1, [wr.key, hk[kc]], [pb.key])
                    cp("act", zbl.ap[:, :, 1:65], pb.ap[0:64, 0:128].rearrange("p (h t) -> p h t", h=2), [pb.key], [zbl.key])
                    cp("act", zbg.ap[:, 1:65], pb.ap[:, 128:192], [pb.key], [zbg.key])
                    for n, dst, tmpb, mub in (("r", R, B1, pg("mu_r")), ("k", K, B1, pg("mu_k")), ("v", V8, T8, p64b("mu_v", 64))):
                        z = zb[n]
                        tt("dve", tmpb.ap, z.ap[:, :, 0:64], z.ap[:, :, 1:65], ALU.subtract, [z.key], [tmpb.key])
                        tt("pool", tmpb.ap, tmpb.ap, mub, ALU.mult, [tmpb.key, "pk64"], [tmpb.key])
                        tt("dve", dst.ap, tmpb.ap, z.ap[:, :, 1:65], ALU.add, [tmpb.key, z.key], [dst.key])
                        cp("act", z.ap[:, :, 0:1], z.ap[:, :, 64:65], [z.key], [z.key])
                    tt("dve", lw.ap, zbl.ap[:, :, 0:64], zbl.ap[:, :, 1:65], ALU.subtract, [zbl.key], [lw.key])
                    o_w, _ = P64["mu_w"]
                    tt("dve", lw.ap, lw.ap, pk64[:, o_w:o_w + 2].unsqueeze(2).to_broadcast([64, 2, 64]), ALU.mult, [lw.key, "pk64"], [lw.key])
                    tt("dve", lw.ap, lw.ap, zbl.ap[:, :, 1:65], ALU.add, [lw.key, zbl.key], [lw.key])
                    cp("act", zbl.ap[:, :, 0:1], zbl.ap[:, :, 64:65], [zbl.key], [zbl.key])
                    tt("dve", lg.ap, zbg.ap[:, 0:64], zbg.ap[:, 1:65], ALU.subtract, [zbg.key], [lg.key])
                    stt(lg.ap, lg.ap, pk("mu_g"), zbg.ap[:, 1:65], ALU.mult, ALU.add, [lg.key, zbg.key, "pk128"], [lg.key])
                    cp("act", zbg.ap[:, 0:1], zbg.ap[:, 64:65], [zbg.key], [zbg.key])
                    act(lw.ap[:, 0, :], lw.ap[:, 0, :], AF.Tanh, [lw.key], [lw.key])
                    act(lg.ap, lg.ap, AF.Tanh, [lg.key], [lg.key], scale=0.5)
                    ts("pool", lg.ap, lg.ap, 0.5, 0.5, ALU.mult, ALU.add, [lg.key], [lg.key])
                    pb = PS(4)
                    for h in range(HP):
                        hgl = hg * HP + h
                        mm(pb.ap[0:64, h * 64:(h + 1) * 64], dup[:, hgl * 64:(hgl + 1) * 64], lw.ap[:, 0, :], True, True, ["lora", lw.key], [pb.key])
                    tt("dve", B1.ap, pv(pb), pg("w0"), ALU.add, [pb.key, "pk64"], [B1.key])
                    act(B1.ap, B1.ap, AF.Tanh, [B1.key], [B1.key], scale=0.5)
                    ts("pool", B1.ap, B1.ap, 0.5, 0.5, ALU.mult, ALU.add, [B1.key], [B1.key])
                    P.op("dve", lambda e, B1=B1, B2=B2: e.tensor_tensor_scan(out=f2(B2), data0=scm, data1=f2(B1), initial=0.0,
                                                                             op0=ALU.mult, op1=ALU.add), [B1.key, scanm.key], [B2.key])
                    act(B3.ap, B2.ap, AF.Exp, [B2.key], [B3.key], scale=-C0)
                    act(B4.ap, B2.ap, AF.Exp, [B2.key], [B4.key], scale=C0)
                    tt("pool", B1.ap, B2.ap, B1.ap, ALU.subtract, [B2.key, B1.key], [B1.key])
                    act(B1.ap, B1.ap, AF.Exp, [B1.key], [B1.key], scale=-C0)
                    cp("act", pcs.ap, B3.ap[:, :, 63:64], [B3.key], [pcs.key])
                    pb = PS(5)
                    for h in range(HP):
                        hgl = hg * HP + h
                        mm(pb.ap[0:64, h * 64:(h + 1) * 64], iup[:, hgl * 64:(hgl + 1) * 64], lw.ap[:, 1, :], True, True, ["lora", lw.key], [pb.key])
                    tt("dve", B2.ap, pv(pb), pg("a0"), ALU.add, [pb.key, "pk64"], [B2.key])
                    act(B2.ap, B2.ap, AF.Tanh, [B2.key], [B2.key], scale=0.5)
                    ts("pool", B2.ap, B2.ap, 0.5, 0.5, ALU.mult, ALU.add, [B2.key], [B2.key])
                    if l == 0:
                        if hg == 0:
                            dma("sp", vf_scr[c], f2(V8), [V8.key], [("vf", c)], "vf")
                    else:
                        dma("sp", f2(B5), vf_scr[c, :, hg * HP * 64:(hg + 1) * HP * 64], [("vf", c)], [B5.key], "vf")
                        pb = PS(6)
                        for h in range(NH):
                            mm(pb.ap[0:32, 0:64], vdn[:, h, :], V8.ap[:, h, :], h == 0, h == NH - 1, ["lora", V8.key], [pb.key])
                        cp("act", vd.ap, pb.ap[0:32, 0:64], [pb.key], [vd.key])
                        pb = PS(7)
                        for h in range(HP):
                            hgl = hg * HP + h
                            mm(pb.ap[0:64, h * 64:(h + 1) * 64], vup[:, hgl * 64:(hgl + 1) * 64], vd.ap, True, True, ["lora", vd.key], [pb.key])
                        tt("dve", B6.ap, pv(pb), pg("vrb"), ALU.add, [pb.key, "pk64"], [B6.key])
                        act(B6.ap, B6.ap, AF.Tanh, [B6.key], [B6.key], scale=0.5)
                        ts("pool", B6.ap, B6.ap, 0.5, 0.5, ALU.mult, ALU.add, [B6.key], [B6.key])
                        tt("dve", B5.ap, B5.ap, V.ap, ALU.subtract, [B5.key, V.key], [B5.key])
                        tt("pool", B5.ap, B5.ap, B6.ap, ALU.mult, [B5.key, B6.key], [B5.key])
                        tt("dve", V.ap, V.ap, B5.ap, ALU.add, [V.key, B5.key], [V.key])
                    tt("pool", B5.ap, K.ap, pg("k_k"), ALU.mult, [K.key, "pk64"], [B5.key])
                    act(B6.ap, B5.ap, AF.Square, [B5.key], [B6.key])
                    pb = PS(6)
                    mm(pb.ap[0:64, 0:W4], o64, f2(B6), True, True, [ones_f.key, B6.key], [pb.key])
                    ts("dve", B6.ap, pv(pb), 1e-24, None, ALU.max, None, [pb.key], [B6.key])
                    rsqrt(B6.ap, B6.ap, 1.0, c_zero, [B6.key], [B6.key], Nn)
                    tt("dve", B5.ap, B5.ap, B6.ap, ALU.mult, [B5.key, B6.key], [B5.key])
                    stt(B6.ap, B5.ap, -1.0, B2.ap, ALU.mult, ALU.mult, [B5.key, B2.key], [B6.key])
                    tt("pool", B6.ap, B6.ap, B4.ap, ALU.mult, [B6.key, B4.key], [B6.key])
                    tt("dve", B5.ap, B5.ap, B1.ap, ALU.mult, [B5.key, B1.key], [B5.key])
                    tt("pool", B1.ap, B2.ap, pg("k_a"), ALU.mult, [B2.key, "pk64"], [B1.key])
                    tt("pool", B1.ap, B1.ap, omk_t.ap[:, hg * HP:(hg + 1) * HP].unsqueeze(2).to_broadcast([64, HP, 64]), ALU.add,
                       [B1.key, omk_t.key], [B1.key])
                    tt("dve", B1.ap, B1.ap, K.ap, ALU.mult, [B1.key, K.key], [B1.key])
                    tt("pool", B2.ap, R.ap, B1.ap, ALU.mult, [R.key, B1.key], [B2.key])
                    tt("pool", B2.ap, B2.ap, pg("r_k"), ALU.mult, [B2.key, "pk64"], [B2.key])
                    pb = PS(7)
                    mm(pb.ap[0:64, 0:W4], o64, f2(B2), True, True, [ones_f.key, B2.key], [pb.key])
                    tt("dve", B2.ap, pv(pb), V.ap, ALU.mult, [pb.key, V.key], [B2.key])
                    tt("dve", K.ap, B1.ap, B4.ap, ALU.mult, [B1.key, B4.key], [K.key])
                    tt("pool", R.ap, R.ap, B3.ap, ALU.mult, [R.key, B3.key], [R.key])
                    pb = PS(4)
                    for h in range(HP):
                        hgl = hg * HP + h
                        mm(pb.ap[0:64, h * 64:(h + 1) * 64], gup[:, hgl * 64:(hgl + 1) * 64], lg.ap, True, True, ["lora", lg.key], [pb.key])
                    cp("act", B3.ap, pv(pb), [pb.key], [B3.key])
                    for src, dstb, bank in ((V, Vt, 0), (K, Ktt, 1), (B6, Btt, 2)):
                        pb = PS(bank)
                        for h in range(HP):
                            tr(pb.ap[0:64, h * 64:(h + 1) * 64], src.ap[:, h, :], idf, [src.key, ident_f.key], [pb.key])
                        cp("act", f2(dstb), pb.ap[0:64, 0:W4], [pb.key], [dstb.key])
                    pb, pb2 = PS(3), PS(5)
                    for h in range(HP):
                        mm(pb.ap[0:64, h * 64:(h + 1) * 64], B6.ap[:, h, :], B5.ap[:, h, :], True, True, [B6.key, B5.key], [pb.key])
                        mm(pb2.ap[0:64, h * 64:(h + 1) * 64], B5.ap[:, h, :], B6.ap[:, h, :], True, True, [B6.key, B5.key], [pb2.key])
                    tt("dve", Nn.ap, pv(pb), mUb, ALU.mult, [pb.key, mU.key], [Nn.key])
                    tt("dve", NTt.ap, pv(pb2), mLb, ALU.mult, [pb2.key, mL.key], [NTt.key])
                    tt("pool", S_.ap, Nn.ap, idb, ALU.add, [Nn.key, ident_f.key], [S_.key])
                    pb = PS(6)
                    for h in range(HP):
                        mm(pb.ap[0:64, h * 64:(h + 1) * 64], K.ap[:, h, :], B5.ap[:, h, :], True, True, [K.key, B5.key], [pb.key])
                    tt("dve", AakT.ap, pv(pb), mUb, ALU.mult, [pb.key, mU.key], [AakT.key])
                    pb = PS(7)
                    for h in range(HP):
                        mm(pb.ap[0:64, h * 64:(h + 1) * 64], K.ap[:, h, :], R.ap[:, h, :], True, True, [K.key, R.key], [pb.key])
                    tt("dve", ArkT.ap, pv(pb), mUib, ALU.mult, [pb.key, mUi.key], [ArkT.key])
                    pb = PS(4)
                    for h in range(HP):
                        mm(pb.ap[0:64, h * 64:(h + 1) * 64], B6.ap[:, h, :], R.ap[:, h, :], True, True, [B6.key, R.key], [pb.key])
                    tt("dve", ArbT.ap, pv(pb), mUib, ALU.mult, [pb.key, mUi.key], [ArbT.key])
                    pb = PS(6)
                    for h in range(HP):
                        mm(pb.ap[0:64, h * 64:(h + 1) * 64], B5.ap[:, h, :], M.ap[:, h, :], True, False, [B5.key, M.key], [pb.key])
                        mm(pb.ap[0:64, h * 64:(h + 1) * 64], AakT.ap[:, h, :], Vt.ap[:, h, :], False, True, [AakT.key, Vt.key], [pb.key])
                    cp("act", f2(RHS), pb.ap[0:64, 0:W4], [pb.key], [RHS.key])
                    Pc, PTc, Pn, PTn = Nn, NTt, Pa, PTa
                    for lev in range(1, 6):
                        last = lev == 5
                        pbT = PS(lev % 2)
                        for h in range(HP):
                            mm(pbT.ap[0:64, h * 64:(h + 1) * 64], Pc.ap[:, h, :], PTc.ap[:, h, :], True, True, [Pc.key, PTc.key], [pbT.key])
                        if not last:
                            pbN = PS(2 + lev % 2)
                            for h in range(HP):
                                mm(pbN.ap[0:64, h * 64:(h + 1) * 64], PTc.ap[:, h, :], Pc.ap[:, h, :], True, True, [Pc.key, PTc.key], [pbN.key])
                        cp("act", f2(PTn), pbT.ap[0:64, 0:W4], [pbT.key], [PTn.key])
                        if not last:
                            cp("dve", f2(Pn), pbN.ap[0:64, 0:W4], [pbN.key], [Pn.key])
                        pbS = PS(5)
                        for h in range(HP):
                            mm(pbS.ap[0:64, h * 64:(h + 1) * 64], PTn.ap[:, h, :], S_.ap[:, h, :], True, True, [PTn.key, S_.key], [pbS.key])
                        tt("dve", f2(S_), f2(S_), pbS.ap[0:64, 0:W4], ALU.add, [pbS.key, S_.key], [S_.key])
                        Pc, PTc, Pn, PTn = Pn, PTn, Pc, PTc
                    pb = PS(7)
                    for h in range(HP):
                        mm(pb.ap[0:64, h * 64:(h + 1) * 64], S_.ap[:, h, :], RHS.ap[:, h, :], True, True, [S_.key, RHS.key], [pb.key])
                    cp("act", f2(U_), pb.ap[0:64, 0:W4], [pb.key], [U_.key])
                    pby = PS(4)
                    for h in range(HP):
                        o_ = pby.ap[0:64, h * 64:(h + 1) * 64]
                        mm(o_, M.ap[:, h, :], R.ap[:, h, :], True, False, [M.key, R.key], [pby.key])
                        mm(o_, Vt.ap[:, h, :], ArkT.ap[:, h, :], False, False, [Vt.key, ArkT.key], [pby.key])
                        mm(o_, U_.ap[:, h, :], ArbT.ap[:, h, :], False, True, [U_.key, ArbT.key], [pby.key])
                    pb = PS(0)
                    for h in range(HP):
                        o_ = pb.ap[0:64, h * 64:(h + 1) * 64]
                        mm(o_, Ktt.ap[:, h, :], Vt.ap[:, h, :], True, False, [Ktt.key, Vt.key], [pb.key])
                        mm(o_, Btt.ap[:, h, :], U_.ap[:, h, :], False, True, [Btt.key, U_.key], [pb.key])
                    tt("dve", M.ap, M.ap, pv(pb), ALU.add, [M.key, pb.key], [M.key])
                    tt("pool", M.ap, M.ap, pcs.ap.to_broadcast([64, HP, 64]), ALU.mult, [M.key, pcs.key], [M.key])
                    cp("act", f2(B1), pby.ap[0:64, 0:W4], [pby.key], [B1.key])
                    pb = PS(1)
                    mm(pb.ap[0:64, 0:W4], o64, f2(B1), True, True, [ones_f.key, B1.key], [pb.key])
                    stt(f2(B1), pb.ap[0:64, 0:W4], -1.0 / 64, f2(B1), ALU.mult, ALU.add, [pb.key, B1.key], [B1.key])
                    act(B4.ap, B1.ap, AF.Square, [B1.key], [B4.key])
                    pb = PS(2)
                    mm(pb.ap[0:64, 0:W4], o64, f2(B4), True, True, [ones_f.key, B4.key], [pb.key])
                    rsqrt(f2(B4), pb.ap[0:64, 0:W4], 1.0 / 64, c_eps_g, [pb.key], [B4.key], Buf(f2(Nn), Nn.key))
                    tt("dve", B1.ap, B1.ap, B4.ap, ALU.mult, [B1.key, B4.key], [B1.key])
                    tt("pool", B1.ap, B1.ap, pg("lnw"), ALU.mult, [B1.key, "pk64"], [B1.key])
                    tt("pool", B1.ap, B1.ap, pg("lnb"), ALU.add, [B1.key, "pk64"], [B1.key])
                    tt("dve", B1.ap, B1.ap, B2.ap, ALU.add, [B1.key, B2.key], [B1.key])
                    tt("dve", yr.ap, B1.ap, B3.ap, ALU.mult, [B1.key, B3.key], [yr.key])
                    pb = PS(3)
                    for dc in range(KC):
                        for h in range(HP):
                            mm(pb.ap[:, dc * 64:(dc + 1) * 64], wout_t[:, hg * HP + h, dc * 128:(dc + 1) * 128], yr.ap[:, h, :],
                               h == 0, h == HP - 1, [woutb.key, yr.key], [pb.key])
                    tt("dve", xtmp.ap, pb.ap.rearrange("p (k t) -> p k t", k=KC), gate1b, ALU.mult, [pb.key, "der"], [xtmp.key])
                    xk = [("xT", kc, t0 // 512) for kc in range(KC)]
                    tt("pool", xT[:, :, tsl], xT[:, :, tsl], xtmp.ap, ALU.add, [xtmp.key] + xk, xk)
                phase_end()

            pass
            phase_begin()
            dma("pool", wout_t[:], w_out_d[l, 512:1024, :].rearrange("(h p) c -> p h c", p=64), [], [woutb.key], "wout")
            qT = A.alloc("qT", 128, [T], BF16)
            kT = A.alloc("kT", 128, [T], BF16)
            Vs = A.alloc("Vs", 128, [NG, 128], BF16)
            qraw = [A.alloc("qraw%d" % i, 128, [512]) for i in range(2)]
            qsq = [A.alloc("qsq%d" % i, 128, [512]) for i in range(2)]
            etmp = [A.alloc("etmp%d" % i, 128, [512]) for i in range(2)]
            spb = [A.alloc("spb%d" % i, 128, [512], BF16) for i in range(2)]
            wTb = [A.alloc("wTb%d" % i, 128, [512], BF16) for i in range(2)]
            spsum2 = [A.alloc("spsum%d" % i, 128, [512]) for i in range(2)]
            spsb = [A.alloc("spsb%d" % i, 128, [512], BF16) for i in range(2)]
            osb = A.alloc("osb", 64, [512])
            osq = A.alloc("osq", 64, [512])
            ors = A.alloc("ors", 64, [512])
            otm = A.alloc("otm", 64, [512])
            ysb = A.alloc("ysb", 64, [2, 512], BF16)
            it = [0]
            for hp in range(4):
                sl = slot(slot_rr[0] % 3)
                slot_rr[0] += 1
                load_cols(sl, [(C_RWKV + hp * 128, 128), (C_RWKV + 512 + hp * 128, 128), (C_RWKV + 1024 + hp * 128, 128)], wl)
                for which, dst, gcol in ((0, qT, der[:, 48:49]), (1, kT, pk("kg"))):
                    for tti in range(NTT):
                        tsl = slice(tti * 512, (tti + 1) * 512)
                        pb = PS(which * 2 + tti % 2)
                        for kc in range(KC):
                            mm(pb.ap, sl.ap[:, kc, which * 128:(which + 1) * 128], hT[:, kc, tsl], kc == 0, kc == KC - 1,
                               [sl.key, ("hT", kc, tti)], [pb.key])
                        qr, qs = qraw[tti % 2], qsq[tti % 2]
                        cp("act", qr.ap, pb.ap, [pb.key], [qr.key])
                        act(qs.ap, qr.ap, AF.Square, [qr.key], [qs.key])
                        pb2 = PS(4 + tti % 2)
                        mm(pb2.ap, blk_f.ap, qs.ap, True, True, [blk_f.key, qs.key], [pb2.key])
                        rsqrt(qs.ap, pb2.ap, 1.0 / 64, c_eps_n, [pb2.key], [qs.key], etmp[tti % 2])
                        tt("dve", qr.ap, qr.ap, qs.ap, ALU.mult, [qr.key, qs.key], [qr.key])
                        ts("dve", dst.ap[:, tsl], qr.ap, gcol, None, ALU.mult, None, [qr.key, "der", "pk128"], [(dst.key, tti)])
                for g in range(NG):
                    pb = PS(6 + g % 2)
                    for kc in range(KC):
                        mm(pb.ap[:, 0:128], hT[:, kc, g * 128:(g + 1) * 128], sl.ap[:, kc, 256:384], kc == 0, kc == KC - 1,
                           [sl.key, ("hT", kc, g // 4)], [pb.key])
                    cp("act", Vs.ap[:, g, :], pb.ap[:, 0:128], [pb.key], [(Vs.key, g)])
                qk_all = [(qT.key, i) for i in range(NTT)] + [(kT.key, i) for i in range(NTT)]
                for QT in range(NTT):
                    qsl = slice(QT * 512, (QT + 1) * 512)
                    nkb = 4 * QT + 4
                    for hh in range(2):
                        po = PS(4 + hh)
                        mm(po.ap[0:64, :], zeros_b.ap, kT.ap[:, qsl], True, False, [zeros_b.key] + qk_all, [po.key])
                        memset("pool", spsum2[hh].ap, 0.0, [spsum2[hh].key])
                    for sbk in range(nkb - 1, -1, -1):
                        for hh in range(2):
                            pbase = hh * 64
                            po = PS(4 + hh)
                            spsum = spsum2[hh]
                            c0 = max(0, 128 * sbk - 512 * QT)
                            diag = sbk >= 4 * QT
                            i2 = hh
                            ksl = slice(sbk * 128, (sbk + 1) * 128)
                            q_ap = qT.ap[pbase:pbase + 64, QT * 512 + c0:(QT + 1) * 512]
                            k_ap = kT.ap[pbase:pbase + 64, ksl]
                            ps1 = PS(i2)
                            mm(ps1.ap[:, c0:512], k_ap, q_ap, True, True, qk_all, [ps1.key])
                            et, sp, wT_, sps = etmp[i2], spb[i2], wTb[i2], spsb[i2]
                            act(et.ap[:, c0:512], ps1.ap[:, c0:512], AF.Exp, [ps1.key], [et.key])
                            act(sp.ap[:, c0:512], et.ap[:, c0:512], AF.Ln, [et.key, "c_small"], [sp.key], bias=c_one)
                            if diag:
                                asel(sp.ap[:, c0:c0 + 128], sp.ap[:, c0:c0 + 128], [[1, 128]], ALU.is_gt, 0, -1, [sp.key], [sp.key])
                            ps2 = PS(2 + i2)
                            first = sbk == nkb - 1
                            mm(ps2.ap[:, c0:512], k_ap, q_ap, True, False, qk_all, [ps2.key])
                            mm(ps2.ap[:, c0:512], ntri_b.ap, sp.ap[:, c0:512], False, first, [ntri_b.key, sp.key], [ps2.key])
                            if not first:
                                cp("act", sps.ap[:, c0:512], spsum.ap[:, c0:512], [spsum.key], [sps.key])
                                mm(ps2.ap[:, c0:512], nones_b.ap, sps.ap[:, c0:512], False, True, [nones_b.key, sps.key], [ps2.key])
                            act(wT_.ap[:, c0:512], ps2.ap[:, c0:512], AF.Exp, [ps2.key], [wT_.key])
                            if diag:
                                asel(wT_.ap[:, c0:c0 + 128], wT_.ap[:, c0:c0 + 128], [[1, 128]], ALU.is_gt, 0, -1, [wT_.key], [wT_.key])
                            mm(po.ap[0:64, c0:512], Vs.ap[:, sbk, pbase:pbase + 64], wT_.ap[:, c0:512], False, sbk == 0,
                               [(Vs.key, sbk), wT_.key], [po.key])
                            if sbk > 0:
                                tt("dve", spsum.ap[:, c0:512], spsum.ap[:, c0:512], sp.ap[:, c0:512], ALU.add, [spsum.key, sp.key], [spsum.key])
                    for hh in range(2):
                        po = PS(4 + hh)
                        cp("act", osb.ap, po.ap[0:64, :], [po.key], [osb.key])
                        act(osq.ap, osb.ap, AF.Square, [osb.key], [osq.key])
                        pb = PS(6)
                        mm(pb.ap[0:64, :], ones_f.ap[0:64, 0:64], osq.ap, True, True, [ones_f.key, osq.key], [pb.key])
                        rsqrt(ors.ap, pb.ap[0:64, :], 1.0 / 64, c_eps_n, [pb.key], [ors.key], otm)
                        tt("dve", osb.ap, osb.ap, ors.ap, ALU.mult, [osb.key, ors.key], [osb.key])
                        ts("dve", ysb.ap[:, hh, :], osb.ap, p64("sbg", hp * 2 + hh), None, ALU.mult, None, [osb.key, "pk64"], [(ysb.key, hh)])
                    for dc in range(KC):
                        pb = PS(6 + dc % 2)
                        for hh in range(2):
                            mm(pb.ap, wout_t[:, hp * 2 + hh, dc * 128:(dc + 1) * 128], ysb.ap[:, hh, :], hh == 0, hh == 1,
                               [woutb.key, (ysb.key, hh)], [pb.key])
                        stt(xT[:, dc, qsl], pb.ap, der[:, 16 + dc:17 + dc], xT[:, dc, qsl], ALU.mult, ALU.add,
                            [pb.key, "der", ("xT", dc, QT)], [("xT", dc, QT)])
            phase_end()
            sub_end()

        def moe_sublayer(l, s):
            sub_begin()
            phase_begin()
            tmpA = [A.alloc("nt%d" % i, 128, [512]) for i in range(4)]
            tmpH = [A.alloc("nh%d" % i, 128, [512]) for i in range(KC)]
            rw = A.alloc("rw", 128, [KC, E])
            dma("sp", rw.ap, rw_d[l].rearrange("(k p) e -> p k e", p=128), [], [rw.key], "misc")
            b2s = A.alloc("b2s", E, [D])
            dma("sp", b2s.ap, b2_d[l], [], [b2s.key], "misc")
            lgp = PS(6)
            lgv = lgp.ap[:, 0:NG * E].rearrange("p (g e) -> p g e", g=NG)

            def router_tile(tti, tiles):
                for g4 in range(4):
                    g = tti * 4 + g4
                    for kc in range(KC):
                        mm(lgv[:, g, :], tiles[kc].ap[:, g4 * 128:(g4 + 1) * 128], rw.ap[:, kc, :], kc == 0, kc == KC - 1,
                           [tiles[kc].key, rw.key], [lgp.key])
            rms_modulate(24, 32, tmpA, router_tile, tmpH)
            lgb = A.alloc("lgb", 128, [NG, E])
            gm = A.alloc("gm", 128, [NG, E])
            top8 = A.alloc("top8", 128, [NG, 8])
            gsum = A.alloc("gsum", 128, [NG, 1])
            o_rb, _ = P128["rb"]
            tt("dve", lgb.ap, lgv, pk128[:, o_rb:o_rb + E].unsqueeze(1).to_broadcast([128, NG, E]), ALU.add, [lgp.key, "pk128"], [lgb.key])
            for g in range(NG):
                P.op("dve", lambda e, g=g: e.max(out=top8.ap[:, g, :], in_=lgb.ap[:, g, :]), [lgb.key], [top8.key])
            tt("dve", gm.ap, lgb.ap, top8.ap[:, :, 3:4].to_broadcast([128, NG, E]), ALU.is_ge, [lgb.key, top8.key], [gm.key])
            tt("dve", lgb.ap, lgb.ap, top8.ap[:, :, 0:1].to_broadcast([128, NG, E]), ALU.subtract, [lgb.key, top8.key], [lgb.key])
            act(lgb.ap, lgb.ap, AF.Exp, [lgb.key], [lgb.key])
            tt("dve", gm.ap, gm.ap, lgb.ap, ALU.mult, [gm.key, lgb.key], [gm.key])
            P.op("dve", lambda e: e.tensor_reduce(out=gsum.ap[:, :, 0], in_=gm.ap, axis=AX.X, op=ALU.add), [gm.key], [gsum.key])
            P.op("dve", lambda e: e.reciprocal(out=gsum.ap, in_=gsum.ap), [gsum.key], [gsum.key])
            tt("dve", gm.ap, gm.ap, gsum.ap.to_broadcast([128, NG, E]), ALU.mult, [gm.key, gsum.key], [gm.key])
            GT = A.alloc("GT", E, [T])
            for tti in range(NTT):
                pb = PS(tti % 2)
                for g4 in range(4):
                    g = tti * 4 + g4
                    tr(pb.ap[0:E, g4 * 128:(g4 + 1) * 128], gm.ap[:, g, :], ident_f.ap, [gm.key, ident_f.key], [pb.key])
                cp("act", GT.ap[:, tti * 512:(tti + 1) * 512], pb.ap[0:E, :], [pb.key], [GT.key])
            dma("sp", gT_scr, GT.ap, [GT.key], ["gT_scr"], "gts")
            for tti in range(NTT):
                tsl = slice(tti * 512, (tti + 1) * 512)
                for dc in range(KC):
                    pb = PS(2 + dc % 2)
                    mm(pb.ap, b2s.ap[:, dc * 128:(dc + 1) * 128], GT.ap[:, tsl], True, True, [b2s.key, GT.key], [pb.key])
                    stt(xT[:, dc, tsl], pb.ap, der[:, 40 + dc:41 + dc], xT[:, dc, tsl], ALU.mult, ALU.add,
                        [pb.key, "der", ("xT", dc, tti)], [("xT", dc, tti)])
            phase_end()
            phase_begin()
            b1T = A.alloc("b1T", 128, [E, 16])
            dma("sp", b1T.ap, b1T_d[l].rearrange("p (e c) -> p e c", e=E), [], [b1T.key], "misc")
            ts("pool", b1T.ap[:, :, 8:16], b1T.ap[:, :, 8:16], 1.0, None, ALU.add, None, [b1T.key], [b1T.key])
            actT = A.alloc("actT", 128, [4, T], BF16)
            gb = [A.alloc("gb%d" % i, 128, [T]) for i in range(2)]
            gt_ = [A.alloc("g_%d" % i, 128, [512]) for i in range(2)]
            lt_ = [A.alloc("l_%d" % i, 128, [512]) for i in range(2)]
            st_ = [A.alloc("s_%d" % i, 128, [512], BF16) for i in range(2)]
            gg_t = [A.alloc("gg_%d" % i, 128, [512]) for i in range(2)]
            ctr = [0]
            pending = [None]

            def flush_pending():
                if pending[0] is not None:
                    f_ = pending[0]
                    pending[0] = None
                    f_()
            loads = []
            for e in range(E):
                for half in range(2):
                    loads += [(w1_d[l, e, half * 2]), (w1_d[l, e, half * 2 + 1]), (w2_d[l, e, half])]
            issued = [0]
            gidx = [0]

            def next_group():
                gi = gidx[0]
                gidx[0] += 1
                while issued[0] < min(gi + 3, len(loads)):
                    i_ = issued[0]
                    sl_ = slot(i_ % 3)
                    dma("pool", sl_.ap.rearrange("p k c -> p (k c)"), loads[i_], [], [sl_.key], "w")
                    issued[0] += 1
                return slot(gi % 3)

            for e in range(E):
                gbe = gb[e % 2]
                dma("sp", gbe.ap, gT_scr[e:e + 1, :].partition_broadcast(128), ["gT_scr"], [gbe.key], "gb")
                for half in range(2):
                    for cgi in range(2):
                        cg = half * 2 + cgi
                        sl = next_group()
                        for tti in range(NTT):
                            tsl = slice(tti * 512, (tti + 1) * 512)
                            for j in range(2):
                                i2 = ctr[0] % 2
                                ctr[0] += 1
                                ch = cg * 2 + j
                                chl = cgi * 2 + j
                                psg, psl = PS(i2 * 2), PS(i2 * 2 + 1)
                                for kc in range(KC):
                                    mm(psg.ap, sl.ap[:, kc, j * 128:(j + 1) * 128], hT[:, kc, tsl], kc == 0, kc == KC - 1,
                                       [sl.key, ("hT", kc, tti)], [psg.key])
                                for kc in range(KC):
                                    mm(psl.ap, sl.ap[:, kc, 256 + j * 128:256 + (j + 1) * 128], hT[:, kc, tsl], kc == 0, kc == KC - 1,
                                       [sl.key, ("hT", kc, tti)], [psl.key])
                                g_, s_, l_, gg_ = gt_[i2], st_[i2], lt_[i2], gg_t[i2]
                                ts("dve", g_.ap, psg.ap, b1T.ap[:, e, ch:ch + 1], 7.0, ALU.add, ALU.min, [psg.key, b1T.key], [g_.key])
                                tt("dve", gg_.ap, g_.ap, gbe.ap[:, tsl], ALU.mult, [g_.key, gbe.key], [gg_.key])
                                act(s_.ap, g_.ap, AF.Sigmoid, [g_.key], [s_.key], scale=ALPHA)
                                ts("dve", l_.ap, psl.ap, b1T.ap[:, e, 8 + ch:9 + ch], -6.0, ALU.add, ALU.max, [psl.key, b1T.key], [l_.key])
                                tt("pool", gg_.ap, gg_.ap, s_.ap, ALU.mult, [gg_.key, s_.key], [gg_.key])
                                flush_pending()
                                pending[0] = (lambda l_=l_, gg_=gg_, chl=chl, tsl=tsl, tti=tti:
                                              stt(actT.ap[:, chl, tsl], l_.ap, 8.0, gg_.ap, ALU.min, ALU.mult,
                                                  [l_.key, gg_.key], [(actT.key, chl, tti)]))
                    flush_pending()
                    sl = next_group()
                    sl4 = sl.ap.rearrange("p k c -> p (k c)").rearrange("p (k c) -> p k c", k=4)
                    for tti in range(NTT):
                        tsl = slice(tti * 512, (tti + 1) * 512)
                        for dc in range(KC):
                            i2 = ctr[0] % 2
                            ctr[0] += 1
                            pb = PS(4 + i2)
                            for kc in range(4):
                                mm(pb.ap, sl4[:, kc, dc * 128:(dc + 1) * 128], actT.ap[:, kc, tsl], kc == 0, kc == 3,
                                   [sl.key, (actT.key, kc, tti)], [pb.key])
                            stt(xT[:, dc, tsl], pb.ap, der[:, 40 + dc:41 + dc], xT[:, dc, tsl], ALU.mult, ALU.add,
                                [pb.key, "der", ("xT", dc, tti)], [("xT", dc, tti)])
            phase_end()
            sub_end()

        for s in range(NSEQ):
            seq_begin()
            for kc in range(KC):
                dma("sp", xT[:, kc, :], xT_d[s, kc * 128:(kc + 1) * 128, :], [("xT", kc, t) for t in range(NTT)],
                    [("xT", kc, t) for t in range(NTT)], "xin")
            for l in range(DEPTH):
                load_layer_params(l, s)
                attention_sublayer(l, s)
                moe_sublayer(l, s)
            for kc in range(KC):
                dma("sp", outT_d[s, kc * 128:(kc + 1) * 128, :], xT[:, kc, :], [("xT", kc, t) for t in range(NTT)],
                    [("out", s, kc)], "xout")
            seq_end()
        P.emit()
    return nc


def pack_params(inp, DEPTH, E):
    pk128 = np.zeros((DEPTH, 128, P128_W), np.float32)
    pk64 = np.zeros((DEPTH, 64, P64_W), np.float32)

    def put128(l, name, arr):
        o, w = P128[name]
        pk128[l, :, o:o + w] = arr

    def put64(l, name, arr):
        o, w = P64[name]
        pk64[l, :, o:o + w] = arr

    def hl(v):
        return np.asarray(v).reshape(8, 64).T
    for l in range(DEPTH):
        put128(l, "ada_b", inp["ada_b"][l].reshape(48, 128).T)
        put128(l, "n1g", inp["norm1_g"][l].reshape(8, 128).T)
        put128(l, "n2g", inp["norm2_g"][l].reshape(8, 128).T)
        mu = inp["shift_mu"][l]
        put128(l, "mu_g", mu[1664:1792].reshape(128, 1))
        put128(l, "qg", np.tile(inp["q_norm_g"][l], 2).reshape(128, 1))
        put128(l, "kg", np.tile(inp["k_norm_g"][l], 2).reshape(128, 1))
        put128(l, "rb", np.broadcast_to(inp["router_b"][l][None, :], (128, E)) if E == 32 else
               np.pad(np.broadcast_to(inp["router_b"][l][None, :], (128, E)), ((0, 0), (0, 32 - E))))
        put64(l, "mu_r", hl(mu[0:512]))
        put64(l, "mu_k", hl(mu[512:1024]))
        put64(l, "mu_v", hl(mu[1024:1536]))
        put64(l, "mu_w", mu[1536:1600].reshape(64, 1))
        put64(l, "mu_a", mu[1600:1664].reshape(64, 1))
        put64(l, "w0", hl(inp["decay_w0"][l]))
        put64(l, "a0", hl(inp["iclr_a0"][l]))
        put64(l, "k_k", hl(inp["k_k"][l]))
        put64(l, "k_a", hl(inp["k_a"][l]))
        put64(l, "r_k", np.asarray(inp["r_k"][l]).T)
        put64(l, "lnw", hl(inp["lnx_w"][l]))
        put64(l, "lnb", hl(inp["lnx_b"][l]))
        if l > 0:
            put64(l, "vrb", hl(inp["vres_b"][l - 1]))
        put64(l, "sbg", hl(inp["sb_out_g"][l]))
    b1T = np.ascontiguousarray(
        np.asarray(inp["exp_b1"]).reshape(DEPTH, E, 16, 128).transpose(0, 3, 1, 2).reshape(DEPTH, 128, E * 16))
    return pk128, pk64, b1T


_CACHE = {}


def run_model(inp, T, NB, E, DEPTH, n_cores):
    NSEQ = NB // n_cores
    key = (T, NSEQ, E, DEPTH)
    if key not in _CACHE:
        _CACHE[key] = build_program(T, NSEQ, E, DEPTH)
    nc = _CACHE[key]
    f = lambda a: np.ascontiguousarray(np.asarray(a, dtype=np.float32))
    pk128, pk64, b1T = pack_params(inp, DEPTH, E)
    x = np.asarray(inp["x"], dtype=np.float32)
    c = np.asarray(inp["c"], dtype=np.float32)
    w1 = np.asarray(inp["exp_w1"], dtype=np.float32).reshape(DEPTH, E, 8, 128, 2, 4, 256)
    w1r = np.ascontiguousarray(w1.transpose(0, 1, 5, 3, 2, 4, 6)).reshape(DEPTH, E, 4, 128, 4096)
    w2 = np.asarray(inp["exp_w2"], dtype=np.float32).reshape(DEPTH, E, 2, 4, 128, 1024)
    w2r = np.ascontiguousarray(w2.transpose(0, 1, 2, 4, 3, 5)).reshape(DEPTH, E, 2, 128, 4096)
    shared = {
        "ada_w": f(inp["ada_w"]), "w_in": f(inp["w_in"]), "w_out": f(inp["w_out"]),
        "exp_w1r": w1r, "exp_w2r": w2r, "pk128": pk128, "pk64": pk64, "b1T": b1T,
        "exp_b2": f(inp["exp_b2"]), "router_w": f(inp["router_w"]), "decay_up": f(inp["decay_up"]),
        "iclr_up": f(inp["iclr_up"]), "gate_up": f(inp["gate_up"]),
        "vres_down": f(inp["vres_down"]), "vres_up": f(inp["vres_up"]),
    }
    in_maps = []
    for ci in range(n_cores):
        xs = x[ci * NSEQ:(ci + 1) * NSEQ]
        cs = c[ci * NSEQ:(ci + 1) * NSEQ]
        m = dict(shared)
        m["xT"] = np.ascontiguousarray(xs.transpose(0, 2, 1))
        m["cT"] = np.ascontiguousarray(cs.T.reshape(KC, 128, NSEQ).transpose(1, 0, 2))
        in_maps.append(m)
    res = run_bass_kernel_spmd(nc, in_maps, core_ids=list(range(n_cores)))
    outs = [np.asarray(r["outT"]).transpose(0, 2, 1) for r in res.results]
    return np.ascontiguousarray(np.concatenate(outs, axis=0).astype(np.float32))


def kernel(**inputs):
    return run_model(inputs, T=2048, NB=32, E=32, DEPTH=2, n_cores=8)
```

```python
import math
from contextlib import ExitStack
import numpy as np
import concourse.bass as bass
import concourse.mybir as mybir
from concourse.bass_utils import run_bass_kernel_spmd

F32 = mybir.dt.float32
BF16 = mybir.dt.bfloat16
AF = mybir.ActivationFunctionType
ALU = mybir.AluOpType
AX = mybir.AxisListType

ENG = ("pe", "act", "dve", "pool", "sp")
SEM_ROT = 30000


class Op:
    __slots__ = ("eng", "fn", "deps", "raw", "signal", "is_dma", "token", "pos")

    def __init__(self, eng, fn, is_dma):
        self.eng = eng
        self.fn = fn
        self.deps = []
        self.raw = ()
        self.signal = False
        self.is_dma = is_dma
        self.token = None
        self.pos = 0


class Prog:
    def __init__(self, nc, stack):
        self.nc = nc
        self.stack = stack
        self.ops = []
        self.by_eng = {e: [] for e in ENG}
        self.last_w = {}
        self.readers = {}
        self.dma_counts = {}
        self.pending_bar = {e: [] for e in ENG}
        self.last_dma_tokens = {}
        self.dma_rr = {e: 0 for e in ENG}
        self.dma_last_on_sem = {}
        self.flushed = {e: 0 for e in ENG}
        self.cnt = {e: 0 for e in ENG}
        self.waited = {e: {} for e in ENG}
        self.sem_cache = {}

    def op(self, eng, fn, reads=(), writes=(), dma_sem=None):
        o = Op(eng, fn, dma_sem is not None)
        deps = set()
        raw = set()
        for k in reads:
            w = self.last_w.get(k)
            if w is not None:
                deps.add(w)
                raw.add(w)
        for k in writes:
            w = self.last_w.get(k)
            if w is not None:
                deps.add(w)
            for r in self.readers.get(k, ()):
                deps.add(r)
        for b in self.pending_bar[eng]:
            deps.add(b)
            raw.add(b)
        self.pending_bar[eng] = []
        deps.discard(o)
        o.deps = list(deps)
        o.raw = raw
        if dma_sem is not None:
            K = 16
            dma_sem = "%s%d" % (eng, self.dma_rr[eng] % K)
            self.dma_rr[eng] += 1
            prev = self.dma_last_on_sem.get(dma_sem)
            if prev is not None:
                o.deps.append(prev)
            self.dma_last_on_sem[dma_sem] = o
            c = self.dma_counts.get(dma_sem, 0) + 16
            self.dma_counts[dma_sem] = c
            o.token = (dma_sem, c)
            self.last_dma_tokens[dma_sem] = o
        for k in reads:
            self.readers.setdefault(k, []).append(o)
        for k in writes:
            self.last_w[k] = o
            self.readers[k] = []
        o.pos = len(self.by_eng[eng])
        self.ops.append(o)
        self.by_eng[eng].append(o)
        return o

    def barrier(self):
        lasts = []
        for e in ENG:
            for o in reversed(self.by_eng[e][self.flushed[e]:]):
                if not o.is_dma:
                    o.signal = True
                    lasts.append(o)
                    break
        lasts += list(self.last_dma_tokens.values())
        for e in ENG:
            self.pending_bar[e] = list(set(self.pending_bar[e]) | set(lasts))
        self.last_w = {}
        self.readers = {}

    @staticmethod
    def _needs_wait(o, d):
        if d.is_dma:
            return True
        if d.eng != o.eng:
            return True
        if o.is_dma:
            return True
        if d.eng != "pe" and d in o.raw and (o.pos - d.pos) <= 2:
            return True
        return False

    def get_sem(self, name):
        if name not in self.sem_cache:
            self.sem_cache[name] = self.stack.enter_context(self.nc.semaphore(name))
        return self.sem_cache[name]

    def flush(self, final=False):
        nc = self.nc
        new = {e: self.by_eng[e][self.flushed[e]:] for e in ENG}
        for e in ENG:
            for o in new[e]:
                for d in o.deps:
                    if not d.is_dma and self._needs_wait(o, d):
                        assert not (d.fn is None and not d.signal), "dependency on an already-emitted unsignalled op"
                        d.signal = True
        for e in ENG:
            for o in new[e]:
                if o.is_dma:
                    if not isinstance(o.token[0], str) or not o.token[0].startswith("d_"):
                        o.token = ("d_%s" % str(o.token[0]), o.token[1])
                elif o.signal:
                    self.cnt[e] += 1
                    c = self.cnt[e]
                    o.token = ("e_%s_%d" % (e, (c - 1) // SEM_ROT), (c - 1) % SEM_ROT + 1)
                else:
                    o.token = None
        for e in ENG:
            for o in new[e]:
                if o.token is not None:
                    self.get_sem(o.token[0])
        engs = {"pe": nc.tensor, "act": nc.scalar, "dve": nc.vector, "pool": nc.gpsimd, "sp": nc.sync}
        with nc.Block() as block:
            def run(e):
                eng = engs[e]
                waited = self.waited[e]
                for o in new[e]:
                    need = {}
                    for d in o.deps:
                        if not self._needs_wait(o, d):
                            continue
                        sk, v = d.token
                        if waited.get(sk, 0) >= v:
                            continue
                        if need.get(sk, 0) < v:
                            need[sk] = v
                    for sk, v in need.items():
                        eng.wait_ge(self.get_sem(sk), v)
                        waited[sk] = v
                    ins = o.fn(eng)
                    if o.is_dma:
                        ins.then_inc(self.get_sem(o.token[0]), 16)
                    elif o.signal:
                        ins.then_inc(self.get_sem(o.token[0]), 1)
                    o.fn = None

            @block.tensor
            def _(t):
                run("pe")

            @block.scalar
            def _(s):
                run("act")

            @block.vector
            def _(v):
                run("dve")

            @block.gpsimd
            def _(g):
                run("pool")

            @block.sync
            def _(s):
                run("sp")
                if final:
                    for sk, o in self.last_dma_tokens.items():
                        s.wait_ge(self.get_sem(o.token[0]), o.token[1])
        for e in ENG:
            self.flushed[e] = len(self.by_eng[e])

    def emit(self):
        self.barrier()
        self.flush(final=True)


class Buf:
    __slots__ = ("ap", "key")

    def __init__(self, ap, key):
        self.ap = ap
        self.key = key


class Arena:
    def __init__(self, ap_f32, name):
        self.ap = ap_f32
        self.W = ap_f32.shape[1]
        self.off = 0
        self.name = name
        self.gen = 0

    def reset(self):
        self.off = 0
        self.gen += 1

    def alloc(self, tag, parts, free, dtype=F32):
        n = 1
        for f in free:
            n *= f
        words = n if dtype == F32 else (n + 1) // 2
        assert self.off + words <= self.W, (self.name, tag, self.off, words, self.W)
        v = self.ap[0:parts, self.off:self.off + words]
        self.off += words
        if dtype != F32:
            v = v.bitcast(dtype)[:, 0:n]
        if len(free) == 2:
            v = v.rearrange("p (a b) -> p a b", a=free[0])
        elif len(free) == 3:
            v = v.rearrange("p (a b c) -> p a b c", a=free[0], b=free[1])
        return Buf(v, (self.name, self.gen, tag))


D = 1024
KC = 8
HD = 64
NH = 8
D_RWKV = 512
C_RWKV = 3 * D_RWKV + 64 + 64 + 128
C_IN = C_RWKV + 3 * 512
NORM_EPS = 1e-6
GN_EPS = 1e-5 * HD
ALPHA = 1.702
C0 = math.exp(-0.5)

P128 = {}
_o = 0
for _n, _w in (("ada_b", 48), ("n1g", 8), ("n2g", 8), ("mu_g", 1), ("qg", 1), ("kg", 1), ("rb", 32)):
    P128[_n] = (_o, _w)
    _o += _w
P128_W = _o
P64 = {}
_o = 0
for _n, _w in (("mu_r", 8), ("mu_k", 8), ("mu_v", 8), ("mu_w", 1), ("mu_a", 1), ("w0", 8), ("a0", 8),
               ("k_k", 8), ("k_a", 8), ("r_k", 8), ("lnw", 8), ("lnb", 8), ("vrb", 8), ("sbg", 8)):
    P64[_n] = (_o, _w)
    _o += _w
P64_W = _o


def build_program(T, NSEQ, E, DEPTH=2, debug=False):
    assert T % 512 == 0
    NTT = T // 512
    NCH = T // 64
    NG = T // 128
    nc = bass.Bass("TRN2", target_bir_lowering=False)

    def din(name, shape):
        return nc.dram_tensor(name, list(shape), F32, kind="ExternalInput").ap()

    xT_d = din("xT", [NSEQ, D, T])
    cT_d = din("cT", [128, KC, NSEQ])
    ada_w_d = din("ada_w", [DEPTH, D, 6 * D])
    w_in_d = din("w_in", [DEPTH, D, C_IN])
    w_out_d = din("w_out", [DEPTH, D, D])
    w1_d = din("exp_w1r", [DEPTH, E, 4, 128, 4096])
    w2_d = din("exp_w2r", [DEPTH, E, 2, 128, 4096])
    pk128_d = din("pk128", [DEPTH, 128, P128_W])
    pk64_d = din("pk64", [DEPTH, 64, P64_W])
    b1T_d = din("b1T", [DEPTH, 128, E * 16])
    b2_d = din("exp_b2", [DEPTH, E, D])
    rw_d = din("router_w", [DEPTH, D, E])
    dup_d = din("decay_up", [DEPTH, 64, 512])
    iup_d = din("iclr_up", [DEPTH, 64, 512])
    gup_d = din("gate_up", [DEPTH, 128, 512])
    vdn_d = din("vres_down", [max(DEPTH - 1, 1), 512, 32])
    vup_d = din("vres_up", [max(DEPTH - 1, 1), 32, 512])
    outT_d = nc.dram_tensor("outT", [NSEQ, D, T], F32, kind="ExternalOutput").ap()
    gT_scr = nc.dram_tensor("gT_scr", [E, T], F32, kind="Internal").ap()
    vf_scr = nc.dram_tensor("vf_scr", [T // 64, 64, NH * 64], F32, kind="Internal").ap()
    dbg_d = {}

    st = ExitStack()
    with st:
        def sb(name, shape, dt=F32):
            return st.enter_context(nc.sbuf_tensor(name, list(shape), dt))

        P = Prog(nc, st)

        xT = hT = wsl_t = wout_t = local_t = A = psum = None
        uid = [0]
        scopes = {"seq": None, "sub": None, "phase": None}

        def uname(n):
            uid[0] += 1
            return "%s_%d" % (n, uid[0])

        def seq_begin():
            nonlocal xT
            scopes["seq"] = ExitStack()
            xT = scopes["seq"].enter_context(nc.sbuf_tensor(uname("xT"), [128, KC, T], F32))

        def seq_end():
            P.barrier()
            scopes["seq"].close()

        def sub_begin():
            nonlocal hT, wsl_t, wout_t
            ss = scopes["sub"] = ExitStack()
            hT = ss.enter_context(nc.sbuf_tensor(uname("hT"), [128, KC, T], BF16))
            wsl_t = ss.enter_context(nc.sbuf_tensor(uname("wsl"), [128, 3 * 512 * KC], BF16))
            wout_t = ss.enter_context(nc.sbuf_tensor(uname("wout"), [64, 8, D], BF16))

        def sub_end():
            scopes["sub"].close()

        def phase_begin():
            nonlocal local_t, A, psum
            ps_ = scopes["phase"] = ExitStack()
            local_t = ps_.enter_context(nc.sbuf_tensor(uname("loc"), [128, 49 * 256], F32))
            A = Arena(local_t[:], "loc")
            psum = [ps_.enter_context(nc.psum_tensor(uname("ps"), [128, 512], F32)) for i in range(8)]

        def phase_end():
            P.barrier()
            scopes["phase"].close()
        cst_t = sb("consts", [128, 1100])
        cstb_t = sb("constsb", [128, 512], BF16)
        pk128 = sb("pk128_sb", [128, P128_W])
        pk64 = sb("pk64_sb", [64, P64_W])
        mods = sb("mods", [128, DEPTH * 48 * NSEQ])
        der = sb("derived", [128, 64])
        lora_t = sb("lora", [128, 512 * 3 + 8 * 32 + 512])
        state_t = sb("state", [64, 4 * 64])
        carry_t = sb("carry", [128, 32])

        def PS(i):
            return Buf(psum[i][:], ("ps", i))

        def mm(out, lhsT, rhs, start, stop, reads, writes):
            P.op("pe", lambda e: e.matmul(out, lhsT=lhsT, rhs=rhs, start=start, stop=stop), reads, writes)

        def tr(out, in_, ident, reads, writes):
            P.op("pe", lambda e: e.transpose(out, in_, ident), reads, writes)

        def act(out, in_, func, reads, writes, bias=None, scale=None, eng="act"):
            kw = {}
            if bias is not None:
                kw["bias"] = bias
            if scale is not None:
                kw["scale"] = scale
            P.op(eng, lambda e: e.activation(out=out, in_=in_, func=func, **kw), reads, writes)

        def tt(eng, out, in0, in1, op, reads, writes):
            P.op(eng, lambda e: e.tensor_tensor(out=out, in0=in0, in1=in1, op=op), reads, writes)

        def ts(eng, out, in0, s1, s2, op0, op1, reads, writes):
            if s2 is None:
                P.op(eng, lambda e: e.tensor_scalar(out=out, in0=in0, scalar1=s1, scalar2=None, op0=op0), reads, writes)
            else:
                P.op(eng, lambda e: e.tensor_scalar(out=out, in0=in0, scalar1=s1, scalar2=s2, op0=op0, op1=op1), reads, writes)

        def stt(out, in0, scalar, in1, op0, op1, reads, writes):
            P.op("dve", lambda e: e.scalar_tensor_tensor(out=out, in0=in0, scalar=scalar, in1=in1, op0=op0, op1=op1), reads, writes)

        def cp(eng, out, in_, reads, writes):
            if eng == "act":
                P.op("act", lambda e: e.activation(out=out, in_=in_, func=AF.Copy), reads, writes)
            else:
                P.op(eng, lambda e: e.tensor_copy(out=out, in_=in_), reads, writes)

        def memset(eng, ap, val, writes):
            P.op(eng, lambda e: e.memset(ap, val), (), writes)

        def dma(eng, out, in_, reads, writes, sem):
            P.op(eng, lambda e: e.dma_start(out=out, in_=in_), reads, writes, dma_sem=sem)

        def rsqrt(out, in_, scale, bias, reads, writes, tmp):
            act(tmp.ap, in_, AF.Ln, list(reads) + ["c_small"], [tmp.key], bias=bias[0:in_.shape[0], :], scale=scale)
            act(out, tmp.ap, AF.Exp, [tmp.key], writes, scale=-0.5)

        ones_f = Buf(cst_t[:, 0:128], "c_ones")
        ident_f = Buf(cst_t[:, 128:256], "c_ident")
        blk_f = Buf(cst_t[:, 256:384], "c_blk")
        mU = Buf(cst_t[0:64, 384:448], "c_mU")
        mL = Buf(cst_t[0:64, 448:512], "c_mL")
        mUi = Buf(cst_t[0:64, 512:576], "c_mUi")
        scanm = Buf(cst_t[0:64, 576:1088], "c_scan")
        ntri_b = Buf(cstb_t[:, 0:128], "c_ntri")
        nones_b = Buf(cstb_t[:, 128:256], "c_nones")
        zeros_b = Buf(cstb_t[:, 256:320], "c_zeros")
        memset("pool", ones_f.ap, 1.0, [ones_f.key])
        c_eps_n = cst_t[:, 1088:1089]
        c_eps_g = cst_t[:, 1089:1090]
        c_one = cst_t[:, 1090:1091]
        c_zero = cst_t[:, 1091:1092]
        memset("pool", c_eps_n, NORM_EPS, ["c_small"])
        memset("pool", c_eps_g, GN_EPS, ["c_small"])
        memset("pool", c_one, 1.0, ["c_small"])
        memset("pool", c_zero, 0.0, ["c_small"])
        memset("pool", cst_t[:, 256:384], 0.0, [blk_f.key])
        memset("pool", cst_t[0:64, 256:320], 1.0, [blk_f.key])
        memset("pool", cst_t[64:128, 320:384], 1.0, [blk_f.key])

        def asel(out, in_, pattern, cmp_op, base, cm, reads, writes, fill=0.0):
            P.op("pool", lambda e: e.affine_select(out=out, in_=in_, pattern=pattern, compare_op=cmp_op,
                                                   fill=fill, base=base, channel_multiplier=cm), reads, writes)
        asel(ident_f.ap, ones_f.ap, [[-1, 128]], ALU.is_equal, 0, 1, [ones_f.key], [ident_f.key])
        asel(mU.ap, ones_f.ap[0:64, 0:64], [[1, 64]], ALU.is_gt, 0, -1, [ones_f.key], [mU.key])
        asel(mL.ap, ones_f.ap[0:64, 0:64], [[-1, 64]], ALU.is_gt, 0, 1, [ones_f.key], [mL.key])
        asel(mUi.ap, ones_f.ap[0:64, 0:64], [[1, 64]], ALU.is_ge, 0, -1, [ones_f.key], [mUi.key])
        memset("pool", scanm.ap, 1.0, [scanm.key])
        memset("pool", scanm.ap.rearrange("p (h t) -> p h t", h=NH)[:, :, 0:1], 0.0, [scanm.key])
        memset("pool", nones_b.ap, -1.0, [nones_b.key])
        memset("pool", zeros_b.ap, 0.0, [zeros_b.key])
        asel(ntri_b.ap, nones_b.ap, [[-1, 128]], ALU.is_ge, 0, 1, [nones_b.key], [ntri_b.key])

        def slot(i):
            return Buf(wsl_t[:, i * 4096:(i + 1) * 4096].rearrange("p (k c) -> p k c", k=KC), ("wslot", i))
        slot_rr = [0]

        def load_cols(slot_b, segs, w_ap):
            off = 0
            for (c0, n) in segs:
                src = w_ap[:, c0:c0 + n].rearrange("(k p) c -> p k c", p=128)
                dma("pool", slot_b.ap[:, :, off:off + n], src, [], [slot_b.key], ("w", slot_b.key[1]))
                off += n

        sub_begin()
        phase_begin()
        ct = A.alloc("ct", 128, [KC, NSEQ])
        ctb = A.alloc("ctb", 128, [KC, NSEQ], BF16)
        dma("sp", ct.ap, cT_d, [], [ct.key], "misc")
        act(ctb.ap, ct.ap, AF.Silu, [ct.key], [ctb.key])
        mods_v = mods[:].rearrange("p (l j s) -> p l j s", l=DEPTH, j=48)
        mods_b = Buf(mods_v, "mods")
        for l in range(DEPTH):
            pkb = A.alloc("pkb%d" % l, 128, [48])
            dma("sp", pkb.ap, pk128_d[l, :, 0:48], [], [pkb.key], "misc")
            for gi in range(12):
                sl = slot(slot_rr[0] % 3)
                slot_rr[0] += 1
                load_cols(sl, [(gi * 512, 512)], ada_w_d[l])
                pb = PS(gi % 2)
                for j in range(4):
                    for kc in range(KC):
                        mm(pb.ap[:, j * NSEQ:(j + 1) * NSEQ], sl.ap[:, kc, j * 128:(j + 1) * 128], ctb.ap[:, kc, :],
                           kc == 0, kc == KC - 1, [sl.key, ctb.key], [pb.key])
                for j in range(4):
                    jj = gi * 4 + j
                    act(mods_v[:, l, jj, :], pb.ap[:, j * NSEQ:(j + 1) * NSEQ], AF.Identity, [pb.key, pkb.key],
                        [mods_b.key], bias=pkb.ap[:, jj:jj + 1])
        phase_end()
        sub_end()

        def pk(name, c=None):
            o, w = P128[name]
            return pk128[:, o:o + w] if c is None else pk128[:, o + c:o + c + 1]

        def p64(name, h=None):
            o, w = P64[name]
            return pk64[:, o:o + w] if h is None else pk64[:, o + h:o + h + 1]

        def p64b(name, n):
            o, w = P64[name]
            return pk64[:, o:o + w].unsqueeze(2).to_broadcast([64, w, n])

        der_b = Buf(der[:], "der")
        omk_t = Buf(carry_t[0:64, 24:32], "omk")

        def load_layer_params(l, s):
            dma("sp", pk128[:], pk128_d[l], [], ["pk128"], "misc")
            dma("sp", pk64[:], pk64_d[l], [], ["pk64"], "misc")
            mv = mods_v[:, l, :, s]
            o1, _ = P128["n1g"]
            o2, _ = P128["n2g"]
            stt(der[:, 0:8], mv[:, 8:16], 1.0, pk128[:, o1:o1 + 8], ALU.add, ALU.mult, ["mods", "pk128"], ["der"])
            cp("dve", der[:, 8:16], mv[:, 0:8], ["mods"], ["der"])
            cp("dve", der[:, 16:24], mv[:, 16:24], ["mods"], ["der"])
            stt(der[:, 24:32], mv[:, 32:40], 1.0, pk128[:, o2:o2 + 8], ALU.add, ALU.mult, ["mods", "pk128"], ["der"])
            cp("dve", der[:, 32:40], mv[:, 24:32], ["mods"], ["der"])
            cp("dve", der[:, 40:48], mv[:, 40:48], ["mods"], ["der"])
            ts("dve", der[:, 48:49], pk("qg"), 0.125, None, ALU.mult, None, ["pk128"], ["der"])
            ts("dve", omk_t.ap, p64("k_a"), -1.0, 1.0, ALU.mult, ALU.add, ["pk64"], [omk_t.key])
            dma("sp", lora_t[0:64, 0:512], dup_d[l], [], ["lora"], "misc")
            dma("sp", lora_t[0:64, 512:1024], iup_d[l], [], ["lora"], "misc")
            dma("sp", lora_t[:, 1024:1536], gup_d[l], [], ["lora"], "misc")
            if l > 0:
                dma("sp", lora_t[0:64, 1536:1792].rearrange("p (h c) -> p h c", h=NH),
                    vdn_d[l - 1].rearrange("(h p) c -> p h c", p=64), [], ["lora"], "misc")
                dma("sp", lora_t[0:32, 1792:2304], vup_d[l - 1], [], ["lora"], "misc")

        def rms_modulate(gcol, shcol, tmpA, emit_tile, tmpH=None):
            for tti in range(NTT):
                tsl = slice(tti * 512, (tti + 1) * 512)
                pss = PS(7)
                for kc in range(KC):
                    sq = tmpA[kc % 2]
                    act(sq.ap, xT[:, kc, tsl], AF.Square, [("xT", kc, tti)], [sq.key])
                    mm(pss.ap, ones_f.ap, sq.ap, kc == 0, kc == KC - 1, [ones_f.key, sq.key], [pss.key])
                rstd = tmpA[2]
                rsqrt(rstd.ap, pss.ap, 1.0 / D, c_eps_n, [pss.key], [rstd.key], tmpA[3])
                tiles = []
                for kc in range(KC):
                    t1 = tmpH[kc] if tmpH is not None else tmpA[4 + kc % 2]
                    tt("dve", t1.ap, xT[:, kc, tsl], rstd.ap, ALU.mult, [("xT", kc, tti), rstd.key], [t1.key])
                    ts("pool", t1.ap, t1.ap, der[:, gcol + kc:gcol + kc + 1], der[:, shcol + kc:shcol + kc + 1],
                       ALU.mult, ALU.add, [t1.key, "der"], [t1.key])
                    cp("act", hT[:, kc, tsl], t1.ap, [t1.key], [("hT", kc, tti)])
                    tiles.append(t1)
                if emit_tile is not None:
                    emit_tile(tti, tiles)

        def attention_sublayer(l, s):
            sub_begin()
            phase_begin()
            tmpA = [A.alloc("nt%d" % i, 128, [512]) for i in range(6)]
            rms_modulate(0, 8, tmpA, None)
            phase_end()

            wl = w_in_d[l]
            woutb = Buf(wout_t[:], "wout")
            dma("pool", wout_t[:], w_out_d[l, 0:512, :].rearrange("(h p) c -> p h c", p=64), [], [woutb.key], "wout")
            HP = 4
            for hg in range(2):
                phase_begin()
                wr = Buf(wsl_t[:, 0:KC * 1280].rearrange("p (k c) -> p k c", k=KC), ("wslot", "rwkv", hg))
                segs = [(hg * 256, 256, 0), (512 + hg * 256, 256, 256), (1024, 512, 512), (1536, 256, 1024)]
                for (c0_, n_, o_) in segs:
                    dma("pool", wr.ap[:, :, o_:o_ + n_], wl[:, c0_:c0_ + n_].rearrange("(k p) c -> p k c", p=128),
                        [], [wr.key, ("wslot", 0), ("wslot", 1), ("wslot", 2)], "wrwkv")

                def HB(tag, dt=F32, nh=HP):
                    return A.alloc(tag, 64, [nh, 64], dt)
                zb = {"r": A.alloc("zb_r", 64, [HP, 65]), "k": A.alloc("zb_k", 64, [HP, 65]), "v": A.alloc("zb_v", 64, [NH, 65])}
                zbl = A.alloc("zb_l", 64, [2, 65])
                zbg = A.alloc("zb_g", 128, [65])
                for b_ in list(zb.values()) + [zbl, zbg]:
                    memset("pool", b_.ap, 0.0, [b_.key])
                R, K = HB("R"), HB("K")
                V8 = HB("V8", nh=NH)
                V = Buf(V8.ap[:, hg * HP:(hg + 1) * HP, :], V8.key)
                T8 = HB("T8", nh=NH)
                B1, B2, B3, B4, B5, B6 = (HB("B%d" % i) for i in range(1, 7))
                lw = A.alloc("lw", 64, [2, 64])
                lg = A.alloc("lg", 128, [64])
                pcs = A.alloc("pcs", 64, [HP, 1])
                vd = A.alloc("vd", 32, [64])
                Nn, NTt, Pa, PTa = HB("N"), HB("NT"), HB("Pa"), HB("PTa")
                S_ = HB("S")
                AakT, ArkT, ArbT = HB("AakT"), HB("ArkT"), HB("ArbT")
                Vt, Ktt, Btt = HB("Vt"), HB("Ktt"), HB("Btt")
                RHS, U_ = B1, B4
                yr = A.alloc("yr", 64, [HP, 64], BF16)
                xtmp = A.alloc("xtmp", 128, [KC, 64])
                M = Buf(state_t[:].rearrange("p (h v) -> p h v", h=HP), "state")
                memset("pool", M.ap, 0.0, [M.key])
                dup = lora_t[0:64, 0:512]
                iup = lora_t[0:64, 512:1024]
                gup = lora_t[:, 1024:1536]
                vdn = lora_t[0:64, 1536:1792].rearrange("p (h c) -> p h c", h=NH)
                vup = lora_t[0:32, 1792:2304]
                idf = ident_f.ap[0:64, 0:64]
                o64 = ones_f.ap[0:64, 0:64]
                mUb = mU.ap.unsqueeze(1).to_broadcast([64, HP, 64])
                mLb = mL.ap.unsqueeze(1).to_broadcast([64, HP, 64])
                mUib = mUi.ap.unsqueeze(1).to_broadcast([64, HP, 64])
                idb = idf.unsqueeze(1).to_broadcast([64, HP, 64])
                gate1b = der[:, 16:24].unsqueeze(2).to_broadcast([128, KC, 64])
                scm = scanm.ap[:, 0:HP * 64]
                W4 = HP * 64

                def f2(b_):
                    return b_.ap.rearrange("p h t -> p (h t)")

                def pv(pb_, nh=HP):
                    return pb_.ap[0:64, 0:nh * 64].rearrange("p (h t) -> p h t", h=nh)

                def pg(name, n=64):
                    o, w = P64[name]
                    return pk64[:, o + hg * HP:o + (hg + 1) * HP].unsqueeze(2).to_broadcast([64, HP, n])

                for c in range(NCH):
                    t0 = c * 64
                    tsl = slice(t0, t0 + 64)
                    hk = [("hT", kc, t0 // 512) for kc in range(KC)]
                    pb = PS(0)
                    for gi in range(2):
                        for h in range(HP):
                            col = gi * 256 + h * 64
                            oc = gi * 256 + h * 64
                            for kc in range(KC):
                                mm(pb.ap[0:64, oc:oc + 64], wr.ap[:, kc, col:col + 64], hT[:, kc, tsl],
                                   kc == 0, kc == KC - 1, [wr.key, hk[kc]], [pb.key])
                    cp("act", zb["r"].ap[:, :, 1:65], pb.ap[0:64, 0:256].rearrange("p (h t) -> p h t", h=HP), [pb.key], [zb["r"].key])
                    cp("act", zb["k"].ap[:, :, 1:65], pb.ap[0:64, 256:512].rearrange("p (h t) -> p h t", h=HP), [pb.key], [zb["k"].key])
                    pb = PS(1)
                    for h in range(NH):
                        for kc in range(KC):
                            mm(pb.ap[0:64, h * 64:(h + 1) * 64], wr.ap[:, kc, 512 + h * 64:512 + (h + 1) * 64], hT[:, kc, tsl],
                               kc == 0, kc == KC - 1, [wr.key, hk[kc]], [pb.key])
                    cp("act", zb["v"].ap[:, :, 1:65], pv(pb, NH), [pb.key], [zb["v"].key])
                    pb = PS(3)
                    for j in range(2):
                        for kc in range(KC):
                            mm(pb.ap[0:64, j * 64:(j + 1) * 64], wr.ap[:, kc, 1024 + j * 64:1024 + (j + 1) * 64], hT[:, kc, tsl],
                               kc == 0, kc == KC - 1, [wr.key, hk[kc]], [pb.key])
                    for kc in range(KC):
                        mm(pb.ap[:, 128:192], wr.ap[:, kc, 1152:1280], hT[:, kc, tsl], kc == 0, kc == KC - 1, [wr.key, hk[kc]], [pb.key])
                    cp("act", zbl.ap[:, :, 1:65], pb.ap[0:64, 0:128].rearrange("p (h t) -> p h t", h=2), [pb.key], [zbl.key])
                    cp("act", zbg.ap[:, 1:65], pb.ap[:, 128:192], [pb.key], [zbg.key])
                    for n, dst, tmpb, mub in (("r", R, B1, pg("mu_r")), ("k", K, B1, pg("mu_k")), ("v", V8, T8, p64b("mu_v", 64))):
                        z = zb[n]
                        tt("dve", tmpb.ap, z.ap[:, :, 0:64], z.ap[:, :, 1:65], ALU.subtract, [z.key], [tmpb.key])
                        tt("dve", tmpb.ap, tmpb.ap, mub, ALU.mult, [tmpb.key, "pk64"], [tmpb.key])
                        tt("dve", dst.ap, tmpb.ap, z.ap[:, :, 1:65], ALU.add, [tmpb.key, z.key], [dst.key])
                        cp("act", z.ap[:, :, 0:1], z.ap[:, :, 64:65], [z.key], [z.key])
                    tt("dve", lw.ap, zbl.ap[:, :, 0:64], zbl.ap[:, :, 1:65], ALU.subtract, [zbl.key], [lw.key])
                    o_w, _ = P64["mu_w"]
                    tt("dve", lw.ap, lw.ap, pk64[:, o_w:o_w + 2].unsqueeze(2).to_broadcast([64, 2, 64]), ALU.mult, [lw.key, "pk64"], [lw.key])
                    tt("dve", lw.ap, lw.ap, zbl.ap[:, :, 1:65], ALU.add, [lw.key, zbl.key], [lw.key])
                    cp("act", zbl.ap[:, :, 0:1], zbl.ap[:, :, 64:65], [zbl.key], [zbl.key])
                    tt("dve", lg.ap, zbg.ap[:, 0:64], zbg.ap[:, 1:65], ALU.subtract, [zbg.key], [lg.key])
                    stt(lg.ap, lg.ap, pk("mu_g"), zbg.ap[:, 1:65], ALU.mult, ALU.add, [lg.key, zbg.key, "pk128"], [lg.key])
                    cp("act", zbg.ap[:, 0:1], zbg.ap[:, 64:65], [zbg.key], [zbg.key])
                    act(lw.ap[:, 0, :], lw.ap[:, 0, :], AF.Tanh, [lw.key], [lw.key])
                    act(lg.ap, lg.ap, AF.Tanh, [lg.key], [lg.key], scale=0.5)
                    ts("dve", lg.ap, lg.ap, 0.5, 0.5, ALU.mult, ALU.add, [lg.key], [lg.key])
                    pb = PS(4)
                    for h in range(HP):
                        hgl = hg * HP + h
                        mm(pb.ap[0:64, h * 64:(h + 1) * 64], dup[:, hgl * 64:(hgl + 1) * 64], lw.ap[:, 0, :], True, True, ["lora", lw.key], [pb.key])
                    tt("dve", B1.ap, pv(pb), pg("w0"), ALU.add, [pb.key, "pk64"], [B1.key])
                    act(B1.ap, B1.ap, AF.Tanh, [B1.key], [B1.key], scale=0.5)
                    ts("dve", B1.ap, B1.ap, 0.5, 0.5, ALU.mult, ALU.add, [B1.key], [B1.key])
                    P.op("dve", lambda e, B1=B1, B2=B2: e.tensor_tensor_scan(out=f2(B2), data0=scm, data1=f2(B1), initial=0.0,
                                                                             op0=ALU.mult, op1=ALU.add), [B1.key, scanm.key], [B2.key])
                    act(B3.ap, B2.ap, AF.Exp, [B2.key], [B3.key], scale=-C0)
                    act(B4.ap, B2.ap, AF.Exp, [B2.key], [B4.key], scale=C0)
                    tt("dve", B1.ap, B2.ap, B1.ap, ALU.subtract, [B2.key, B1.key], [B1.key])
                    act(B1.ap, B1.ap, AF.Exp, [B1.key], [B1.key], scale=-C0)
                    cp("act", pcs.ap, B3.ap[:, :, 63:64], [B3.key], [pcs.key])
                    pb = PS(5)
                    for h in range(HP):
                        hgl = hg * HP + h
                        mm(pb.ap[0:64, h * 64:(h + 1) * 64], iup[:, hgl * 64:(hgl + 1) * 64], lw.ap[:, 1, :], True, True, ["lora", lw.key], [pb.key])
                    tt("dve", B2.ap, pv(pb), pg("a0"), ALU.add, [pb.key, "pk64"], [B2.key])
                    act(B2.ap, B2.ap, AF.Tanh, [B2.key], [B2.key], scale=0.5)
                    ts("dve", B2.ap, B2.ap, 0.5, 0.5, ALU.mult, ALU.add, [B2.key], [B2.key])
                    if l == 0:
                        if hg == 0:
                            dma("sp", vf_scr[c], f2(V8), [V8.key], [("vf", c)], "vf")
                    else:
                        dma("sp", f2(B5), vf_scr[c, :, hg * HP * 64:(hg + 1) * HP * 64], [("vf", c)], [B5.key], "vf")
                        pb = PS(6)
                        for h in range(NH):
                            mm(pb.ap[0:32, 0:64], vdn[:, h, :], V8.ap[:, h, :], h == 0, h == NH - 1, ["lora", V8.key], [pb.key])
                        cp("act", vd.ap, pb.ap[0:32, 0:64], [pb.key], [vd.key])
                        pb = PS(7)
                        for h in range(HP):
                            hgl = hg * HP + h
                            mm(pb.ap[0:64, h * 64:(h + 1) * 64], vup[:, hgl * 64:(hgl + 1) * 64], vd.ap, True, True, ["lora", vd.key], [pb.key])
                        tt("dve", B6.ap, pv(pb), pg("vrb"), ALU.add, [pb.key, "pk64"], [B6.key])
                        act(B6.ap, B6.ap, AF.Tanh, [B6.key], [B6.key], scale=0.5)
                        ts("dve", B6.ap, B6.ap, 0.5, 0.5, ALU.mult, ALU.add, [B6.key], [B6.key])
                        tt("dve", B5.ap, B5.ap, V.ap, ALU.subtract, [B5.key, V.key], [B5.key])
                        tt("dve", B5.ap, B5.ap, B6.ap, ALU.mult, [B5.key, B6.key], [B5.key])
                        tt("dve", V.ap, V.ap, B5.ap, ALU.add, [V.key, B5.key], [V.key])
                    tt("dve", B5.ap, K.ap, pg("k_k"), ALU.mult, [K.key, "pk64"], [B5.key])
                    act(B6.ap, B5.ap, AF.Square, [B5.key], [B6.key])
                    pb = PS(6)
                    mm(pb.ap[0:64, 0:W4], o64, f2(B6), True, True, [ones_f.key, B6.key], [pb.key])
                    ts("dve", B6.ap, pv(pb), 1e-24, None, ALU.max, None, [pb.key], [B6.key])
                    rsqrt(B6.ap, B6.ap, 1.0, c_zero, [B6.key], [B6.key], Nn)
                    tt("dve", B5.ap, B5.ap, B6.ap, ALU.mult, [B5.key, B6.key], [B5.key])
                    stt(B6.ap, B5.ap, -1.0, B2.ap, ALU.mult, ALU.mult, [B5.key, B2.key], [B6.key])
                    tt("dve", B6.ap, B6.ap, B4.ap, ALU.mult, [B6.key, B4.key], [B6.key])
                    tt("dve", B5.ap, B5.ap, B1.ap, ALU.mult, [B5.key, B1.key], [B5.key])
                    tt("dve", B1.ap, B2.ap, pg("k_a"), ALU.mult, [B2.key, "pk64"], [B1.key])
                    tt("dve", B1.ap, B1.ap, omk_t.ap[:, hg * HP:(hg + 1) * HP].unsqueeze(2).to_broadcast([64, HP, 64]), ALU.add,
                       [B1.key, omk_t.key], [B1.key])
                    tt("dve", B1.ap, B1.ap, K.ap, ALU.mult, [B1.key, K.key], [B1.key])
                    tt("dve", B2.ap, R.ap, B1.ap, ALU.mult, [R.key, B1.key], [B2.key])
                    tt("dve", B2.ap, B2.ap, pg("r_k"), ALU.mult, [B2.key, "pk64"], [B2.key])
                    pb = PS(7)
                    mm(pb.ap[0:64, 0:W4], o64, f2(B2), True, True, [ones_f.key, B2.key], [pb.key])
                    tt("dve", B2.ap, pv(pb), V.ap, ALU.mult, [pb.key, V.key], [B2.key])
                    tt("dve", K.ap, B1.ap, B4.ap, ALU.mult, [B1.key, B4.key], [K.key])
                    tt("dve", R.ap, R.ap, B3.ap, ALU.mult, [R.key, B3.key], [R.key])
                    pb = PS(4)
                    for h in range(HP):
                        hgl = hg * HP + h
                        mm(pb.ap[0:64, h * 64:(h + 1) * 64], gup[:, hgl * 64:(hgl + 1) * 64], lg.ap, True, True, ["lora", lg.key], [pb.key])
                    cp("act", B3.ap, pv(pb), [pb.key], [B3.key])
                    for src, dstb, bank in ((V, Vt, 0), (K, Ktt, 1), (B6, Btt, 2)):
                        pb = PS(bank)
                        for h in range(HP):
                            tr(pb.ap[0:64, h * 64:(h + 1) * 64], src.ap[:, h, :], idf, [src.key, ident_f.key], [pb.key])
                        cp("act", f2(dstb), pb.ap[0:64, 0:W4], [pb.key], [dstb.key])
                    pb, pb2 = PS(3), PS(5)
                    for h in range(HP):
                        mm(pb.ap[0:64, h * 64:(h + 1) * 64], B6.ap[:, h, :], B5.ap[:, h, :], True, True, [B6.key, B5.key], [pb.key])
                        mm(pb2.ap[0:64, h * 64:(h + 1) * 64], B5.ap[:, h, :], B6.ap[:, h, :], True, True, [B6.key, B5.key], [pb2.key])
                    tt("dve", Nn.ap, pv(pb), mUb, ALU.mult, [pb.key, mU.key], [Nn.key])
                    tt("dve", NTt.ap, pv(pb2), mLb, ALU.mult, [pb2.key, mL.key], [NTt.key])
                    tt("dve", S_.ap, Nn.ap, idb, ALU.add, [Nn.key, ident_f.key], [S_.key])
                    pb = PS(6)
                    for h in range(HP):
                        mm(pb.ap[0:64, h * 64:(h + 1) * 64], K.ap[:, h, :], B5.ap[:, h, :], True, True, [K.key, B5.key], [pb.key])
                    tt("dve", AakT.ap, pv(pb), mUb, ALU.mult, [pb.key, mU.key], [AakT.key])
                    pb = PS(7)
                    for h in range(HP):
                        mm(pb.ap[0:64, h * 64:(h + 1) * 64], K.ap[:, h, :], R.ap[:, h, :], True, True, [K.key, R.key], [pb.key])
                    tt("dve", ArkT.ap, pv(pb), mUib, ALU.mult, [pb.key, mUi.key], [ArkT.key])
                    pb = PS(4)
                    for h in range(HP):
                        mm(pb.ap[0:64, h * 64:(h + 1) * 64], B6.ap[:, h, :], R.ap[:, h, :], True, True, [B6.key, R.key], [pb.key])
                    tt("dve", ArbT.ap, pv(pb), mUib, ALU.mult, [pb.key, mUi.key], [ArbT.key])
                    pb = PS(6)
                    for h in range(HP):
                        mm(pb.ap[0:64, h * 64:(h + 1) * 64], B5.ap[:, h, :], M.ap[:, h, :], True, False, [B5.key, M.key], [pb.key])
                        mm(pb.ap[0:64, h * 64:(h + 1) * 64], AakT.ap[:, h, :], Vt.ap[:, h, :], False, True, [AakT.key, Vt.key], [pb.key])
                    cp("act", f2(RHS), pb.ap[0:64, 0:W4], [pb.key], [RHS.key])
                    Pc, PTc, Pn, PTn = Nn, NTt, Pa, PTa
                    for lev in range(1, 6):
                        last = lev == 5
                        pbT = PS(lev % 2)
                        for h in range(HP):
                            mm(pbT.ap[0:64, h * 64:(h + 1) * 64], Pc.ap[:, h, :], PTc.ap[:, h, :], True, True, [Pc.key, PTc.key], [pbT.key])
                        if not last:
                            pbN = PS(2 + lev % 2)
                            for h in range(HP):
                                mm(pbN.ap[0:64, h * 64:(h + 1) * 64], PTc.ap[:, h, :], Pc.ap[:, h, :], True, True, [Pc.key, PTc.key], [pbN.key])
                        cp("act", f2(PTn), pbT.ap[0:64, 0:W4], [pbT.key], [PTn.key])
                        if not last:
                            cp("dve", f2(Pn), pbN.ap[0:64, 0:W4], [pbN.key], [Pn.key])
                        pbS = PS(5)
                        for h in range(HP):
                            mm(pbS.ap[0:64, h * 64:(h + 1) * 64], PTn.ap[:, h, :], S_.ap[:, h, :], True, True, [PTn.key, S_.key], [pbS.key])
                        tt("dve", f2(S_), f2(S_), pbS.ap[0:64, 0:W4], ALU.add, [pbS.key, S_.key], [S_.key])
                        Pc, PTc, Pn, PTn = Pn, PTn, Pc, PTc
                    pb = PS(7)
                    for h in range(HP):
                        mm(pb.ap[0:64, h * 64:(h + 1) * 64], S_.ap[:, h, :], RHS.ap[:, h, :], True, True, [S_.key, RHS.key], [pb.key])
                    cp("act", f2(U_), pb.ap[0:64, 0:W4], [pb.key], [U_.key])
                    pby = PS(4)
                    for h in range(HP):
                        o_ = pby.ap[0:64, h * 64:(h + 1) * 64]
                        mm(o_, M.ap[:, h, :], R.ap[:, h, :], True, False, [M.key, R.key], [pby.key])
                        mm(o_, Vt.ap[:, h, :], ArkT.ap[:, h, :], False, False, [Vt.key, ArkT.key], [pby.key])
                        mm(o_, U_.ap[:, h, :], ArbT.ap[:, h, :], False, True, [U_.key, ArbT.key], [pby.key])
                    pb = PS(0)
                    for h in range(HP):
                        o_ = pb.ap[0:64, h * 64:(h + 1) * 64]
                        mm(o_, Ktt.ap[:, h, :], Vt.ap[:, h, :], True, False, [Ktt.key, Vt.key], [pb.key])
                        mm(o_, Btt.ap[:, h, :], U_.ap[:, h, :], False, True, [Btt.key, U_.key], [pb.key])
                    tt("dve", M.ap, M.ap, pv(pb), ALU.add, [M.key, pb.key], [M.key])
                    tt("dve", M.ap, M.ap, pcs.ap.to_broadcast([64, HP, 64]), ALU.mult, [M.key, pcs.key], [M.key])
                    cp("act", f2(B1), pby.ap[0:64, 0:W4], [pby.key], [B1.key])
                    pb = PS(1)
                    mm(pb.ap[0:64, 0:W4], o64, f2(B1), True, True, [ones_f.key, B1.key], [pb.key])
                    stt(f2(B1), pb.ap[0:64, 0:W4], -1.0 / 64, f2(B1), ALU.mult, ALU.add, [pb.key, B1.key], [B1.key])
                    act(B4.ap, B1.ap, AF.Square, [B1.key], [B4.key])
                    pb = PS(2)
                    mm(pb.ap[0:64, 0:W4], o64, f2(B4), True, True, [ones_f.key, B4.key], [pb.key])
                    rsqrt(f2(B4), pb.ap[0:64, 0:W4], 1.0 / 64, c_eps_g, [pb.key], [B4.key], Buf(f2(Nn), Nn.key))
                    tt("dve", B1.ap, B1.ap, B4.ap, ALU.mult, [B1.key, B4.key], [B1.key])
                    tt("dve", B1.ap, B1.ap, pg("lnw"), ALU.mult, [B1.key, "pk64"], [B1.key])
                    tt("dve", B1.ap, B1.ap, pg("lnb"), ALU.add, [B1.key, "pk64"], [B1.key])
                    tt("dve", B1.ap, B1.ap, B2.ap, ALU.add, [B1.key, B2.key], [B1.key])
                    tt("dve", yr.ap, B1.ap, B3.ap, ALU.mult, [B1.key, B3.key], [yr.key])
                    pb = PS(3)
                    for dc in range(KC):
                        for h in range(HP):
                            mm(pb.ap[:, dc * 64:(dc + 1) * 64], wout_t[:, hg * HP + h, dc * 128:(dc + 1) * 128], yr.ap[:, h, :],
                               h == 0, h == HP - 1, [woutb.key, yr.key], [pb.key])
                    tt("dve", xtmp.ap, pb.ap.rearrange("p (k t) -> p k t", k=KC), gate1b, ALU.mult, [pb.key, "der"], [xtmp.key])
                    xk = [("xT", kc, t0 // 512) for kc in range(KC)]
                    tt("dve", xT[:, :, tsl], xT[:, :, tsl], xtmp.ap, ALU.add, [xtmp.key] + xk, xk)
                phase_end()

            pass
            phase_begin()
            dma("pool", wout_t[:], w_out_d[l, 512:1024, :].rearrange("(h p) c -> p h c", p=64), [], [woutb.key], "wout")
            qT = A.alloc("qT", 128, [T], BF16)
            kT = A.alloc("kT", 128, [T], BF16)
            Vs = A.alloc("Vs", 128, [NG, 128], BF16)
            qraw = [A.alloc("qraw%d" % i, 128, [512]) for i in range(2)]
            qsq = [A.alloc("qsq0", 128, [512])] * 2
            qtmp = A.alloc("qtmp", 128, [512])
            etmp = [A.alloc("etmp%d" % i, 128, [512], BF16) for i in range(4)]
            spb = [A.alloc("spb%d" % i, 128, [512], BF16) for i in range(4)]
            wTb = [A.alloc("wTb%d" % i, 128, [512], BF16) for i in range(4)]
            spsum2 = [A.alloc("spsum%d" % i, 128, [512]) for i in range(2)]
            spsb = [A.alloc("spsb%d" % i, 128, [512], BF16) for i in range(4)]
            osb = A.alloc("osb", 64, [512])
            osq = A.alloc("osq", 64, [512])
            ors = A.alloc("ors", 64, [512])
            otm = osq
            ysb = A.alloc("ysb", 64, [2, 512], BF16)
            it = [0]
            itc = [0, 0]
            for hp in range(4):
                sl = slot(slot_rr[0] % 3)
                slot_rr[0] += 1
                load_cols(sl, [(C_RWKV + hp * 128, 128), (C_RWKV + 512 + hp * 128, 128), (C_RWKV + 1024 + hp * 128, 128)], wl)
                for which, dst, gcol in ((0, qT, der[:, 48:49]), (1, kT, pk("kg"))):
                    for tti in range(NTT):
                        tsl = slice(tti * 512, (tti + 1) * 512)
                        pb = PS(which * 2 + tti % 2)
                        for kc in range(KC):
                            mm(pb.ap, sl.ap[:, kc, which * 128:(which + 1) * 128], hT[:, kc, tsl], kc == 0, kc == KC - 1,
                               [sl.key, ("hT", kc, tti)], [pb.key])
                        qr, qs = qraw[tti % 2], qsq[tti % 2]
                        cp("act", qr.ap, pb.ap, [pb.key], [qr.key])
                        act(qs.ap, qr.ap, AF.Square, [qr.key], [qs.key])
                        pb2 = PS(4 + tti % 2)
                        mm(pb2.ap, blk_f.ap, qs.ap, True, True, [blk_f.key, qs.key], [pb2.key])
                        rsqrt(qs.ap, pb2.ap, 1.0 / 64, c_eps_n, [pb2.key], [qs.key], qtmp)
                        tt("dve", qr.ap, qr.ap, qs.ap, ALU.mult, [qr.key, qs.key], [qr.key])
                        ts("dve", dst.ap[:, tsl], qr.ap, gcol, None, ALU.mult, None, [qr.key, "der", "pk128"], [(dst.key, tti)])
                for g in range(NG):
                    pb = PS(6 + g % 2)
                    for kc in range(KC):
                        mm(pb.ap[:, 0:128], hT[:, kc, g * 128:(g + 1) * 128], sl.ap[:, kc, 256:384], kc == 0, kc == KC - 1,
                           [sl.key, ("hT", kc, g // 4)], [pb.key])
                    cp("act", Vs.ap[:, g, :], pb.ap[:, 0:128], [pb.key], [(Vs.key, g)])
                qk_all = [(qT.key, i) for i in range(NTT)] + [(kT.key, i) for i in range(NTT)]
                for QT in range(NTT):
                    qsl = slice(QT * 512, (QT + 1) * 512)
                    nkb = 4 * QT + 4
                    for hh in range(2):
                        po = PS(4 + hh)
                        mm(po.ap[0:64, :], zeros_b.ap, kT.ap[:, qsl], True, False, [zeros_b.key] + qk_all, [po.key])
                        memset("pool", spsum2[hh].ap, 0.0, [spsum2[hh].key])
                    for sbk in range(nkb - 1, -1, -1):
                        for hh in range(2):
                            pbase = hh * 64
                            po = PS(4 + hh)
                            spsum = spsum2[hh]
                            c0 = max(0, 128 * sbk - 512 * QT)
                            diag = sbk >= 4 * QT
                            i2 = hh
                            itc[hh] += 1
                            i4 = hh * 2 + itc[hh] % 2
                            ksl = slice(sbk * 128, (sbk + 1) * 128)
                            q_ap = qT.ap[pbase:pbase + 64, QT * 512 + c0:(QT + 1) * 512]
                            k_ap = kT.ap[pbase:pbase + 64, ksl]
                            ps1 = PS(i2)
                            mm(ps1.ap[:, c0:512], k_ap, q_ap, True, True, qk_all, [ps1.key])
                            et, sp, wT_, sps = etmp[i4], spb[i4], wTb[i4], spsb[i4]
                            act(et.ap[:, c0:512], ps1.ap[:, c0:512], AF.Exp, [ps1.key], [et.key])
                            act(sp.ap[:, c0:512], et.ap[:, c0:512], AF.Ln, [et.key, "c_small"], [sp.key], bias=c_one)
                            if diag:
                                asel(sp.ap[:, c0:c0 + 128], sp.ap[:, c0:c0 + 128], [[1, 128]], ALU.is_gt, 0, -1, [sp.key], [sp.key])
                            ps2 = PS(2 + i2)
                            first = sbk == nkb - 1
                            mm(ps2.ap[:, c0:512], k_ap, q_ap, True, False, qk_all, [ps2.key])
                            mm(ps2.ap[:, c0:512], ntri_b.ap, sp.ap[:, c0:512], False, first, [ntri_b.key, sp.key], [ps2.key])
                            if not first:
                                cp("act", sps.ap[:, c0:512], spsum.ap[:, c0:512], [spsum.key], [sps.key])
                                mm(ps2.ap[:, c0:512], nones_b.ap, sps.ap[:, c0:512], False, True, [nones_b.key, sps.key], [ps2.key])
                            act(wT_.ap[:, c0:512], ps2.ap[:, c0:512], AF.Exp, [ps2.key], [wT_.key])
                            if diag:
                                asel(wT_.ap[:, c0:c0 + 128], wT_.ap[:, c0:c0 + 128], [[1, 128]], ALU.is_gt, 0, -1, [wT_.key], [wT_.key])
                            mm(po.ap[0:64, c0:512], Vs.ap[:, sbk, pbase:pbase + 64], wT_.ap[:, c0:512], False, sbk == 0,
                               [(Vs.key, sbk), wT_.key], [po.key])
                            if sbk > 0:
                                tt("dve", spsum.ap[:, c0:512], spsum.ap[:, c0:512], sp.ap[:, c0:512], ALU.add, [spsum.key, sp.key], [spsum.key])
                    for hh in range(2):
                        po = PS(4 + hh)
                        cp("act", osb.ap, po.ap[0:64, :], [po.key], [osb.key])
                        act(osq.ap, osb.ap, AF.Square, [osb.key], [osq.key])
                        pb = PS(6)
                        mm(pb.ap[0:64, :], ones_f.ap[0:64, 0:64], osq.ap, True, True, [ones_f.key, osq.key], [pb.key])
                        rsqrt(ors.ap, pb.ap[0:64, :], 1.0 / 64, c_eps_n, [pb.key], [ors.key], otm)
                        tt("dve", osb.ap, osb.ap, ors.ap, ALU.mult, [osb.key, ors.key], [osb.key])
                        ts("dve", ysb.ap[:, hh, :], osb.ap, p64("sbg", hp * 2 + hh), None, ALU.mult, None, [osb.key, "pk64"], [(ysb.key, hh)])
                    for dc in range(KC):
                        pb = PS(6 + dc % 2)
                        for hh in range(2):
                            mm(pb.ap, wout_t[:, hp * 2 + hh, dc * 128:(dc + 1) * 128], ysb.ap[:, hh, :], hh == 0, hh == 1,
                               [woutb.key, (ysb.key, hh)], [pb.key])
                        stt(xT[:, dc, qsl], pb.ap, der[:, 16 + dc:17 + dc], xT[:, dc, qsl], ALU.mult, ALU.add,
                            [pb.key, "der", ("xT", dc, QT)], [("xT", dc, QT)])
            phase_end()
            sub_end()

        def moe_sublayer(l, s):
            sub_begin()
            phase_begin()
            tmpA = [A.alloc("nt%d" % i, 128, [512]) for i in range(4)]
            tmpH = [A.alloc("nh%d" % i, 128, [512]) for i in range(KC)]
            rw = A.alloc("rw", 128, [KC, E])
            dma("sp", rw.ap, rw_d[l].rearrange("(k p) e -> p k e", p=128), [], [rw.key], "misc")
            b2s = A.alloc("b2s", E, [D])
            dma("sp", b2s.ap, b2_d[l], [], [b2s.key], "misc")
            lgp = PS(6)
            lgv = lgp.ap[:, 0:NG * E].rearrange("p (g e) -> p g e", g=NG)

            def router_tile(tti, tiles):
                for g4 in range(4):
                    g = tti * 4 + g4
                    for kc in range(KC):
                        mm(lgv[:, g, :], tiles[kc].ap[:, g4 * 128:(g4 + 1) * 128], rw.ap[:, kc, :], kc == 0, kc == KC - 1,
                           [tiles[kc].key, rw.key], [lgp.key])
            rms_modulate(24, 32, tmpA, router_tile, tmpH)
            lgb = A.alloc("lgb", 128, [NG, E])
            gm = A.alloc("gm", 128, [NG, E])
            top8 = A.alloc("top8", 128, [NG, 8])
            gsum = A.alloc("gsum", 128, [NG, 1])
            o_rb, _ = P128["rb"]
            tt("dve", lgb.ap, lgv, pk128[:, o_rb:o_rb + E].unsqueeze(1).to_broadcast([128, NG, E]), ALU.add, [lgp.key, "pk128"], [lgb.key])
            for g in range(NG):
                P.op("dve", lambda e, g=g: e.max(out=top8.ap[:, g, :], in_=lgb.ap[:, g, :]), [lgb.key], [top8.key])
            tt("dve", gm.ap, lgb.ap, top8.ap[:, :, 3:4].to_broadcast([128, NG, E]), ALU.is_ge, [lgb.key, top8.key], [gm.key])
            tt("dve", lgb.ap, lgb.ap, top8.ap[:, :, 0:1].to_broadcast([128, NG, E]), ALU.subtract, [lgb.key, top8.key], [lgb.key])
            act(lgb.ap, lgb.ap, AF.Exp, [lgb.key], [lgb.key])
            tt("dve", gm.ap, gm.ap, lgb.ap, ALU.mult, [gm.key, lgb.key], [gm.key])
            P.op("dve", lambda e: e.tensor_reduce(out=gsum.ap[:, :, 0], in_=gm.ap, axis=AX.X, op=ALU.add), [gm.key], [gsum.key])
            P.op("dve", lambda e: e.reciprocal(out=gsum.ap, in_=gsum.ap), [gsum.key], [gsum.key])
            tt("dve", gm.ap, gm.ap, gsum.ap.to_broadcast([128, NG, E]), ALU.mult, [gm.key, gsum.key], [gm.key])
            GT = A.alloc("GT", E, [T])
            for tti in range(NTT):
                pb = PS(tti % 2)
                for g4 in range(4):
                    g = tti * 4 + g4
                    tr(pb.ap[0:E, g4 * 128:(g4 + 1) * 128], gm.ap[:, g, :], ident_f.ap, [gm.key, ident_f.key], [pb.key])
                cp("act", GT.ap[:, tti * 512:(tti + 1) * 512], pb.ap[0:E, :], [pb.key], [GT.key])
            dma("sp", gT_scr, GT.ap, [GT.key], ["gT_scr"], "gts")
            for tti in range(NTT):
                tsl = slice(tti * 512, (tti + 1) * 512)
                for dc in range(KC):
                    pb = PS(2 + dc % 2)
                    mm(pb.ap, b2s.ap[:, dc * 128:(dc + 1) * 128], GT.ap[:, tsl], True, True, [b2s.key, GT.key], [pb.key])
                    stt(xT[:, dc, tsl], pb.ap, der[:, 40 + dc:41 + dc], xT[:, dc, tsl], ALU.mult, ALU.add,
                        [pb.key, "der", ("xT", dc, tti)], [("xT", dc, tti)])
            phase_end()
            phase_begin()
            b1T = A.alloc("b1T", 128, [E, 16])
            dma("sp", b1T.ap, b1T_d[l].rearrange("p (e c) -> p e c", e=E), [], [b1T.key], "misc")
            ts("pool", b1T.ap[:, :, 8:16], b1T.ap[:, :, 8:16], 1.0, None, ALU.add, None, [b1T.key], [b1T.key])
            actT = A.alloc("actT", 128, [4, T], BF16)
            gb = [A.alloc("gb%d" % i, 128, [T]) for i in range(2)]
            gt_ = [A.alloc("g_%d" % i, 128, [512]) for i in range(2)]
            lt_ = [A.alloc("l_%d" % i, 128, [512]) for i in range(2)]
            st_ = [A.alloc("s_%d" % i, 128, [512], BF16) for i in range(2)]
            gg_t = [A.alloc("gg_%d" % i, 128, [512]) for i in range(2)]
            ctr = [0]
            pending = [None]

            def flush_pending():
                if pending[0] is not None:
                    f_ = pending[0]
                    pending[0] = None
                    f_()
            loads = []
            for e in range(E):
                for half in range(2):
                    loads += [(w1_d[l, e, half * 2]), (w1_d[l, e, half * 2 + 1]), (w2_d[l, e, half])]
            issued = [0]
            gidx = [0]

            def next_group():
                gi = gidx[0]
                gidx[0] += 1
                while issued[0] < min(gi + 3, len(loads)):
                    i_ = issued[0]
                    sl_ = slot(i_ % 3)
                    dma("pool", sl_.ap.rearrange("p k c -> p (k c)"), loads[i_], [], [sl_.key], "w")
                    issued[0] += 1
                return slot(gi % 3)

            for e in range(E):
                gbe = gb[e % 2]
                dma("sp", gbe.ap, gT_scr[e:e + 1, :].partition_broadcast(128), ["gT_scr"], [gbe.key], "gb")
                for half in range(2):
                    for cgi in range(2):
                        cg = half * 2 + cgi
                        sl = next_group()
                        for tti in range(NTT):
                            tsl = slice(tti * 512, (tti + 1) * 512)
                            for j in range(2):
                                i2 = ctr[0] % 2
                                ctr[0] += 1
                                ch = cg * 2 + j
                                chl = cgi * 2 + j
                                psg, psl = PS(i2 * 2), PS(i2 * 2 + 1)
                                for kc in range(KC):
                                    mm(psg.ap, sl.ap[:, kc, j * 128:(j + 1) * 128], hT[:, kc, tsl], kc == 0, kc == KC - 1,
                                       [sl.key, ("hT", kc, tti)], [psg.key])
                                for kc in range(KC):
                                    mm(psl.ap, sl.ap[:, kc, 256 + j * 128:256 + (j + 1) * 128], hT[:, kc, tsl], kc == 0, kc == KC - 1,
                                       [sl.key, ("hT", kc, tti)], [psl.key])
                                g_, s_, l_, gg_ = gt_[i2], st_[i2], lt_[i2], gg_t[i2]
                                ts("dve", g_.ap, psg.ap, b1T.ap[:, e, ch:ch + 1], 7.0, ALU.add, ALU.min, [psg.key, b1T.key], [g_.key])
                                tt("dve", gg_.ap, g_.ap, gbe.ap[:, tsl], ALU.mult, [g_.key, gbe.key], [gg_.key])
                                act(s_.ap, g_.ap, AF.Sigmoid, [g_.key], [s_.key], scale=ALPHA)
                                ts("dve", l_.ap, psl.ap, b1T.ap[:, e, 8 + ch:9 + ch], -6.0, ALU.add, ALU.max, [psl.key, b1T.key], [l_.key])
                                tt("pool", gg_.ap, gg_.ap, s_.ap, ALU.mult, [gg_.key, s_.key], [gg_.key])
                                flush_pending()
                                pending[0] = (lambda l_=l_, gg_=gg_, chl=chl, tsl=tsl, tti=tti:
                                              stt(actT.ap[:, chl, tsl], l_.ap, 8.0, gg_.ap, ALU.min, ALU.mult,
                                                  [l_.key, gg_.key], [(actT.key, chl, tti)]))
                    flush_pending()
                    sl = next_group()
                    sl4 = sl.ap.rearrange("p k c -> p (k c)").rearrange("p (k c) -> p k c", k=4)
                    for tti in range(NTT):
                        tsl = slice(tti * 512, (tti + 1) * 512)
                        for dc in range(KC):
                            i2 = ctr[0] % 2
                            ctr[0] += 1
                            pb = PS(4 + i2)
                            for kc in range(4):
                                mm(pb.ap, sl4[:, kc, dc * 128:(dc + 1) * 128], actT.ap[:, kc, tsl], kc == 0, kc == 3,
                                   [sl.key, (actT.key, kc, tti)], [pb.key])
                            stt(xT[:, dc, tsl], pb.ap, der[:, 40 + dc:41 + dc], xT[:, dc, tsl], ALU.mult, ALU.add,
                                [pb.key, "der", ("xT", dc, tti)], [("xT", dc, tti)])
            phase_end()
            sub_end()

        for s in range(NSEQ):
            seq_begin()
            for kc in range(KC):
                dma("sp", xT[:, kc, :], xT_d[s, kc * 128:(kc + 1) * 128, :], [("xT", kc, t) for t in range(NTT)],
                    [("xT", kc, t) for t in range(NTT)], "xin")
            for l in range(DEPTH):
                load_layer_params(l, s)
                attention_sublayer(l, s)
                moe_sublayer(l, s)
            for kc in range(KC):
                dma("sp", outT_d[s, kc * 128:(kc + 1) * 128, :], xT[:, kc, :], [("xT", kc, t) for t in range(NTT)],
                    [("out", s, kc)], "xout")
            seq_end()
        P.emit()
    return nc


def pack_params(inp, DEPTH, E):
    pk128 = np.zeros((DEPTH, 128, P128_W), np.float32)
    pk64 = np.zeros((DEPTH, 64, P64_W), np.float32)

    def put128(l, name, arr):
        o, w = P128[name]
        pk128[l, :, o:o + w] = arr

    def put64(l, name, arr):
        o, w = P64[name]
        pk64[l, :, o:o + w] = arr

    def hl(v):
        return np.asarray(v).reshape(8, 64).T
    for l in range(DEPTH):
        put128(l, "ada_b", inp["ada_b"][l].reshape(48, 128).T)
        put128(l, "n1g", inp["norm1_g"][l].reshape(8, 128).T)
        put128(l, "n2g", inp["norm2_g"][l].reshape(8, 128).T)
        mu = inp["shift_mu"][l]
        put128(l, "mu_g", mu[1664:1792].reshape(128, 1))
        put128(l, "qg", np.tile(inp["q_norm_g"][l], 2).reshape(128, 1))
        put128(l, "kg", np.tile(inp["k_norm_g"][l], 2).reshape(128, 1))
        put128(l, "rb", np.broadcast_to(inp["router_b"][l][None, :], (128, E)) if E == 32 else
               np.pad(np.broadcast_to(inp["router_b"][l][None, :], (128, E)), ((0, 0), (0, 32 - E))))
        put64(l, "mu_r", hl(mu[0:512]))
        put64(l, "mu_k", hl(mu[512:1024]))
        put64(l, "mu_v", hl(mu[1024:1536]))
        put64(l, "mu_w", mu[1536:1600].reshape(64, 1))
        put64(l, "mu_a", mu[1600:1664].reshape(64, 1))
        put64(l, "w0", hl(inp["decay_w0"][l]))
        put64(l, "a0", hl(inp["iclr_a0"][l]))
        put64(l, "k_k", hl(inp["k_k"][l]))
        put64(l, "k_a", hl(inp["k_a"][l]))
        put64(l, "r_k", np.asarray(inp["r_k"][l]).T)
        put64(l, "lnw", hl(inp["lnx_w"][l]))
        put64(l, "lnb", hl(inp["lnx_b"][l]))
        if l > 0:
            put64(l, "vrb", hl(inp["vres_b"][l - 1]))
        put64(l, "sbg", hl(inp["sb_out_g"][l]))
    b1T = np.ascontiguousarray(
        np.asarray(inp["exp_b1"]).reshape(DEPTH, E, 16, 128).transpose(0, 3, 1, 2).reshape(DEPTH, 128, E * 16))
    return pk128, pk64, b1T


_CACHE = {}


def run_model(inp, T, NB, E, DEPTH, n_cores):
    NSEQ = NB // n_cores
    key = (T, NSEQ, E, DEPTH)
    if key not in _CACHE:
        _CACHE[key] = build_program(T, NSEQ, E, DEPTH)
    nc = _CACHE[key]
    f = lambda a: np.ascontiguousarray(np.asarray(a, dtype=np.float32))
    pk128, pk64, b1T = pack_params(inp, DEPTH, E)
    x = np.asarray(inp["x"], dtype=np.float32)
    c = np.asarray(inp["c"], dtype=np.float32)
    w1 = np.asarray(inp["exp_w1"], dtype=np.float32).reshape(DEPTH, E, 8, 128, 2, 4, 256)
    w1r = np.ascontiguousarray(w1.transpose(0, 1, 5, 3, 2, 4, 6)).reshape(DEPTH, E, 4, 128, 4096)
    w2 = np.asarray(inp["exp_w2"], dtype=np.float32).reshape(DEPTH, E, 2, 4, 128, 1024)
    w2r = np.ascontiguousarray(w2.transpose(0, 1, 2, 4, 3, 5)).reshape(DEPTH, E, 2, 128, 4096)
    shared = {
        "ada_w": f(inp["ada_w"]), "w_in": f(inp["w_in"]), "w_out": f(inp["w_out"]),
        "exp_w1r": w1r, "exp_w2r": w2r, "pk128": pk128, "pk64": pk64, "b1T": b1T,
        "exp_b2": f(inp["exp_b2"]), "router_w": f(inp["router_w"]), "decay_up": f(inp["decay_up"]),
        "iclr_up": f(inp["iclr_up"]), "gate_up": f(inp["gate_up"]),
        "vres_down": f(inp["vres_down"]), "vres_up": f(inp["vres_up"]),
    }
    in_maps = []
    for ci in range(n_cores):
        xs = x[ci * NSEQ:(ci + 1) * NSEQ]
        cs = c[ci * NSEQ:(ci + 1) * NSEQ]
        m = dict(shared)
        m["xT"] = np.ascontiguousarray(xs.transpose(0, 2, 1))
        m["cT"] = np.ascontiguousarray(cs.T.reshape(KC, 128, NSEQ).transpose(1, 0, 2))
        in_maps.append(m)
    res = run_bass_kernel_spmd(nc, in_maps, core_ids=list(range(n_cores)))
    outs = [np.asarray(r["outT"]).transpose(0, 2, 1) for r in res.results]
    return np.ascontiguousarray(np.concatenate(outs, axis=0).astype(np.float32))


def kernel(**inputs):
    return run_model(inputs, T=2048, NB=32, E=32, DEPTH=2, n_cores=8)
```
